# Optimizing a Trainium2 kernel written in Bass

```python
import math
import jax, jax.numpy as jnp
from jax import lax
import numpy as np

D_MODEL = 2048
BATCH = 2
SEQ = 16384
DEPTH = 2

N_MIXERS = 2
N_ATTN_LAYERS = (DEPTH + N_MIXERS - 1) // N_MIXERS
N_DN_LAYERS = DEPTH // N_MIXERS

DEEPNORM_ALPHA = (2.0 * DEPTH) ** 0.25
DEEPNORM_BETA = (8.0 * DEPTH) ** -0.25
LN_EPS = 1e-5

ATTN_HEAD_DIM = 128
ATTN_HEADS_PER_GROUP = 8
DILATED_GROUPS = ((128, 1), (512, 4), (2048, 16))
N_GROUPS = len(DILATED_GROUPS)
ATTN_WIDTH = ATTN_HEADS_PER_GROUP * ATTN_HEAD_DIM
ATTN_IN_WIDTH = N_GROUPS * 3 * ATTN_WIDTH
ATTN_BLOCK = 128

NUM_BUCKETS = 32
MAX_DISTANCE = 2048

DN_QK_HEADS = 16
DN_V_HEADS = 32
DN_HEAD_K = 128
DN_HEAD_V = 128
DN_CONV = 4
DN_CHUNK = 64
DN_Q_W = DN_QK_HEADS * DN_HEAD_K
DN_V_W = DN_V_HEADS * DN_HEAD_V
DN_CONV_W = 2 * DN_Q_W + DN_V_W
DN_IN_WIDTH = DN_CONV_W + DN_V_W + 2 * DN_V_HEADS
DN_EPS = 1e-6

N_EXPERTS = 32
TOP_K = 4
D_FF = 2048
SWIGLU_LIMIT = 7.0
SWIGLU_ALPHA = 1.702
MOE_BLOCK = 256

kernel_name = 'hybrid_dilated_attn_gdn_moe_deepnorm'


def layer_norm(x, g, b):
    xf = x.astype(jnp.float32)
    mu = jnp.mean(xf, axis=-1, keepdims=True)
    var = jnp.mean(jnp.square(xf - mu), axis=-1, keepdims=True)
    return ((xf - mu) * lax.rsqrt(var + LN_EPS) * g + b).astype(x.dtype)


def t5_bucket(dist):
    max_exact = NUM_BUCKETS // 2
    d_f = jnp.maximum(dist, 1).astype(jnp.float32)
    large = max_exact + (jnp.log(d_f / max_exact) / math.log(MAX_DISTANCE / max_exact)
                         * (NUM_BUCKETS - max_exact)).astype(jnp.int32)
    large = jnp.minimum(large, NUM_BUCKETS - 1)
    return jnp.where(dist < max_exact, dist, large)


def dilated_group_attention(q, k, v, bias_table, window, dilation):
    B_, S_, H, HD = q.shape
    d = dilation
    J = window // d
    L = S_ // d
    bq = min(ATTN_BLOCK, L)
    nb = -(-L // bq)
    Lp = nb * bq
    n_prev = -(-J // bq)

    def streams(t):
        return jnp.transpose(t.reshape(B_, L, d, H, HD), (0, 2, 1, 3, 4))

    qs = jnp.pad(streams(q), ((0, 0), (0, 0), (0, Lp - L), (0, 0), (0, 0)))
    qs = qs.reshape(B_, d, nb, bq, H, HD)
    kv_pad = ((0, 0), (0, 0), (n_prev * bq, Lp - L), (0, 0), (0, 0))
    ks = jnp.pad(streams(k), kv_pad).reshape(B_, d, nb + n_prev, bq, H, HD)
    vs = jnp.pad(streams(v), kv_pad).reshape(B_, d, nb + n_prev, bq, H, HD)
    kw = jnp.concatenate([ks[:, :, i:i + nb] for i in range(n_prev + 1)], axis=3)
    vw = jnp.concatenate([vs[:, :, i:i + nb] for i in range(n_prev + 1)], axis=3)
    W = (n_prev + 1) * bq

    a_idx = jnp.arange(bq, dtype=jnp.int32)
    c_idx = jnp.arange(W, dtype=jnp.int32)
    steps = a_idx[:, None] + n_prev * bq - c_idx[None, :]
    k_pos = (jnp.arange(nb, dtype=jnp.int32)[:, None, None] * bq - n_prev * bq
             + c_idx[None, None, :])
    valid = (steps >= 0)[None] & (steps <= J)[None] & (k_pos >= 0)
    bucket = t5_bucket(jnp.maximum(steps, 0) * d)
    bias = jnp.transpose(bias_table[bucket], (2, 0, 1)).astype(jnp.float32)

    logits = jnp.einsum('brnqhd,brnkhd->brnhqk', qs, kw).astype(jnp.float32) * (HD ** -0.5) + bias
    logits = jnp.where(valid[:, None], logits, -jnp.inf)
    lse = jax.nn.logsumexp(logits, axis=-1)
    p = jnp.exp(logits - lse[..., None])
    o = jnp.einsum('brnhqk,brnkhd->brnqhd', p.astype(v.dtype), vw)

    o = o.reshape(B_, d, Lp, H, HD)[:, :, :L]
    o = jnp.transpose(o, (0, 2, 1, 3, 4)).reshape(B_, S_, H, HD)
    lse = jnp.transpose(lse, (0, 1, 2, 4, 3)).reshape(B_, d, Lp, H)[:, :, :L]
    lse = jnp.transpose(lse, (0, 2, 1, 3)).reshape(B_, S_, H)
    return o, lse


def dilated_attention_mixer(x, w_in, w_out, rel_bias):
    B_, S_, _ = x.shape
    H = ATTN_HEADS_PER_GROUP
    proj = x @ w_in
    outs, lses = [], []
    for g, (window, dilation) in enumerate(DILATED_GROUPS):
        qkv = proj[..., g * 3 * ATTN_WIDTH:(g + 1) * 3 * ATTN_WIDTH].reshape(B_, S_, 3, H, ATTN_HEAD_DIM)
        o, lse = dilated_group_attention(qkv[:, :, 0], qkv[:, :, 1], qkv[:, :, 2],
                                         rel_bias[:, g * H:(g + 1) * H], window, dilation)
        outs.append(o)
        lses.append(lse)
    wts = jax.nn.softmax(jnp.stack(lses), axis=0)
    o = jnp.einsum('gbsh,gbshd->bshd', wts, jnp.stack(outs).astype(jnp.float32)).astype(x.dtype)
    return o.reshape(B_, S_, ATTN_WIDTH) @ w_out


def causal_short_conv(x, w):
    K = w.shape[0]
    S_ = x.shape[1]
    xp = jnp.pad(x, ((0, 0), (K - 1, 0), (0, 0)))
    return sum(xp[:, j:j + S_] * w[j] for j in range(K))


def l2_normalize(t):
    tf = t.astype(jnp.float32)
    return tf * lax.rsqrt(jnp.sum(tf * tf, axis=-1, keepdims=True) + DN_EPS)


def chunk_gated_delta_rule(q, k, v, g, beta):
    B_, S_, H, DK = q.shape
    DV = v.shape[-1]
    C = DN_CHUNK
    N = S_ // C

    def chunks(t):
        t = t.astype(jnp.float32).reshape((B_, N, C, H) + t.shape[3:])
        return jnp.moveaxis(t, 3, 1)

    qc = chunks(q) * (DK ** -0.5)
    kc = chunks(k)
    vc = chunks(v)
    bc = chunks(beta)
    gc = jnp.cumsum(chunks(g), axis=-1)
    incl = jnp.tril(jnp.ones((C, C), dtype=bool))
    strict = jnp.tril(jnp.ones((C, C), dtype=bool), -1)
    decay = jnp.exp(jnp.where(incl, gc[..., :, None] - gc[..., None, :], -jnp.inf))
    kb = kc * bc[..., None]
    a_mat = jnp.where(strict, jnp.einsum('bhnid,bhnjd->bhnij', kb, kc) * decay, 0.0)
    eye = jnp.eye(C, dtype=jnp.float32)
    rhs = jnp.concatenate([vc * bc[..., None], kb * jnp.exp(gc)[..., None]], axis=-1)
    uw = lax.linalg.triangular_solve(eye + a_mat, rhs, left_side=True, lower=True, unit_diagonal=True)
    u, w = uw[..., :DV], uw[..., DV:]
    qk = jnp.einsum('bhnid,bhnjd->bhnij', qc, kc) * decay

    def step(state, inp):
        q_i, k_i, u_i, w_i, qk_i, g_i = inp
        v_new = u_i - jnp.einsum('bhck,bhkv->bhcv', w_i, state)
        o_i = (jnp.einsum('bhck,bhkv->bhcv', q_i * jnp.exp(g_i)[..., None], state)
               + jnp.einsum('bhij,bhjv->bhiv', qk_i, v_new))
        g_last = g_i[..., -1]
        k_dec = k_i * jnp.exp(g_last[..., None] - g_i)[..., None]
        state = state * jnp.exp(g_last)[..., None, None] + jnp.einsum('bhck,bhcv->bhkv', k_dec, v_new)
        return state, o_i

    xs = tuple(jnp.moveaxis(t, 2, 0) for t in (qc, kc, u, w, qk, gc))
    state0 = jnp.zeros((B_, H, DK, DV), jnp.float32)
    _, o = lax.scan(step, state0, xs)
    return jnp.transpose(o, (1, 0, 3, 2, 4)).reshape(B_, S_, H, DV)


def gated_deltanet_mixer(x, w_in, conv_w, a_log, dt_bias, norm_g, w_out):
    B_, S_, _ = x.shape
    proj = x @ w_in
    o1 = DN_CONV_W
    o2 = o1 + DN_V_W
    o3 = o2 + DN_V_HEADS
    qkv = jax.nn.silu(causal_short_conv(proj[..., :o1], conv_w))
    z = proj[..., o1:o2].reshape(B_, S_, DN_V_HEADS, DN_HEAD_V)
    b_raw = proj[..., o2:o3]
    a_raw = proj[..., o3:]
    rep = DN_V_HEADS // DN_QK_HEADS
    q = jnp.repeat(l2_normalize(qkv[..., :DN_Q_W].reshape(B_, S_, DN_QK_HEADS, DN_HEAD_K)), rep, axis=2)
    k = jnp.repeat(l2_normalize(qkv[..., DN_Q_W:2 * DN_Q_W].reshape(B_, S_, DN_QK_HEADS, DN_HEAD_K)), rep, axis=2)
    v = qkv[..., 2 * DN_Q_W:].reshape(B_, S_, DN_V_HEADS, DN_HEAD_V)
    beta = jax.nn.sigmoid(b_raw.astype(jnp.float32))
    g = -jnp.exp(a_log.astype(jnp.float32)) * jax.nn.softplus(a_raw.astype(jnp.float32)
                                                           + dt_bias.astype(jnp.float32))
    o = chunk_gated_delta_rule(q, k, v, g, beta)
    o = o * lax.rsqrt(jnp.mean(o * o, axis=-1, keepdims=True) + DN_EPS) * norm_g
    o = o * jax.nn.silu(z.astype(jnp.float32))
    return o.astype(x.dtype).reshape(B_, S_, DN_V_W) @ w_out


def clamped_swiglu(h):
    glu = jnp.minimum(h[..., :D_FF], SWIGLU_LIMIT)
    lin = jnp.clip(h[..., D_FF:], -SWIGLU_LIMIT, SWIGLU_LIMIT)
    return glu * jax.nn.sigmoid(SWIGLU_ALPHA * glu) * (lin + 1.0)


def moe_ffn(x, router_w, router_b, w_gate_up, b_gate_up, w_down, b_down):
    B_, S_, D_ = x.shape
    n_tok = B_ * S_
    x2d = x.reshape(n_tok, D_)
    logits = (x2d @ router_w).astype(jnp.float32) + router_b.astype(jnp.float32)
    top_logits, top_idx = lax.top_k(logits, TOP_K)
    gates = jax.nn.softmax(top_logits, axis=-1)
    n_pairs = n_tok * TOP_K
    flat_e = top_idx.reshape(n_pairs).astype(jnp.int32)
    flat_tok = jnp.repeat(jnp.arange(n_tok, dtype=jnp.int32), TOP_K)
    flat_gate = gates.reshape(n_pairs)
    order = jnp.argsort(flat_e)
    e_sorted = flat_e[order]
    counts = jnp.bincount(flat_e, length=N_EXPERTS).astype(jnp.int32)
    starts = jnp.cumsum(counts) - counts
    padded = (counts + MOE_BLOCK - 1) // MOE_BLOCK * MOE_BLOCK
    padded_end = jnp.cumsum(padded)
    padded_start = padded_end - padded
    dest = padded_start[e_sorted] + (jnp.arange(n_pairs, dtype=jnp.int32) - starts[e_sorted])
    n_blocks = -(-(n_pairs + N_EXPERTS * (MOE_BLOCK - 1)) // MOE_BLOCK)
    n_slots = n_blocks * MOE_BLOCK
    slot_tok = jnp.full((n_slots,), n_tok, jnp.int32).at[dest].set(flat_tok[order])
    slot_gate = jnp.zeros((n_slots,), jnp.float32).at[dest].set(flat_gate[order])
    block_expert = jnp.minimum(
        jnp.searchsorted(padded_end, jnp.arange(n_blocks, dtype=jnp.int32) * MOE_BLOCK, side='right'),
        N_EXPERTS - 1)

    def expert_block(args):
        tok_b, gate_b, e = args
        xb = x2d.at[tok_b].get(mode='fill', fill_value=0)
        h = xb @ w_gate_up[e] + b_gate_up[e]
        yb = clamped_swiglu(h) @ w_down[e] + b_down[e]
        return yb * gate_b[:, None].astype(yb.dtype)

    ys = lax.map(expert_block, (slot_tok.reshape(n_blocks, MOE_BLOCK),
                                slot_gate.reshape(n_blocks, MOE_BLOCK), block_expert))
    y = jax.ops.segment_sum(ys.reshape(n_slots, D_), slot_tok, num_segments=n_tok + 1)[:n_tok]
    return y.reshape(B_, S_, D_)


def setup_inputs(seed: int = 0) -> dict:
    key = jax.random.key(seed)
    ks = jax.random.split(key, 20)
    f32 = jnp.float32

    def nrm(k, shape, scale):
        return jax.random.normal(k, shape, f32) * scale

    x = nrm(ks[0], (BATCH, SEQ, D_MODEL), 1.0)
    rel_bias = nrm(ks[1], (NUM_BUCKETS, N_GROUPS * ATTN_HEADS_PER_GROUP), 0.2)
    attn_w_in = nrm(ks[2], (N_ATTN_LAYERS, D_MODEL, ATTN_IN_WIDTH), D_MODEL ** -0.5)
    attn_w_out = nrm(ks[3], (N_ATTN_LAYERS, ATTN_WIDTH, D_MODEL), DEEPNORM_BETA * ATTN_WIDTH ** -0.5)
    dn_w_in = nrm(ks[4], (N_DN_LAYERS, D_MODEL, DN_IN_WIDTH), D_MODEL ** -0.5)
    dn_conv_w = nrm(ks[5], (N_DN_LAYERS, DN_CONV, DN_CONV_W), DN_CONV ** -0.5)
    dn_a_log = jnp.log(jax.random.uniform(ks[6], (N_DN_LAYERS, DN_V_HEADS), f32, 1.0, 16.0))
    dt = jnp.exp(jax.random.uniform(ks[7], (N_DN_LAYERS, DN_V_HEADS), f32,
                                    math.log(1e-3), math.log(1e-1)))
    dn_dt_bias = dt + jnp.log(-jnp.expm1(-dt))
    dn_norm_g = 1.0 + nrm(ks[8], (N_DN_LAYERS, DN_HEAD_V), 0.02)
    dn_w_out = nrm(ks[9], (N_DN_LAYERS, DN_V_W, D_MODEL), DEEPNORM_BETA * DN_V_W ** -0.5)
    ln1_g = 1.0 + nrm(ks[10], (DEPTH, D_MODEL), 0.02)
    ln1_b = nrm(ks[11], (DEPTH, D_MODEL), 0.02)
    router_w = nrm(ks[12], (DEPTH, D_MODEL, N_EXPERTS), D_MODEL ** -0.5)
    router_b = nrm(ks[13], (DEPTH, N_EXPERTS), 0.01)
    w_gate_up = nrm(ks[14], (DEPTH, N_EXPERTS, D_MODEL, 2 * D_FF), D_MODEL ** -0.5)
    b_gate_up = nrm(ks[15], (DEPTH, N_EXPERTS, 2 * D_FF), 0.01)
    w_down = nrm(ks[16], (DEPTH, N_EXPERTS, D_FF, D_MODEL), DEEPNORM_BETA * D_FF ** -0.5)
    b_down = nrm(ks[17], (DEPTH, N_EXPERTS, D_MODEL), 0.01)
    ln2_g = 1.0 + nrm(ks[18], (DEPTH, D_MODEL), 0.02)
    ln2_b = nrm(ks[19], (DEPTH, D_MODEL), 0.02)
    return {'x': x, 'rel_bias': rel_bias, 'attn_w_in': attn_w_in, 'attn_w_out': attn_w_out,
            'dn_w_in': dn_w_in, 'dn_conv_w': dn_conv_w, 'dn_a_log': dn_a_log, 'dn_dt_bias': dn_dt_bias,
            'dn_norm_g': dn_norm_g, 'dn_w_out': dn_w_out, 'ln1_g': ln1_g, 'ln1_b': ln1_b,
            'router_w': router_w, 'router_b': router_b, 'w_gate_up': w_gate_up, 'b_gate_up': b_gate_up,
            'w_down': w_down, 'b_down': b_down, 'ln2_g': ln2_g, 'ln2_b': ln2_b}


def reference(x, rel_bias, attn_w_in, attn_w_out, dn_w_in, dn_conv_w, dn_a_log, dn_dt_bias,
              dn_norm_g, dn_w_out, ln1_g, ln1_b, router_w, router_b, w_gate_up, b_gate_up,
              w_down, b_down, ln2_g, ln2_b):
    for i in range(DEPTH):
        j = i // N_MIXERS
        if i % N_MIXERS == 0:
            h = dilated_attention_mixer(x, attn_w_in[j], attn_w_out[j], rel_bias)
        else:
            h = gated_deltanet_mixer(x, dn_w_in[j], dn_conv_w[j], dn_a_log[j], dn_dt_bias[j],
                                     dn_norm_g[j], dn_w_out[j])
        x = layer_norm(DEEPNORM_ALPHA * x + h, ln1_g[i], ln1_b[i])
        f = moe_ffn(x, router_w[i], router_b[i], w_gate_up[i], b_gate_up[i], w_down[i], b_down[i])
        x = layer_norm(DEEPNORM_ALPHA * x + f, ln2_g[i], ln2_b[i])
    return x
```

```python
import numpy as np
import concourse.bass as bass
import concourse.mybir as mybir
from contextlib import ExitStack
from concourse.bass_utils import run_bass_kernel_spmd

F32 = mybir.dt.float32
BF16 = mybir.dt.bfloat16
I32 = mybir.dt.int32
ALU = mybir.AluOpType
AF = mybir.ActivationFunctionType
AX = mybir.AxisListType


class Prog:
    ENG = ('pe', 'dve', 'act', 'pool', 'sp')
    EPOCH = 20000
    ND = 40
    NDI = 4

    def __init__(self, nc, es):
        self.nc, self.es = nc, es
        self.q = {e: [] for e in self.ENG}
        self.cnt = {e: 0 for e in self.ENG}
        self.epoch = {e: 0 for e in self.ENG}
        self.sems = {}
        self.seen = {e: {} for e in self.ENG}
        self.lastw = {}
        self.reads = {}
        self.dcnt = [0] * (self.ND + self.NDI)
        self.dnext = 0
        self.inext = 0
        self.nsem = 0
        self.ntile = 0

    def sem(self, key):
        if key not in self.sems:
            self.nsem += 1
            self.sems[key] = self.es.enter_context(self.nc.semaphore('s%d' % self.nsem))
        return self.sems[key]

    def sb(self, shape, dt, name=None):
        self.ntile += 1
        return self.es.enter_context(self.nc.sbuf_tensor(name or 't%d' % self.ntile, list(shape), dt))

    def ps(self, shape, dt, name=None):
        self.ntile += 1
        return self.es.enter_context(self.nc.psum_tensor(name or 'p%d' % self.ntile, list(shape), dt))

    def _wait(self, eng, tok):
        key, val = tok
        if eng == 'pe' and key[0] == 'e' and key[1] == 'pe':
            return
        if self.seen[eng].get(key, 0) >= val:
            return
        self.seen[eng][key] = val
        s = self.sem(key)
        self.q[eng].append(lambda e, s=s, val=val: e.wait_ge(s, val))

    def _deps(self, eng, reads, writes):
        for k in reads:
            t = self.lastw.get(k)
            if t:
                self._wait(eng, t)
        for k in writes:
            t = self.lastw.get(k)
            if t:
                self._wait(eng, t)
            for t in self.reads.get(k, ()):
                self._wait(eng, t)

    def _commit(self, tok, reads, writes):
        for k in reads:
            lst = self.reads.setdefault(k, [])
            for i, t in enumerate(lst):
                if t[0] == tok[0]:
                    lst[i] = tok
                    break
            else:
                lst.append(tok)
        for k in writes:
            self.lastw[k] = tok
            self.reads[k] = []

    def op(self, eng, fn, reads=(), writes=()):
        self._deps(eng, reads, writes)
        self.cnt[eng] += 1
        if self.cnt[eng] > self.EPOCH:
            self.epoch[eng] += 1
            self.cnt[eng] = 1
        key = ('e', eng, self.epoch[eng])
        s = self.sem(key)
        self.q[eng].append(lambda e, s=s, fn=fn: fn(e).then_inc(s, 1))
        self._commit((key, self.cnt[eng]), reads, writes)

    def dma(self, eng, fn, reads=(), writes=(), ind=False):
        if ind:
            i = self.ND + self.inext
            self.inext = (self.inext + 1) % self.NDI
        else:
            i = self.dnext
            self.dnext = (i + 1) % self.ND
        key = ('d', i)
        if self.dcnt[i]:
            self._wait(eng, (key, self.dcnt[i]))
        self._deps(eng, reads, writes)
        self.dcnt[i] += 16
        s = self.sem(key)
        self.q[eng].append(lambda e, s=s, fn=fn: fn(e).then_inc(s, 16))
        self._commit((key, self.dcnt[i]), reads, writes)

    def finish(self):
        for i in range(self.ND + self.NDI):
            if self.dcnt[i]:
                self._wait('sp', (('d', i), self.dcnt[i]))
        for e in ('pe', 'dve', 'act', 'pool'):
            if self.cnt[e]:
                self._wait('sp', (('e', e, self.epoch[e]), self.cnt[e]))
        self.flush()

    def flush(self):
        q = self.q
        self.q = {e: [] for e in self.ENG}
        with self.nc.Block() as block:
            @block.tensor
            def _(e):
                for f in q['pe']:
                    f(e)

            @block.vector
            def _(e):
                for f in q['dve']:
                    f(e)

            @block.scalar
            def _(e):
                for f in q['act']:
                    f(e)

            @block.gpsimd
            def _(e):
                for f in q['pool']:
                    f(e)

            @block.sync
            def _(e):
                for f in q['sp']:
                    f(e)

    def mm(self, out, lhsT, rhs, start=True, stop=True, reads=(), writes=()):
        self.op('pe', lambda e: e.matmul(out, lhsT, rhs, start=start, stop=stop), reads, writes)

    def tr(self, out, in_, ident, reads=(), writes=()):
        self.op('pe', lambda e: e.transpose(out, in_, ident), reads, writes)

    def ld(self, out, in_, reads=(), writes=(), eng='sp'):
        self.dma(eng, lambda e: e.dma_start(out=out, in_=in_), reads, writes)

    def barrier(self):
        toks = []
        for i in range(self.ND + self.NDI):
            if self.dcnt[i]:
                toks.append((('d', i), self.dcnt[i]))
        for e in ('pe', 'dve', 'act', 'pool'):
            for ep in range(self.epoch[e] + 1):
                c = self.cnt[e] if ep == self.epoch[e] else self.EPOCH
                if c:
                    toks.append((('e', e, ep), c))
        for eng in self.ENG:
            for t in toks:
                self._wait(eng, t)
        self.lastw = {}
        self.reads = {}


D = 2048
ALPHA = 2.0 ** 0.5
LN_EPS = 1e-5
NE = 32
FF = 2048


def consts_np():
    c = {}
    c['c_ident'] = np.eye(128, dtype=np.float32)
    c['c_uts'] = np.triu(np.ones((128, 128), np.float32), 1)
    c['c_uti'] = np.triu(np.ones((128, 128), np.float32), 0)
    c['c_ones'] = np.ones((128, 128), np.float32)
    return c


def load_consts(P, nc, names):
    out = {}
    for n in names:
        d = nc.dram_tensor(n, [128, 128], F32, kind="ExternalInput").ap()
        t = P.sb([128, 128], F32, name='sb_' + n)
        P.ld(t[:], d, writes=[n])
        out[n] = t
    return out


def emit_ln(P, src, dst, gt, bt, tmpk, keys_r, keys_w, st, ag, rstd):
    for i in range(4):
        P.op('dve', lambda e, i=i: e.bn_stats(st[:, i * 6:(i + 1) * 6], src[:, i * 512:(i + 1) * 512]), keys_r, [tmpk + 'st'])
    P.op('dve', lambda e: e.bn_aggr(ag[:], st[:]), [tmpk + 'st'], [tmpk + 'ag'])
    P.op('dve', lambda e: e.tensor_scalar(rstd[:], ag[:, 1:2], LN_EPS, None, ALU.add), [tmpk + 'ag'], [tmpk + 'rs'])
    P.op('act', lambda e: e.activation(rstd[:], rstd[:], AF.Sqrt), [tmpk + 'rs'], [tmpk + 'rs'])
    P.op('dve', lambda e: e.reciprocal(rstd[:], rstd[:]), [tmpk + 'rs'], [tmpk + 'rs'])
    P.op('dve', lambda e: e.tensor_scalar(dst[:], src[:], ag[:, 0:1], rstd[:, 0:1], ALU.subtract, ALU.mult),
         list(keys_r) + [tmpk + 'ag', tmpk + 'rs'], keys_w)
    P.op('pool', lambda e: e.tensor_tensor(dst[:], dst[:], gt[:], ALU.mult), list(keys_w) + ['lng'], keys_w)
    P.op('pool', lambda e: e.tensor_tensor(dst[:], dst[:], bt[:], ALU.add), list(keys_w) + ['lnb'], keys_w)


def emit_moe(P, nc, es, NT, C, x_in, out_d, W, cst, dbg=False):
    NSLOT = NE * C
    NB = C // 128
    H = C // 2
    BIG = float(NSLOT + 4096)
    kind = "ExternalOutput" if dbg else "Internal"
    xg = nc.dram_tensor("moe_xg", [NSLOT + 1, D], BF16, kind=kind).ap()
    yd = nc.dram_tensor("moe_y", [NSLOT + 1, D], F32, kind=kind).ap()
    if dbg:
        d_sl = nc.dram_tensor("dbg_slots", [128, NT * 4], I32, kind="ExternalOutput").ap()
        d_ga = nc.dram_tensor("dbg_gates", [128, NT * 4], F32, kind="ExternalOutput").ap()
    ident, uts, ones = cst['c_ident'], cst['c_uts'], cst['c_ones']
    slots_all = P.sb([128, NT, 4], I32, name='slots_all')
    gates_all = P.sb([128, NT, 4], F32, name='gates_all')
    identb = P.sb([128, 128], BF16, name='identb')
    P.op('dve', lambda e: e.tensor_copy(identb[:], ident[:]), ['c_ident'], ['identb'])
    scat_keys = []
    with ExitStack() as ph:
        def sb(shape, dt, name):
            return ph.enter_context(nc.sbuf_tensor(name, list(shape), dt))

        def ps(name):
            return ph.enter_context(nc.psum_tensor(name, [128, 512], F32))
        xt = [sb([128, D], F32, 'r_xt%d' % i) for i in range(2)]
        xb = [sb([128, D], BF16, 'r_xb%d' % i) for i in range(2)]
        xT = sb([128, 16, 128], F32, 'r_xT')
        rw = sb([128, 16, NE], F32, 'r_rw')
        rb = sb([128, NE], F32, 'r_rb')
        eoff = sb([128, NE], F32, 'r_eoff')
        base = sb([128, NE], F32, 'r_base')
        zt = sb([128, D], F32, 'r_zero')
        pt = [ps('r_pt%d' % i) for i in range(2)]
        pl = ps('r_pl'); pr = ps('r_pr'); pb = ps('r_pb')
        sm = {n: sb([128, NE], F32, 'r_' + n) for n in ('lg', 'mask', 'ex', 'G', 'rank', 'ov', 'slot', 'v', 'oh')}
        t8 = sb([128, 8], F32, 'r_t8'); v8 = sb([128, 8], F32, 'r_v8')
        s1 = {n: sb([128, 1], F32, 'r_' + n) for n in ('negm', 'ssum', 'rs')}
        slotf = sb([128, 4], F32, 'r_slotf')
        P.ld(rw[:], W['router_w'].rearrange("(k p) e -> p k e", p=128), writes=['rw'])
        P.ld(rb[:], W['router_b'].partition_broadcast(128), writes=['rb'])
        P.op('pool', lambda e: e.iota(eoff[:], [[C, NE]], base=0, channel_multiplier=0, allow_small_or_imprecise_dtypes=True), [], ['eoff'])
        P.op('pool', lambda e: e.memset(base[:], 0.0), [], ['base'])
        P.op('pool', lambda e: e.memset(zt[:], 0.0), [], ['zt'])
        P.ld(yd[NSLOT:NSLOT + 1, :], zt[0:1, :], reads=['zt'], writes=['ydummy'])
        for t in range(NT):
            b = t % 2
            kx, kb = 'xt%d' % b, 'xb%d' % b
            P.ld(xt[b][:], x_in[t * 128:(t + 1) * 128, :], writes=[kx])
            P.op('act', lambda e, b=b: e.copy(xb[b][:], xt[b][:]), [kx], [kb])
            for q in range(4):
                pq = pt[q % 2]
                kp = 'pt%d' % (q % 2)
                for j in range(4):
                    k = q * 4 + j
                    P.tr(pq[:, j * 128:(j + 1) * 128], xt[b][:, k * 128:(k + 1) * 128], ident[:], [kx, 'c_ident'], [kp])
                eng = 'dve' if q % 2 == 0 else 'act'
                if eng == 'dve':
                    P.op('dve', lambda e, q=q, pq=pq: e.tensor_copy(xT[:, q * 4:(q + 1) * 4, :], pq[:].rearrange("p (a b) -> p a b", a=4)), [kp], ['xT%d' % q])
                else:
                    P.op('act', lambda e, q=q, pq=pq: e.copy(xT[:, q * 4:(q + 1) * 4, :], pq[:].rearrange("p (a b) -> p a b", a=4)), [kp], ['xT%d' % q])
            for k in range(16):
                P.mm(pl[:, 0:NE], xT[:, k, :], rw[:, k, :], k == 0, k == 15, ['xT%d' % (k // 4), 'rw'], ['pl'])
            lg, mask, ex, G, rank, ov, slot, v, oh = (sm[n] for n in ('lg', 'mask', 'ex', 'G', 'rank', 'ov', 'slot', 'v', 'oh'))
            P.op('dve', lambda e: e.tensor_tensor(lg[:], pl[:, 0:NE], rb[:], ALU.add), ['pl', 'rb'], ['lg'])
            P.op('dve', lambda e: e.max(t8[:], lg[:]), ['lg'], ['t8'])
            P.op('dve', lambda e: e.tensor_scalar(mask[:], lg[:], t8[:, 3:4], None, ALU.is_ge), ['lg', 't8'], ['mask'])
            P.op('dve', lambda e: e.tensor_scalar(s1['negm'][:], t8[:, 0:1], -1.0, None, ALU.mult), ['t8'], ['negm'])
            P.op('act', lambda e: e.activation(ex[:], lg[:], AF.Exp, bias=s1['negm'][:, 0:1], scale=1.0), ['lg', 'negm'], ['ex'])
            P.op('dve', lambda e: e.tensor_tensor(ex[:], ex[:], mask[:], ALU.mult), ['ex', 'mask'], ['ex'])
            P.op('dve', lambda e: e.reduce_sum(s1['ssum'][:], ex[:], AX.X), ['ex'], ['ssum'])
            P.op('dve', lambda e: e.reciprocal(s1['rs'][:], s1['ssum'][:]), ['ssum'], ['rs'])
            P.op('dve', lambda e: e.tensor_scalar(G[:], ex[:], s1['rs'][:, 0:1], None, ALU.mult), ['ex', 'rs'], ['G'])
            P.mm(pr[:, 0:NE], uts[:], mask[:], True, True, ['c_uts', 'mask'], ['pr'])
            P.mm(pb[:, 0:NE], ones[:], mask[:], True, True, ['c_ones', 'mask'], ['pb'])
            P.op('dve', lambda e: e.tensor_tensor(rank[:], pr[:, 0:NE], base[:], ALU.add), ['pr', 'base'], ['rank'])
            P.op('dve', lambda e: e.tensor_tensor(base[:], pb[:, 0:NE], base[:], ALU.add), ['pb', 'base', 'rank'], ['base'])
            P.op('dve', lambda e: e.tensor_scalar(ov[:], rank[:], float(C), None, ALU.is_lt), ['rank'], ['ov'])
            P.op('dve', lambda e: e.tensor_tensor(slot[:], rank[:], eoff[:], ALU.add), ['rank', 'eoff'], ['slot'])
            P.op('dve', lambda e: e.tensor_scalar(slot[:], slot[:], float(-NSLOT), None, ALU.add), ['slot'], ['slot'])
            P.op('dve', lambda e: e.tensor_tensor(slot[:], slot[:], ov[:], ALU.mult), ['slot', 'ov'], ['slot'])
            P.op('dve', lambda e: e.tensor_scalar(slot[:], slot[:], float(NSLOT), None, ALU.add), ['slot'], ['slot'])
            P.op('dve', lambda e: e.tensor_tensor(G[:], G[:], ov[:], ALU.mult), ['G', 'ov'], ['G'])
            P.op('dve', lambda e: e.tensor_scalar(v[:], slot[:], -1.0, BIG, ALU.mult, ALU.add), ['slot'], ['v'])
            P.op('dve', lambda e: e.tensor_tensor(v[:], v[:], mask[:], ALU.mult), ['v', 'mask'], ['v'])
            P.op('dve', lambda e: e.max(v8[:], v[:]), ['v'], ['v8'])
            P.op('dve', lambda e: e.tensor_scalar(slotf[:], v8[:, 0:4], -1.0, BIG, ALU.mult, ALU.add), ['v8'], ['slotf'])
            ks = 'slots%d' % t
            P.op('dve', lambda e, t=t: e.tensor_copy(slots_all[:, t, :], slotf[:]), ['slotf'], [ks])
            for c in range(4):
                P.op('dve', lambda e, c=c: e.tensor_scalar(oh[:], v[:], v8[:, c:c + 1], None, ALU.is_equal), ['v', 'v8'], ['oh'])
                P.op('dve', lambda e: e.tensor_tensor(oh[:], oh[:], G[:], ALU.mult), ['oh', 'G'], ['oh'])
                P.op('dve', lambda e, t=t, c=c: e.reduce_sum(gates_all[:, t, c:c + 1], oh[:], AX.X), ['oh'], ['gates%d_%d' % (t, c)])
            for c in range(4):
                sk = 'scat%d_%d' % (t, c)
                scat_keys.append(sk)
                P.dma('pool', lambda e, t=t, c=c, b=b: e.indirect_dma_start(
                    out=xg, out_offset=bass.IndirectOffsetOnAxis(ap=slots_all[:, t, c:c + 1], axis=0),
                    in_=xb[b][:], in_offset=None), [kb, ks], [sk], ind=True)
        if dbg:
            d_lg = nc.dram_tensor("dbg_lg", [128, NE], F32, kind="ExternalOutput").ap()
            d_xT = nc.dram_tensor("dbg_xT", [128, 16 * 128], F32, kind="ExternalOutput").ap()
            P.barrier()
            P.ld(d_lg, sm['lg'][:])
            P.ld(d_xT, xT[:].rearrange("p a b -> p (a b)"))
            P.ld(d_sl, slots_all[:].rearrange("p t c -> p (t c)"))
            P.ld(d_ga, gates_all[:].rearrange("p t c -> p (t c)"))
        P.barrier()
        P.flush()
    with ExitStack() as ph:
        def sb(shape, dt, name):
            return ph.enter_context(nc.sbuf_tensor(name, list(shape), dt))

        def ps(name, dt=F32, w=512):
            return ph.enter_context(nc.psum_tensor(name, [128, w], dt))
        xgt = sb([128, NB, D], BF16, 'e_xgt')
        xT = sb([128, 16, C], BF16, 'e_xT')
        wgu = [sb([128, 16, 512], BF16, 'e_wgu%d' % i) for i in range(2)]
        wd = [sb([128, 16, 512], BF16, 'e_wd%d' % i) for i in range(2)]
        actT = sb([128, 16, C], BF16, 'e_actT')
        bgu = sb([128, NE * 32], F32, 'e_bgu')
        bd = sb([128, D], F32, 'e_bd')
        gl = [sb([128, H], F32, 'e_gl%d' % i) for i in range(2)]
        sg = [sb([128, H], F32, 'e_sg%d' % i) for i in range(2)]
        ln = [sb([128, H], F32, 'e_ln%d' % i) for i in range(2)]
        ysb = [sb([128, 512], F32, 'e_ysb%d' % i) for i in range(2)]
        ptr = [ps('e_ptr%d' % i, BF16, 1024) for i in range(2)]
        pg = [ps('e_pg%d' % i) for i in range(2)]
        pn = [ps('e_pn%d' % i) for i in range(2)]
        py = [ps('e_py%d' % i) for i in range(2)]
        P.ld(bgu[:], W['bgu_l'], writes=['bgu'])
        nw = 0
        nd = 0
        ny = 0
        nel = 0
        for ex_ in range(NE):
            P.ld(xgt[:], xg[ex_ * C:(ex_ + 1) * C, :].rearrange("(b p) d -> p b d", p=128), writes=['xgt'])
            P.ld(bd[:], W['b_down'][ex_, :].partition_broadcast(128), writes=['bd'])
            n4 = 0
            for bk in range(NB):
                for q in range(4):
                    pq = ptr[n4 % 2]; kp = 'ptr%d' % (n4 % 2)
                    for j in range(4):
                        k = q * 4 + j
                        P.tr(pq[:, j * 128:(j + 1) * 128], xgt[:, bk, k * 128:(k + 1) * 128], identb[:], ['xgt', 'identb'], [kp])
                    if n4 % 2 == 0:
                        P.op('dve', lambda e, q=q, bk=bk, pq=pq: e.tensor_copy(xT[:, q * 4:(q + 1) * 4, bk * 128:(bk + 1) * 128], pq[:, 0:512].rearrange("p (a b) -> p a b", a=4)), [kp], ['xT'])
                    else:
                        P.op('act', lambda e, q=q, bk=bk, pq=pq: e.copy(xT[:, q * 4:(q + 1) * 4, bk * 128:(bk + 1) * 128], pq[:, 0:512].rearrange("p (a b) -> p a b", a=4)), [kp], ['xT'])
                    n4 += 1
            for g in range(8):
                wb_ = wgu[nw % 2]; kw = 'wgu%d' % (nw % 2); nw += 1
                wv = W['w_gate_up'][ex_].rearrange("(k p) f -> p k f", p=128)
                P.ld(wb_[:, :, 0:256], wv[:, :, g * 256:(g + 1) * 256], writes=[kw + 'a'], eng='pool')
                P.ld(wb_[:, :, 256:512], wv[:, :, FF + g * 256:FF + (g + 1) * 256], writes=[kw + 'b'], eng='pool')
                for j in range(2):
                    fj = g * 2 + j
                    for s in range(2):
                        i2 = nel % 2; nel += 1
                        kg, kn = 'pg%d' % i2, 'pn%d' % i2
                        for k in range(16):
                            P.mm(pg[i2][:, 0:H], wb_[:, k, j * 128:(j + 1) * 128], xT[:, k, s * H:(s + 1) * H], k == 0, k == 15, [kw + 'a', 'xT'], [kg])
                        for k in range(16):
                            P.mm(pn[i2][:, 0:H], wb_[:, k, 256 + j * 128:256 + (j + 1) * 128], xT[:, k, s * H:(s + 1) * H], k == 0, k == 15, [kw + 'b', 'xT'], [kn])
                        cg = ex_ * 32 + fj
                        cl = ex_ * 32 + 16 + fj
                        P.op('dve', lambda e, i2=i2, cg=cg: e.tensor_scalar(gl[i2][:], pg[i2][:, 0:H], bgu[:, cg:cg + 1], 7.0, ALU.add, ALU.min), [kg, 'bgu'], ['gl%d' % i2])
                        P.op('act', lambda e, i2=i2: e.activation(sg[i2][:], gl[i2][:], AF.Sigmoid, scale=1.702), ['gl%d' % i2], ['sg%d' % i2])
                        P.op('dve', lambda e, i2=i2, cl=cl: e.tensor_scalar(ln[i2][:], pn[i2][:, 0:H], bgu[:, cl:cl + 1], 7.0, ALU.add, ALU.min), [kn, 'bgu'], ['ln%d' % i2])
                        P.op('pool', lambda e, i2=i2: e.tensor_scalar(ln[i2][:], ln[i2][:], -7.0, 1.0, ALU.max, ALU.add), ['ln%d' % i2], ['ln%d' % i2])
                        P.op('pool', lambda e, i2=i2: e.tensor_tensor(gl[i2][:], gl[i2][:], sg[i2][:], ALU.mult), ['gl%d' % i2, 'sg%d' % i2], ['gl%d' % i2])
                        P.op('dve', lambda e, i2=i2, fj=fj, s=s: e.tensor_tensor(actT[:, fj, s * H:(s + 1) * H], gl[i2][:], ln[i2][:], ALU.mult), ['gl%d' % i2, 'ln%d' % i2], ['actT'])
            for c in range(4):
                wb_ = wd[nd % 2]; kw = 'wd%d' % (nd % 2); nd += 1
                P.ld(wb_[:], W['w_down'][ex_].rearrange("(k p) d -> p k d", p=128)[:, :, c * 512:(c + 1) * 512], writes=[kw], eng='pool')
                for bk in range(NB):
                    i2 = ny % 2; ny += 1
                    for k in range(16):
                        P.mm(py[i2][:], actT[:, k, bk * 128:(bk + 1) * 128], wb_[:, k, :], k == 0, k == 15, ['actT', kw], ['py%d' % i2])
                    P.op('dve', lambda e, i2=i2, c=c: e.tensor_tensor(ysb[i2][:], py[i2][:], bd[:, c * 512:(c + 1) * 512], ALU.add), ['py%d' % i2, 'bd'], ['ysb%d' % i2])
                    P.ld(yd[ex_ * C + bk * 128:ex_ * C + (bk + 1) * 128, c * 512:(c + 1) * 512], ysb[i2][:], reads=['ysb%d' % i2], writes=['yd'], eng='sp')
        P.barrier()
        P.flush()
    with ExitStack() as ph:
        def sb(shape, dt, name):
            return ph.enter_context(nc.sbuf_tensor(name, list(shape), dt))
        xt = [sb([128, D], F32, 'c_xt%d' % i) for i in range(2)]
        yg = [sb([128, D], F32, 'c_yg%d' % i) for i in range(4)]
        ot = [sb([128, D], F32, 'c_ot%d' % i) for i in range(2)]
        gt = sb([128, D], F32, 'c_g'); bt = sb([128, D], F32, 'c_b')
        st = sb([128, 24], F32, 'c_st'); ag = sb([128, 2], F32, 'c_ag'); rstd = sb([128, 1], F32, 'c_rstd')
        P.ld(gt[:], W['ln_g'].partition_broadcast(128), writes=['lng'])
        P.ld(bt[:], W['ln_b'].partition_broadcast(128), writes=['lnb'])
        for t in range(NT):
            b = t % 2
            kx = 'cxt%d' % b
            P.ld(xt[b][:], x_in[t * 128:(t + 1) * 128, :], writes=[kx])
            for c in range(4):
                P.dma('pool', lambda e, t=t, c=c: e.indirect_dma_start(
                    out=yg[c][:], out_offset=None, in_=yd,
                    in_offset=bass.IndirectOffsetOnAxis(ap=slots_all[:, t, c:c + 1], axis=0),
                    ), [], ['yg%d' % c], ind=True)
            P.op('act', lambda e, b=b: e.mul(xt[b][:], xt[b][:], ALPHA), [kx], [kx])
            for c in range(4):
                P.op('dve', lambda e, b=b, c=c, t=t: e.scalar_tensor_tensor(xt[b][:], yg[c][:], gates_all[:, t, c:c + 1], xt[b][:], ALU.mult, ALU.add), ['yg%d' % c, kx], [kx])
            ko = 'cot%d' % b
            emit_ln(P, xt[b], ot[b], gt, bt, 'c_', [kx], [ko], st, ag, rstd)
            P.ld(out_d[t * 128:(t + 1) * 128, :], ot[b][:], reads=[ko])
        P.barrier()
        P.flush()


def build_moe(NT, C, dbg=False):
    nc = bass.Bass("TRN2", target_bir_lowering=False)
    x_in = nc.dram_tensor("x_in", [NT * 128, D], F32, kind="ExternalInput").ap()
    out_d = nc.dram_tensor("out", [NT * 128, D], F32, kind="ExternalOutput").ap()
    W = {}
    W['router_w'] = nc.dram_tensor("router_w", [D, NE], F32, kind="ExternalInput").ap()
    W['router_b'] = nc.dram_tensor("router_b", [NE], F32, kind="ExternalInput").ap()
    W['w_gate_up'] = nc.dram_tensor("w_gate_up", [NE, D, 2 * FF], F32, kind="ExternalInput").ap()
    W['bgu_l'] = nc.dram_tensor("bgu_l", [128, NE * 32], F32, kind="ExternalInput").ap()
    W['w_down'] = nc.dram_tensor("w_down", [NE, FF, D], F32, kind="ExternalInput").ap()
    W['b_down'] = nc.dram_tensor("b_down", [NE, D], F32, kind="ExternalInput").ap()
    W['ln_g'] = nc.dram_tensor("ln_g", [D], F32, kind="ExternalInput").ap()
    W['ln_b'] = nc.dram_tensor("ln_b", [D], F32, kind="ExternalInput").ap()
    with ExitStack() as es:
        P = Prog(nc, es)
        cst = load_consts(P, nc, ['c_ident', 'c_uts', 'c_ones'])
        emit_moe(P, nc, es, NT, C, x_in, out_d, W, cst, dbg)
        P.finish()
    return nc


def moe_inputs(x_sh, router_w, router_b, w_gate_up, b_gate_up, w_down, b_down, ln_g, ln_b):
    c = consts_np()
    bgu_l = np.ascontiguousarray(b_gate_up.reshape(NE, 32, 128).transpose(2, 0, 1).reshape(128, NE * 32))
    com = {'router_w': router_w, 'router_b': router_b, 'w_gate_up': w_gate_up, 'bgu_l': bgu_l,
           'w_down': w_down, 'b_down': b_down, 'ln_g': ln_g, 'ln_b': ln_b,
           'c_ident': c['c_ident'], 'c_uts': c['c_uts'], 'c_ones': c['c_ones']}
    return [dict(com, x_in=np.ascontiguousarray(xs)) for xs in x_sh]


GROUPS = ((128, 1), (512, 4), (2048, 16))
NEG = -30000.0
HALO = 2048


def attn_static():
    import math
    out = {}
    for g, (w, d) in enumerate(GROUPS):
        L = w + 256
        S = np.zeros((33, L), np.float32)
        for n in range(L):
            dl = n - 127
            if dl >= 0 and dl % d == 0 and dl <= w:
                if dl < 16:
                    bk = dl
                else:
                    v = np.log(np.float32(max(dl, 1)) / np.float32(16)) / np.float32(math.log(2048 / 16)) * np.float32(16)
                    bk = min(16 + int(np.float32(v).astype(np.int32)), 31)
                S[bk, n] = 1.0
            else:
                S[32, n] = NEG
        out['c_S%d' % g] = S
    return out


def emit_attn(P, nc, es, NQ, xc, hv_d, W, out_d, cst):
    NTT = 16 + NQ
    NTOK = NTT * 128
    ident = cst['c_ident']
    xT_d = nc.dram_tensor("a_xT", [16, 128, NTOK], BF16, kind="Internal").ap()
    qT_d = nc.dram_tensor("a_qT", [3, 8, 128, NQ * 128], BF16, kind="Internal").ap()
    kT_d = nc.dram_tensor("a_kT", [3, 8, 128, NTOK], BF16, kind="Internal").ap()
    v_d = nc.dram_tensor("a_v", [3, 8, NTOK, 128], BF16, kind="Internal").ap()
    Zs = [[nc.dram_tensor("a_Z%d_%d" % (g, h), [128, GROUPS[g][0] + 256], F32, kind="Internal") for h in range(8)] for g in range(3)]
    identb = es.enter_context(nc.sbuf_tensor('a_identb', [128, 128], BF16))
    P.op('dve', lambda e: e.tensor_copy(identb[:], ident[:]), ['c_ident'], ['a_identb'])
    o_all = es.enter_context(nc.sbuf_tensor('a_oall', [128, NQ, 1024], BF16))
    with ExitStack() as ph:
        def sb(shape, dt, name):
            return ph.enter_context(nc.sbuf_tensor(name, list(shape), dt))
        rbT = sb([33, 24], F32, 'a0_rbT')
        lb = sb([33, 128], F32, 'a0_lb')
        Ssb = [sb([33, GROUPS[g][0] + 256], F32, 'a0_S%d' % g) for g in range(3)]
        fb = [sb([128, 2304], F32, 'a0_fb%d' % i) for i in range(2)]
        pz = [ph.enter_context(nc.psum_tensor('a0_pz%d' % i, [128, 512], F32)) for i in range(2)]
        P.op('pool', lambda e: e.memset(rbT[:], 1.0), [], ['rbT'])
        P.ld(rbT[0:32, :], W['rel_bias'], writes=['rbT'])
        for g in range(3):
            Sd = nc.dram_tensor('c_S%d' % g, [33, GROUPS[g][0] + 256], F32, kind="ExternalInput").ap()
            P.ld(Ssb[g][:], Sd, writes=['S%d' % g])
        n = 0
        for g in range(3):
            L = GROUPS[g][0] + 256
            for h in range(8):
                c = g * 8 + h
                P.op('dve', lambda e, c=c: e.tensor_copy(lb[:], rbT[:, c:c + 1].to_broadcast([33, 128])), ['rbT'], ['lb'])
                f = fb[n % 2]; kf = 'fb%d' % (n % 2); n += 1
                for ci, c0 in enumerate(range(0, L, 512)):
                    cw = min(512, L - c0)
                    pzz = pz[ci % 2]
                    P.mm(pzz[:, 0:cw], lb[:], Ssb[g][:, c0:c0 + cw], True, True, ['lb', 'S%d' % g], ['pz%d' % (ci % 2)])
                    P.op('act', lambda e, pzz=pzz, f=f, c0=c0, cw=cw: e.copy(f[:, c0:c0 + cw], pzz[:, 0:cw]), ['pz%d' % (ci % 2)], [kf])
                P.ld(Zs[g][h].ap(), f[:, 0:L], reads=[kf], writes=['Z'])
        P.barrier()
        P.flush()
    with ExitStack() as ph:
        def sb(shape, dt, name):
            return ph.enter_context(nc.sbuf_tensor(name, list(shape), dt))

        def ps(name, dt=F32, w=512):
            return ph.enter_context(nc.psum_tensor(name, [128, w], dt))
        xt = [sb([128, D], F32, 'a1_xt%d' % i) for i in range(2)]
        xb = [sb([128, D], BF16, 'a1_xb%d' % i) for i in range(2)]
        xTt = [sb([128, 16, 128], BF16, 'a1_xTt%d' % i) for i in range(2)]
        wblk = [sb([128, 16, 512], BF16, 'a1_w%d' % i) for i in range(2)]
        xTg = [sb([128, 16, 512], BF16, 'a1_xTg%d' % i) for i in range(2)]
        stg = [sb([128, 512], BF16, 'a1_stg%d' % i) for i in range(4)]
        ptr = [ps('a1_ptr%d' % i, BF16, 1024) for i in range(2)]
        pm = [ps('a1_pm%d' % i) for i in range(4)]
        n4 = 0
        for t in range(NTT):
            b = t % 2
            P.ld(xt[b][:], xc[t * 128:(t + 1) * 128, :], writes=['xt%d' % b])
            P.op('act', lambda e, b=b: e.copy(xb[b][:], xt[b][:]), ['xt%d' % b], ['xb%d' % b])
            for q in range(4):
                pq = ptr[n4 % 2]; kp = 'ptr%d' % (n4 % 2)
                for j in range(4):
                    k = q * 4 + j
                    P.tr(pq[:, j * 128:(j + 1) * 128], xb[b][:, k * 128:(k + 1) * 128], identb[:], ['xb%d' % b, 'a_identb'], [kp])
                P.op('dve', lambda e, q=q, b=b, pq=pq: e.tensor_copy(xTt[b][:, q * 4:(q + 1) * 4, :], pq[:, 0:512].rearrange("p (a b) -> p a b", a=4)), [kp], ['xTt%d' % b])
                n4 += 1
            P.ld(xT_d[:, :, t * 128:(t + 1) * 128].rearrange("k p t -> p k t"), xTt[b][:], reads=['xTt%d' % b], writes=['xT_d'])
        P.barrier()
        ns = 0
        npm = 0
        for cb in range(18):
            g, part, hh = cb // 6, (cb % 6) // 2, cb % 2
            wb_ = wblk[cb % 2]; kw = 'w%d' % (cb % 2)
            P.ld(wb_[:], W['attn_w_in'].rearrange("(k p) f -> p k f", p=128)[:, :, cb * 512:(cb + 1) * 512], writes=[kw], eng='pool')
            tg0 = 4 if part == 0 else 0
            for tg in range(tg0, NTT // 4):
                xg_ = xTg[tg % 2]; kx = 'xTg%d' % (tg % 2)
                P.ld(xg_[:], xT_d[:, :, tg * 512:(tg + 1) * 512].rearrange("k p t -> p k t"), writes=[kx])
                for j in range(4):
                    pmm = pm[npm % 4]; kpm = 'pm%d' % (npm % 4); npm += 1
                    st_ = stg[ns % 4]; kst = 'stg%d' % (ns % 4); ns += 1
                    if part < 2:
                        h = hh * 4 + j
                        for k in range(16):
                            P.mm(pmm[:], wb_[:, k, j * 128:(j + 1) * 128], xg_[:, k, :], k == 0, k == 15, [kw, kx], [kpm])
                        if ns % 2:
                            P.op('act', lambda e, st_=st_, pmm=pmm: e.copy(st_[:], pmm[:]), [kpm], [kst])
                        else:
                            P.op('dve', lambda e, st_=st_, pmm=pmm: e.tensor_copy(st_[:], pmm[:]), [kpm], [kst])
                        if part == 0:
                            P.ld(qT_d[g, h, :, (tg - 4) * 512:(tg - 3) * 512], st_[:], reads=[kst], writes=['qT_d'])
                        else:
                            P.ld(kT_d[g, h, :, tg * 512:(tg + 1) * 512], st_[:], reads=[kst], writes=['kT_d'])
                    else:
                        for k in range(16):
                            P.mm(pmm[:], xg_[:, k, j * 128:(j + 1) * 128], wb_[:, k, :], k == 0, k == 15, [kw, kx], [kpm])
                        if ns % 2:
                            P.op('act', lambda e, st_=st_, pmm=pmm: e.copy(st_[:], pmm[:]), [kpm], [kst])
                        else:
                            P.op('dve', lambda e, st_=st_, pmm=pmm: e.tensor_copy(st_[:], pmm[:]), [kpm], [kst])
                        r0 = tg * 512 + j * 128
                        P.ld(v_d[g, hh * 4:(hh + 1) * 4, r0:r0 + 128, :].rearrange("h t d -> t h d"),
                             st_[:].rearrange("p (h d) -> p h d", h=4), reads=[kst], writes=['v_d'])
        P.barrier()
        P.flush()
    scale = 128.0 ** -0.5
    with ExitStack() as ph:
        def sb(shape, dt, name):
            return ph.enter_context(nc.sbuf_tensor(name, list(shape), dt))

        def ps(name, dt=F32, w=512):
            return ph.enter_context(nc.psum_tensor(name, [128, w], dt))
        kT = [sb([128, NTOK], BF16, 'a2_kT%d' % g) for g in range(3)]
        qT = [sb([128, NQ * 128], BF16, 'a2_qT%d' % g) for g in range(3)]
        vs = [sb([128, NTT, 129], BF16, 'a2_v%d' % g) for g in range(3)]
        Bt = sb([128, 24, 128], F32, 'a2_Bt')
        tmp = [sb([128, 512], F32, 'a2_tmp%d' % i) for i in range(2)]
        pT = [sb([128, 512], BF16, 'a2_pT%d' % i) for i in range(3)]
        rc = [sb([128, 1], F32, 'a2_rc%d' % i) for i in range(2)]
        hv = sb([128, 1], F32, 'a2_hv')
        pS = [ps('a2_pS%d' % i) for i in range(3)]
        pO = [ps('a2_pO%d' % i) for i in range(2)]
        P.ld(hv[:], hv_d, writes=['hv'])
        for g in range(3):
            P.op('pool', lambda e, g=g: e.memset(vs[g][:, :, 128:129], 1.0), [], ['vs%d' % g])
            P.op('dve', lambda e, g=g: e.tensor_scalar(vs[g][:, 0:16, 128:129], vs[g][:, 0:16, 128:129], hv[:, 0:1], None, ALU.mult), ['vs%d' % g, 'hv'], ['vs%d' % g])
        chunks = []
        bi = 0
        for g in range(3):
            nm = GROUPS[g][0] // 128 + 1
            for m0 in range(0, nm, 4):
                ms = list(range(m0, min(m0 + 4, nm)))
                chunks.append((g, ms, bi))
                bi += len(ms)
        nS = 0
        nO = 0
        for h in range(8):
            for g in range(3):
                P.ld(kT[g][:], kT_d[g, h], writes=['kT%d' % g])
                P.ld(qT[g][:], qT_d[g, h], writes=['qT%d' % g])
                P.ld(vs[g][:, :, 0:128], v_d[g, h].rearrange("(t p) d -> p t d", p=128), writes=['vs%d' % g])
            bi = 0
            for g in range(3):
                L = GROUPS[g][0] + 256
                for m in range(GROUPS[g][0] // 128 + 1):
                    tap = bass.AP(tensor=Zs[g][h], offset=m * 128 + 127, ap=[[L - 1, 128], [1, 128]])
                    P.ld(Bt[:, bi, :], tap, writes=['Bt'])
                    bi += 1
            for qt in range(NQ):
                T = 16 + qt
                io = nO % 2; nO += 1
                first = True
                for (g, ms, b0) in chunks:
                    i3 = nS % 3; nS += 1
                    n = len(ms)
                    for mi, m in enumerate(ms):
                        KT = T - m
                        P.mm(pS[i3][:, mi * 128:(mi + 1) * 128], kT[g][:, KT * 128:(KT + 1) * 128], qT[g][:, qt * 128:(qt + 1) * 128], True, True,
                             ['kT%d' % g, 'qT%d' % g], ['pS%d' % i3])
                    i2 = nS % 2
                    P.op('dve', lambda e, i3=i3, i2=i2, n=n, b0=b0: e.scalar_tensor_tensor(
                        tmp[i2][:, 0:n * 128], pS[i3][:, 0:n * 128], scale, Bt[:, b0:b0 + n, :].rearrange("p a b -> p (a b)"), ALU.mult, ALU.add),
                        ['pS%d' % i3, 'Bt'], ['tmp%d' % i2])
                    P.op('act', lambda e, i3=i3, i2=i2, n=n: e.activation(pT[i3][:, 0:n * 128], tmp[i2][:, 0:n * 128], AF.Exp), ['tmp%d' % i2], ['pT%d' % i3])
                    for mi, m in enumerate(ms):
                        KT = T - m
                        last = (g == 2 and m == 16)
                        P.mm(pO[io][:, 0:129], pT[i3][:, mi * 128:(mi + 1) * 128], vs[g][:, KT, :], first, last, ['pT%d' % i3, 'vs%d' % g], ['pO%d' % io])
                        first = False
                P.op('dve', lambda e, io=io: e.reciprocal(rc[io][:], pO[io][:, 128:129]), ['pO%d' % io], ['rc%d' % io])
                P.op('act', lambda e, io=io, qt=qt, h=h: e.activation(o_all[:, qt, h * 128:(h + 1) * 128], pO[io][:, 0:128], AF.Copy, scale=rc[io][:, 0:1]),
                     ['pO%d' % io, 'rc%d' % io], ['oall%d' % qt])
        P.barrier()
        P.flush()
    with ExitStack() as ph:
        def sb(shape, dt, name):
            return ph.enter_context(nc.sbuf_tensor(name, list(shape), dt))

        def ps(name, dt=F32, w=512):
            return ph.enter_context(nc.psum_tensor(name, [128, w], dt))
        wo = sb([128, 8, D], BF16, 'a3_wo')
        oT = [sb([128, 8, 128], BF16, 'a3_oT%d' % i) for i in range(2)]
        xt = [sb([128, D], F32, 'a3_xt%d' % i) for i in range(2)]
        ot = [sb([128, D], F32, 'a3_ot%d' % i) for i in range(2)]
        gt = sb([128, D], F32, 'a3_g'); bt = sb([128, D], F32, 'a3_b')
        st = sb([128, 24], F32, 'a3_st'); ag = sb([128, 2], F32, 'a3_ag'); rstd = sb([128, 1], F32, 'a3_rstd')
        ptr = [ps('a3_ptr%d' % i, BF16, 1024) for i in range(2)]
        py = [ps('a3_py%d' % i) for i in range(4)]
        P.ld(wo[:], W['attn_w_out'].rearrange("(k p) d -> p k d", p=128), writes=['wo'], eng='pool')
        P.ld(gt[:], W['ln_g'].partition_broadcast(128), writes=['lng'])
        P.ld(bt[:], W['ln_b'].partition_broadcast(128), writes=['lnb'])
        for qt in range(NQ):
            b = qt % 2
            P.ld(xt[b][:], xc[(16 + qt) * 128:(17 + qt) * 128, :], writes=['xt%d' % b])
            for q in range(2):
                pq = ptr[q]; kp = 'ptr%d' % q
                for j in range(4):
                    k = q * 4 + j
                    P.tr(pq[:, j * 128:(j + 1) * 128], o_all[:, qt, k * 128:(k + 1) * 128], identb[:], ['oall%d' % qt, 'a_identb'], [kp])
                P.op('dve', lambda e, q=q, b=b, pq=pq: e.tensor_copy(oT[b][:, q * 4:(q + 1) * 4, :], pq[:, 0:512].rearrange("p (a b) -> p a b", a=4)), [kp], ['oT%d' % b])
            for c in range(4):
                for k in range(8):
                    P.mm(py[c][:], oT[b][:, k, :], wo[:, k, c * 512:(c + 1) * 512], k == 0, k == 7, ['oT%d' % b, 'wo'], ['py%d' % c])
                P.op('dve', lambda e, b=b, c=c: e.scalar_tensor_tensor(xt[b][:, c * 512:(c + 1) * 512], xt[b][:, c * 512:(c + 1) * 512], ALPHA, py[c][:], ALU.mult, ALU.add),
                     ['py%d' % c, 'xt%d' % b], ['xt%d' % b])
            emit_ln(P, xt[b], ot[b], gt, bt, 'a3_', ['xt%d' % b], ['ot%d' % b], st, ag, rstd)
            P.ld(out_d[qt * 128:(qt + 1) * 128, :], ot[b][:], reads=['ot%d' % b])
        P.barrier()
        P.flush()


def build_attn(NQ):
    nc = bass.Bass("TRN2", target_bir_lowering=False)
    xc = nc.dram_tensor("xc", [(16 + NQ) * 128, D], F32, kind="ExternalInput").ap()
    hv_d = nc.dram_tensor("hv", [128, 1], F32, kind="ExternalInput").ap()
    out_d = nc.dram_tensor("out", [NQ * 128, D], F32, kind="ExternalOutput").ap()
    W = {}
    W['rel_bias'] = nc.dram_tensor("rel_bias", [32, 24], F32, kind="ExternalInput").ap()
    W['attn_w_in'] = nc.dram_tensor("attn_w_in", [D, 9216], F32, kind="ExternalInput").ap()
    W['attn_w_out'] = nc.dram_tensor("attn_w_out", [1024, D], F32, kind="ExternalInput").ap()
    W['ln_g'] = nc.dram_tensor("ln_g", [D], F32, kind="ExternalInput").ap()
    W['ln_b'] = nc.dram_tensor("ln_b", [D], F32, kind="ExternalInput").ap()
    with ExitStack() as es:
        P = Prog(nc, es)
        cst = load_consts(P, nc, ['c_ident'])
        emit_attn(P, nc, es, NQ, xc, hv_d, W, out_d, cst)
        P.finish()
    return nc


class RR:
    def __init__(self, items):
        self.items = items
        self.i = 0

    def get(self):
        it = self.items[self.i % len(self.items)]
        self.i += 1
        return it


def dn_consts_np():
    c = {}
    iu = np.triu(np.ones((128, 128), np.float32), 1)
    c['c_negu'] = (-1e4 * iu).astype(np.float32)
    c['c_negl'] = (-1e4 * iu.T).astype(np.float32)
    c['c_strl'] = iu.T.copy()
    return c


def emit_dn(P, nc, es, NB_, NTL, x_d, W, out_d, cst, dbg=False):
    ident, uti, ones = cst['c_ident'], cst['c_uti'], cst['c_ones']
    negu, negl, strl = cst['c_negu'], cst['c_negl'], cst['c_strl']
    scale = 128.0 ** -0.5

    def sb(shape, dt, name):
        return es.enter_context(nc.sbuf_tensor(name, list(shape), dt))

    identb = sb([128, 128], BF16, 'd_identb')
    P.op('dve', lambda e: e.tensor_copy(identb[:], ident[:]), ['c_ident'], ['d_identb'])
    wsl = sb([128, 16, 1544], BF16, 'd_wsl')
    P.ld(wsl[:], W['w_sl'].rearrange("(k p) f -> p k f", p=128), writes=['wsl'], eng='pool')
    cw = sb([128, 8, 4], F32, 'd_cw')
    P.ld(cw[:], W['cw_l'], writes=['cw'])
    ng = sb([128, 128], F32, 'd_ng')
    P.ld(ng[:], W['norm_g'].partition_broadcast(128), writes=['ng'])
    negA = sb([128, 4], F32, 'd_negA')
    dtb = sb([128, 4], F32, 'd_dtb')
    P.ld(negA[:], W['alog_l'].partition_broadcast(128), writes=['negA'])
    P.ld(dtb[:], W['dtb_l'].partition_broadcast(128), writes=['dtb'])
    P.op('act', lambda e: e.activation(negA[:], negA[:], AF.Exp), ['negA'], ['negA'])
    P.op('dve', lambda e: e.tensor_scalar(negA[:], negA[:], -1.0, None, ALU.mult), ['negA'], ['negA'])
    xt = [sb([128, D], F32, 'd_xt%d' % i) for i in range(2)]
    xb = [sb([128, D], BF16, 'd_xb%d' % i) for i in range(2)]
    xT = [sb([128, 16, 128], BF16, 'd_xT%d' % i) for i in range(2)]
    raw = [[sb([128, 131], F32, 'd_raw%d_%d' % (i, c)) for c in range(8)] for i in range(2)]
    qk = [[sb([128, 128], BF16, 'd_qk%d_%d' % (i, c)) for c in range(4)] for i in range(2)]
    vT = [[sb([128, 128], F32, 'd_vT%d_%d' % (i, c)) for c in range(4)] for i in range(2)]
    qf = [[sb([128, 128], F32, 'd_qf%d_%d' % (i, c)) for c in range(2)] for i in range(2)]
    zs = [sb([128, 512], F32, 'd_zs%d' % i) for i in range(2)]
    outt = [sb([128, 512], BF16, 'd_out%d' % i) for i in range(2)]
    sm4 = {n: [sb([128, 4], F32, 'd_%s%d' % (n, i)) for i in range(2)] for n in ('beta', 'nbeta', 'g', 'gc', 'ngc', 'xa')}
    ded = {n: [[sb([128, 128], F32 if n in ('Erow', 'TT', 'vb') else BF16, 'd_%s%d_%d' % (n, i, j)) for j in range(4)] for i in range(2)]
           for n in ('Erow', 'TT', 'QKdT', 'kdec', 'vb', 'qtil')}
    c1 = [[sb([128, 1], F32, 'd_c1%d_%d' % (i, j)) for j in range(4)] for i in range(2)]
    growl = [[sb([128, 1], F32, 'd_gl%d_%d' % (i, j)) for j in range(4)] for i in range(2)]
    Sst = [sb([128, 128], F32, 'd_S%d' % j) for j in range(4)]
    Sbf = [sb([128, 128], BF16, 'd_Sbf%d' % j) for j in range(4)]
    tf = RR([(sb([128, 128], F32, 'd_tf%d' % i), 'tf%d' % i) for i in range(56)])
    tb = RR([(sb([128, 128], BF16, 'd_tb%d' % i), 'tb%d' % i) for i in range(8)])
    t1 = RR([(sb([128, 1], F32, 'd_t1%d' % i), 't1%d' % i) for i in range(16)])
    pxt = es.enter_context(nc.psum_tensor('d_pxt', [128, 1024], BF16))
    pkt = es.enter_context(nc.psum_tensor('d_pkt', [128, 1024], BF16))
    pz = es.enter_context(nc.psum_tensor('d_pz', [128, 512], F32))
    pbk = RR([(es.enter_context(nc.psum_tensor('d_pb%d' % i, [128, 512], F32)), 'pb%d' % i) for i in range(5)])

    def sl(t, j):
        return t[:, j * 128:(j + 1) * 128]

    for b_ in range(NB_):
        for j in range(4):
            P.op('pool', lambda e, j=j: e.memset(Sst[j][:], 0.0), [], ['S%d' % j])
            P.op('pool', lambda e, j=j: e.memset(Sbf[j][:], 0.0), [], ['Sbf%d' % j])
        for c in range(8):
            P.op('pool', lambda e, c=c: e.memset(raw[0][c][:, 0:3], 0.0), [], ['raw0_%d' % c])
        for n in range(NTL):
            par = n % 2
            row0 = (b_ * NTL + n) * 128
            kx, kb_, kT_ = 'xt%d' % par, 'xb%d' % par, 'xT%d' % par
            P.ld(xt[par][:], x_d[row0:row0 + 128, :], writes=[kx])
            P.op('act', lambda e, par=par: e.copy(xb[par][:], xt[par][:]), [kx], [kb_])
            for q in range(4):
                half = q % 2
                for jj in range(4):
                    k = q * 4 + jj
                    P.tr(pxt[:, half * 512 + jj * 128:half * 512 + (jj + 1) * 128], xb[par][:, k * 128:(k + 1) * 128], identb[:], [kb_, 'd_identb'], ['pxt'])
                P.op('dve', lambda e, q=q, par=par, half=half: e.tensor_copy(xT[par][:, q * 4:(q + 1) * 4, :], pxt[:, half * 512:(half + 1) * 512].rearrange("p (a b) -> p a b", a=4)), [], ['pxt', kT_])
            ys = [None] * 4
            for grp in range(2):
                bk_, kbk = pbk.get()
                for ci in range(4):
                    c = grp * 4 + ci
                    for k in range(16):
                        P.mm(sl(bk_, ci), wsl[:, k, c * 128:(c + 1) * 128], xT[par][:, k, :], k == 0, k == 15, ['wsl', kT_], [kbk])
                for ci in range(4):
                    c = grp * 4 + ci
                    kr = 'raw%d_%d' % (par, c)
                    P.op('act', lambda e, c=c, par=par, bk_=bk_, ci=ci: e.copy(raw[par][c][:, 3:131], sl(bk_, ci)), [], [kbk, kr])
                    P.op('pool', lambda e, c=c, par=par: e.tensor_copy(raw[1 - par][c][:, 0:3], raw[par][c][:, 128:131]), [kr], ['raw%d_%d' % (1 - par, c)])
                    acc, ka = tf.get()
                    P.op('dve', lambda e, c=c, par=par, acc=acc: e.tensor_scalar(acc[:], raw[par][c][:, 0:128], cw[:, c, 0:1], None, ALU.mult), [kr, 'cw'], [ka])
                    for jt in range(1, 4):
                        P.op('dve', lambda e, c=c, par=par, acc=acc, jt=jt: e.scalar_tensor_tensor(acc[:], raw[par][c][:, jt:jt + 128], cw[:, c, jt:jt + 1], acc[:], ALU.mult, ALU.add), [kr, 'cw', ka], [ka])
                    if c >= 4:
                        P.op('act', lambda e, c=c, par=par, acc=acc: e.activation(vT[par][c - 4][:], acc[:], AF.Silu), [ka], ['vT%d_%d' % (par, c - 4)])
                    else:
                        y, ky = tf.get()
                        P.op('act', lambda e, acc=acc, y=y: e.activation(y[:], acc[:], AF.Silu), [ka], [ky])
                        ys[c] = (y, ky)
            bk_, kbk = pbk.get()
            sqs = []
            for c in range(4):
                y, ky = ys[c]
                sq, ksq = tf.get()
                P.op('pool', lambda e, y=y, sq=sq: e.tensor_tensor(sq[:], y[:], y[:], ALU.mult), [ky], [ksq])
                sqs.append((sq, ksq))
            for c in range(4):
                P.mm(sl(bk_, c), ones[:], sqs[c][0][:], True, True, ['c_ones', sqs[c][1]], [kbk])
            for c in range(4):
                y, ky = ys[c]
                rn, krn = tf.get()
                P.op('dve', lambda e, rn=rn, bk_=bk_, c=c: e.tensor_scalar(rn[:], sl(bk_, c), 1e-6, None, ALU.add), [], [kbk, krn])
                P.op('act', lambda e, rn=rn: e.activation(rn[:], rn[:], AF.Sqrt), [krn], [krn])
                P.op('dve', lambda e, rn=rn: e.reciprocal(rn[:], rn[:]), [krn], [krn])
                P.op('dve', lambda e, rn=rn, y=y, c=c, par=par: e.tensor_tensor(qk[par][c][:], y[:], rn[:], ALU.mult), [krn, ky], ['qk%d_%d' % (par, c)])
                if c < 2:
                    P.op('pool', lambda e, rn=rn, y=y, c=c, par=par: e.tensor_tensor(qf[par][c][:], y[:], rn[:], ALU.mult), [krn, ky], ['qf%d_%d' % (par, c)])
            for k in range(16):
                P.mm(pz[:], xT[par][:, k, :], wsl[:, k, 1024:1536], k == 0, k == 15, [kT_, 'wsl'], ['pz'])
            P.op('act', lambda e, par=par: e.activation(zs[par][:], pz[:], AF.Silu), [], ['pz', 'zs%d' % par])
            pba, kpba = pbk.get()
            for k in range(16):
                P.mm(pba[:, 0:8], xT[par][:, k, :], wsl[:, k, 1536:1544], k == 0, k == 15, [kT_, 'wsl'], [kpba])
            beta, nbeta, g_, gc, ngc, xa = (sm4[n_][par] for n_ in ('beta', 'nbeta', 'g', 'gc', 'ngc', 'xa'))
            ksm = {n_: 'sm_%s%d' % (n_, par) for n_ in sm4}
            P.op('act', lambda e, beta=beta, pba=pba: e.activation(beta[:], pba[:, 0:4], AF.Sigmoid), [], [kpba, ksm['beta']])
            P.op('dve', lambda e, beta=beta, nbeta=nbeta: e.tensor_scalar(nbeta[:], beta[:], -1.0, None, ALU.mult), [ksm['beta']], [ksm['nbeta']])
            P.op('dve', lambda e, xa=xa, pba=pba: e.tensor_tensor(xa[:], pba[:, 4:8], dtb[:], ALU.add), ['dtb'], [kpba, ksm['xa']])
            P.op('act', lambda e, xa=xa: e.activation(xa[:], xa[:], AF.Exp), [ksm['xa']], [ksm['xa']])
            P.op('dve', lambda e, xa=xa: e.tensor_scalar(xa[:], xa[:], 1.0, None, ALU.add), [ksm['xa']], [ksm['xa']])
            P.op('act', lambda e, xa=xa: e.activation(xa[:], xa[:], AF.Ln), [ksm['xa']], [ksm['xa']])
            P.op('dve', lambda e, xa=xa, g_=g_: e.tensor_tensor(g_[:], xa[:], negA[:], ALU.mult), [ksm['xa'], 'negA'], [ksm['g']])
            P.mm(pba[:, 128:132], uti[:], g_[:], True, True, ['c_uti', ksm['g']], [kpba])
            P.op('dve', lambda e, gc=gc, pba=pba: e.tensor_copy(gc[:], pba[:, 128:132]), [], [kpba, ksm['gc']])
            P.op('dve', lambda e, gc=gc, ngc=ngc: e.tensor_scalar(ngc[:], gc[:], -1.0, None, ALU.mult), [ksm['gc']], [ksm['ngc']])
            dkk = [{n_: 'ded_%s%d_%d' % (n_, par, j) for n_ in ded} for j in range(4)]
            kq_ = ['qk%d_%d' % (par, j // 2) for j in range(4)]
            kk_ = ['qk%d_%d' % (par, 2 + j // 2) for j in range(4)]
            qTs = [qk[par][j // 2] for j in range(4)]
            kTs = [qk[par][2 + j // 2] for j in range(4)]
            bgr, kbgr = pbk.get()
            for j in range(4):
                gbc, kgbc = tf.get()
                P.op('dve', lambda e, gbc=gbc, g_=g_, j=j: e.tensor_scalar(gbc[:], ones[:], g_[:, j:j + 1], None, ALU.mult), ['c_ones', ksm['g']], [kgbc])
                P.mm(sl(bgr, j), gbc[:], uti[:], True, True, [kgbc, 'c_uti'], [kbgr])
            tDs = []; tDTs = []
            for j in range(4):
                Erow = ded['Erow'][par][j]
                P.op('act', lambda e, bgr=bgr, Erow=Erow, j=j: e.activation(Erow[:], sl(bgr, j), AF.Exp), [], [kbgr, dkk[j]['Erow']])
            for j in range(4):
                gl_ = growl[par][j]
                P.op('dve', lambda e, bgr=bgr, gl_=gl_, j=j: e.tensor_copy(gl_[:], sl(bgr, j)[:, 127:128]), [], [kbgr, 'gl%d_%d' % (par, j)])
                tD, ktD = tf.get()
                P.op('dve', lambda e, bgr=bgr, tD=tD, j=j: e.scalar_tensor_tensor(tD[:], sl(bgr, j), -1.0, negu[:], ALU.mult, ALU.add), ['c_negu'], [kbgr, ktD])
                tDT, ktDT = tf.get()
                P.op('dve', lambda e, bgr=bgr, tDT=tDT, j=j: e.tensor_tensor(tDT[:], sl(bgr, j), negl[:], ALU.add), ['c_negl'], [kbgr, ktDT])
                tDs.append((tD, ktD)); tDTs.append((tDT, ktDT))
            Dss = []
            for j in range(4):
                tD, ktD = tDs[j]; tDT, ktDT = tDTs[j]
                P.op('act', lambda e, tD=tD, gc=gc, j=j: e.activation(tD[:], tD[:], AF.Exp, bias=gc[:, j:j + 1], scale=1.0), [ktD, ksm['gc']], [ktD])
                Ds, kDs = tf.get()
                P.op('pool', lambda e, Ds=Ds, tD=tD: e.tensor_tensor(Ds[:], tD[:], strl[:], ALU.mult), [ktD, 'c_strl'], [kDs])
                Dss.append((Ds, kDs))
                P.op('act', lambda e, tDT=tDT, ngc=ngc, j=j: e.activation(tDT[:], tDT[:], AF.Exp, bias=ngc[:, j:j + 1], scale=1.0), [ktDT, ksm['ngc']], [ktDT])
            bkk, kbkk = pbk.get()
            for j in range(4):
                P.mm(sl(bkk, j), kTs[j][:], kTs[j][:], True, True, [kk_[j]], [kbkk])
            Bl = [None] * 4; Ml = [None] * 4; Pl = [None] * 4
            for j in range(4):
                B0, kB0 = tf.get()
                Ds, kDs = Dss[j]
                P.op('dve', lambda e, bkk=bkk, B0=B0, nbeta=nbeta, Ds=Ds, j=j: e.scalar_tensor_tensor(B0[:], sl(bkk, j), nbeta[:, j:j + 1], Ds[:], ALU.mult, ALU.mult), [ksm['nbeta'], kDs], [kbkk, kB0])
                Bl[j] = (B0, kB0)
            bm0, kbm0 = pbk.get()
            for j in range(4):
                P.tr(sl(bm0, j), Bl[j][0][:], ident[:], [Bl[j][1], 'c_ident'], [kbm0])
            for j in range(4):
                M0, kM0 = tf.get()
                P.op('dve', lambda e, bm0=bm0, M0=M0, j=j: e.tensor_copy(M0[:], sl(bm0, j)), [], [kbm0, kM0])
                P0, kP0 = tf.get()
                P.op('dve', lambda e, bm0=bm0, P0=P0, j=j: e.tensor_tensor(P0[:], sl(bm0, j), ident[:], ALU.add), ['c_ident'], [kbm0, kP0])
                Ml[j] = (M0, kM0); Pl[j] = (P0, kP0)
            bkq, kbkq = pbk.get()
            for j in range(4):
                P.mm(sl(bkq, j), kTs[j][:], qTs[j][:], True, True, [kk_[j], kq_[j]], [kbkq])
            for j in range(4):
                QKdT = ded['QKdT'][par][j]
                tDT, ktDT = tDTs[j]
                P.op('dve', lambda e, bkq=bkq, QKdT=QKdT, tDT=tDT, j=j: e.scalar_tensor_tensor(QKdT[:], sl(bkq, j), scale, tDT[:], ALU.mult, ALU.mult), [ktDT], [kbkq, dkk[j]['QKdT']])
            for j in range(4):
                P.tr(pkt[:, j * 128:(j + 1) * 128], kTs[j][:], identb[:], [kk_[j], 'd_identb'], ['pkt'])
            for j in range(4):
                gl_ = growl[par][j]
                decf, kdf = t1.get()
                P.op('act', lambda e, decf=decf, gc=gc, gl_=gl_, j=j: e.activation(decf[:], gc[:, j:j + 1], AF.Exp, bias=gl_[:, 0:1], scale=-1.0), [ksm['gc'], 'gl%d_%d' % (par, j)], [kdf])
                kdec = ded['kdec'][par][j]
                P.op('dve', lambda e, kdec=kdec, decf=decf, j=j: e.tensor_scalar(kdec[:], pkt[:, j * 128:(j + 1) * 128], decf[:, 0:1], None, ALU.mult), [kdf], ['pkt', dkk[j]['kdec']])
            bvt, kbvt = pbk.get()
            for j in range(4):
                P.tr(sl(bvt, j), vT[par][j][:], ident[:], ['vT%d_%d' % (par, j), 'c_ident'], [kbvt])
            for j in range(4):
                vb = ded['vb'][par][j]
                P.op('dve', lambda e, bvt=bvt, vb=vb, beta=beta, j=j: e.tensor_scalar(vb[:], sl(bvt, j), beta[:, j:j + 1], None, ALU.mult), [ksm['beta']], [kbvt, dkk[j]['vb']])
                c1_ = c1[par][j]; kc1 = 'c1%d_%d' % (par, j)
                P.op('act', lambda e, c1_=c1_, gc=gc, j=j: e.activation(c1_[:], gc[:, j:j + 1], AF.Exp), [ksm['gc']], [kc1])
                P.op('dve', lambda e, c1_=c1_, nbeta=nbeta, j=j: e.tensor_tensor(c1_[:], c1_[:], nbeta[:, j:j + 1], ALU.mult), [kc1, ksm['nbeta']], [kc1])
                qtil = ded['qtil'][par][j]
                Erow = ded['Erow'][par][j]
                P.op('dve', lambda e, par=par, qtil=qtil, j=j, Erow=Erow: e.scalar_tensor_tensor(qtil[:], qf[par][j // 2][:], scale, Erow[:], ALU.mult, ALU.mult), ['qf%d_%d' % (par, j // 2), dkk[j]['Erow']], [dkk[j]['qtil']])
            for lv in range(1, 7):
                if lv < 6:
                    bmn, kbmn = pbk.get()
                    for j in range(4):
                        (B, kB), (M, kM) = Bl[j], Ml[j]
                        P.mm(sl(bmn, j), B[:], M[:], True, True, [kB, kM], [kbmn])
                bbn, kbbn = pbk.get()
                for j in range(4):
                    (B, kB), (M, kM) = Bl[j], Ml[j]
                    P.mm(sl(bbn, j), M[:], B[:], True, True, [kB, kM], [kbbn])
                nB = []; nM = []
                for j in range(4):
                    Bn, kBn = tf.get()
                    P.op('act', lambda e, Bn=Bn, j=j, bbn=bbn: e.copy(Bn[:], sl(bbn, j)), [], [kbbn, kBn])
                    nB.append((Bn, kBn))
                for j in range(4):
                    if lv < 6:
                        Mn, kMn = tf.get()
                        P.op('dve', lambda e, Mn=Mn, j=j, bmn=bmn: e.tensor_copy(Mn[:], sl(bmn, j)), [], [kbmn, kMn])
                        nM.append((Mn, kMn))
                    else:
                        nM.append((None, None))
                bpn, kbpn = pbk.get()
                for j in range(4):
                    P.mm(sl(bpn, j), nB[j][0][:], Pl[j][0][:], True, True, [nB[j][1], Pl[j][1]], [kbpn])
                for j in range(4):
                    (Pc, kPc) = Pl[j]
                    if lv < 6:
                        Pn, kPn = tf.get()
                    else:
                        Pn, kPn = ded['TT'][par][j], dkk[j]['TT']
                    P.op('dve', lambda e, Pn=Pn, Pc=Pc, j=j, bpn=bpn: e.tensor_tensor(Pn[:], sl(bpn, j), Pc[:], ALU.add), [kPc], [kbpn, kPn])
                    Pl[j] = (Pn, kPn)
                    Bl[j], Ml[j] = nB[j], nM[j]
            bks, kbks = pbk.get()
            for j in range(4):
                P.mm(sl(bks, j), kTs[j][:], Sbf[j][:], True, True, [kk_[j], 'Sbf%d' % j], [kbks])
            rr = [tf.get() for j in range(4)]
            for j in range(4):
                P.op('dve', lambda e, par=par, j=j, r=rr[j][0], bks=bks: e.scalar_tensor_tensor(r[:], sl(bks, j), c1[par][j][:, 0:1], ded['vb'][par][j][:], ALU.mult, ALU.add),
                     ['c1%d_%d' % (par, j), dkk[j]['vb']], [kbks, rr[j][1]])
            bvn, kbvn = pbk.get()
            for j in range(4):
                P.mm(sl(bvn, j), ded['TT'][par][j][:], rr[j][0][:], True, True, [dkk[j]['TT'], rr[j][1]], [kbvn])
            vn = [tb.get() for j in range(4)]
            for j in range(4):
                P.op('act', lambda e, j=j, v_=vn[j][0], bvn=bvn: e.copy(v_[:], sl(bvn, j)), [], [kbvn, vn[j][1]])
            bo, kbo = pbk.get()
            bst, kbst = pbk.get()
            for j in range(4):
                P.mm(sl(bo, j), ded['qtil'][par][j][:], Sbf[j][:], True, False, [dkk[j]['qtil'], 'Sbf%d' % j], [kbo])
                P.mm(sl(bo, j), ded['QKdT'][par][j][:], vn[j][0][:], False, True, [dkk[j]['QKdT'], vn[j][1]], [kbo])
                P.mm(sl(bst, j), ded['kdec'][par][j][:], vn[j][0][:], True, True, [dkk[j]['kdec'], vn[j][1]], [kbst])
            for j in range(4):
                P.op('dve', lambda e, par=par, j=j, bst=bst: e.scalar_tensor_tensor(Sst[j][:], Sst[j][:], ded['Erow'][par][j][:, 127:128], sl(bst, j), ALU.mult, ALU.add),
                     [dkk[j]['Erow']], [kbst, 'S%d' % j])
                P.op('act', lambda e, j=j: e.copy(Sbf[j][:], Sst[j][:]), ['S%d' % j], ['Sbf%d' % j])
            Oss = []
            for j in range(4):
                Os, kOs = tf.get()
                P.op('dve', lambda e, Os=Os, j=j, bo=bo: e.tensor_copy(Os[:], sl(bo, j)), [], [kbo, kOs])
                Oss.append((Os, kOs))
            for j in range(4):
                Os, kOs = Oss[j]
                ssq, kss = t1.get()
                junk, kj = tf.get()
                P.op('pool', lambda e, ssq=ssq: e.memset(ssq[:], 0.0), [], [kss])
                P.op('act', lambda e, junk=junk, Os=Os, ssq=ssq: e.activation(junk[:], Os[:], AF.Square, accum_out=ssq[:]), [kOs], [kj, kss])
                P.op('dve', lambda e, ssq=ssq: e.tensor_scalar(ssq[:], ssq[:], 1.0 / 128, 1e-6, ALU.mult, ALU.add), [kss], [kss])
                P.op('act', lambda e, ssq=ssq: e.activation(ssq[:], ssq[:], AF.Sqrt), [kss], [kss])
                P.op('dve', lambda e, ssq=ssq: e.reciprocal(ssq[:], ssq[:]), [kss], [kss])
                P.op('dve', lambda e, Os=Os, ssq=ssq: e.scalar_tensor_tensor(Os[:], Os[:], ssq[:, 0:1], ng[:], ALU.mult, ALU.mult), [kss, 'ng'], [kOs])
                P.op('pool', lambda e, Os=Os, j=j, par=par: e.tensor_tensor(outt[par][:, j * 128:(j + 1) * 128], Os[:], zs[par][:, j * 128:(j + 1) * 128], ALU.mult),
                     [kOs, 'zs%d' % par], ['out%d' % par])
            P.ld(out_d[row0:row0 + 128, :], outt[par][:], reads=['out%d' % par])
            if dbg and n == 0 and b_ == 0:
                def dump(name, ap, key, shape, dt=F32):
                    dd = nc.dram_tensor(name, shape, dt, kind="ExternalOutput").ap()
                    P.ld(dd, ap, reads=[key])
                for c in range(4):
                    dump('g_qk%d' % c, qk[par][c][:], 'qk%d_%d' % (par, c), [128, 128], BF16)
                    dump('g_vT%d' % c, vT[par][c][:], 'vT%d_%d' % (par, c), [128, 128])
                    dump('g_raw%d' % c, raw[par][c][:], 'raw%d_%d' % (par, c), [128, 131])
                for n_ in ('beta', 'g', 'gc'):
                    dump('g_' + n_, sm4[n_][par][:], ksm[n_], [128, 4])
                for n_ in ded:
                    dump('g_' + n_, ded[n_][par][0][:], dkk[0][n_], [128, 128], F32 if n_ in ('Erow', 'TT', 'vb') else BF16)
                dump('g_zs', zs[par][:], 'zs%d' % par, [128, 512])
                dump('g_c1', c1[par][0][:], 'c1%d_0' % par, [128, 1])
                dump('g_r', rr[0][0][:], rr[0][1], [128, 128])
                dump('g_vn', vn[0][0][:], vn[0][1], [128, 128], BF16)
                dump('g_Os', Oss[0][0][:], Oss[0][1], [128, 128])
                dump('g_out', outt[par][:], 'out%d' % par, [128, 512], BF16)


def build_dn(NB_, NTL, dbg=False):
    nc = bass.Bass("TRN2", target_bir_lowering=False)
    x_d = nc.dram_tensor("x1", [NB_ * NTL * 128, D], F32, kind="ExternalInput").ap()
    out_d = nc.dram_tensor("out", [NB_ * NTL * 128, 512], BF16, kind="ExternalOutput").ap()
    W = {}
    W['w_sl'] = nc.dram_tensor("w_sl", [D, 1544], F32, kind="ExternalInput").ap()
    W['cw_l'] = nc.dram_tensor("cw_l", [128, 8, 4], F32, kind="ExternalInput").ap()
    W['norm_g'] = nc.dram_tensor("norm_g", [128], F32, kind="ExternalInput").ap()
    W['alog_l'] = nc.dram_tensor("alog_l", [4], F32, kind="ExternalInput").ap()
    W['dtb_l'] = nc.dram_tensor("dtb_l", [4], F32, kind="ExternalInput").ap()
    with ExitStack() as es:
        P = Prog(nc, es)
        cst = load_consts(P, nc, ['c_ident', 'c_uti', 'c_ones', 'c_negu', 'c_negl', 'c_strl'])
        emit_dn(P, nc, es, NB_, NTL, x_d, W, out_d, cst, dbg)
        P.finish()
    return nc


def dn_inputs(core, dn_w_in, dn_conv_w, dn_a_log, dn_dt_bias, dn_norm_g):
    c = core
    qc = slice(2 * c * 128, (2 * c + 2) * 128)
    cols = [dn_w_in[:, qc], dn_w_in[:, 2048 + 2 * c * 128:2048 + (2 * c + 2) * 128],
            dn_w_in[:, 4096 + 4 * c * 128:4096 + (4 * c + 4) * 128],
            dn_w_in[:, 8192 + 4 * c * 128:8192 + (4 * c + 4) * 128],
            dn_w_in[:, 12288 + 4 * c:12288 + 4 * c + 4], dn_w_in[:, 12320 + 4 * c:12320 + 4 * c + 4]]
    w_sl = np.ascontiguousarray(np.concatenate(cols, axis=1))
    cwc = np.concatenate([dn_conv_w[:, qc], dn_conv_w[:, 2048 + 2 * c * 128:2048 + (2 * c + 2) * 128],
                          dn_conv_w[:, 4096 + 4 * c * 128:4096 + (4 * c + 4) * 128]], axis=1)
    cw_l = np.ascontiguousarray(cwc.reshape(4, 8, 128).transpose(2, 1, 0))
    return {'w_sl': w_sl, 'cw_l': cw_l, 'norm_g': np.ascontiguousarray(dn_norm_g),
            'alog_l': np.ascontiguousarray(dn_a_log[4 * c:4 * c + 4]), 'dtb_l': np.ascontiguousarray(dn_dt_bias[4 * c:4 * c + 4])}


def emit_outproj(P, nc, NT, o_d, x_d, w_out, ln_g, ln_b, xm_d, cst):
    ident = cst['c_ident']
    r_d = nc.dram_tensor("op_r", [NT * 128, D], F32, kind="Internal").ap()
    with ExitStack() as ph:
        def sb(shape, dt, name):
            return ph.enter_context(nc.sbuf_tensor(name, list(shape), dt))

        def ps(name, dt=F32, w=512):
            return ph.enter_context(nc.psum_tensor(name, [128, w], dt))
        identb = sb([128, 128], BF16, 'op_identb')
        P.op('dve', lambda e: e.tensor_copy(identb[:], ident[:]), ['c_ident'], ['op_identb'])
        wo = sb([128, 32, 1024], BF16, 'op_wo')
        ob = [sb([128, 4096], BF16, 'op_ob%d' % i) for i in range(2)]
        oT = [sb([128, 32, 128], BF16, 'op_oT%d' % i) for i in range(2)]
        xh = [sb([128, 1024], F32, 'op_xh%d' % i) for i in range(2)]
        ptr = [ps('op_ptr%d' % i, BF16, 1024) for i in range(2)]
        py = [ps('op_py%d' % i) for i in range(4)]
        n4 = 0
        npy = 0
        for ch in range(2):
            P.ld(wo[:], w_out.rearrange("(k p) d -> p k d", p=128)[:, :, ch * 1024:(ch + 1) * 1024], writes=['wo'], eng='pool')
            for t in range(NT):
                b = t % 2
                P.ld(ob[b][:], o_d[t * 128:(t + 1) * 128, :], writes=['ob%d' % b])
                P.ld(xh[b][:], x_d[t * 128:(t + 1) * 128, ch * 1024:(ch + 1) * 1024], writes=['xh%d' % b])
                for q in range(8):
                    pq = ptr[n4 % 2]; kp = 'ptr%d' % (n4 % 2); n4 += 1
                    for j in range(4):
                        k = q * 4 + j
                        P.tr(pq[:, j * 128:(j + 1) * 128], ob[b][:, k * 128:(k + 1) * 128], identb[:], ['ob%d' % b, 'op_identb'], [kp])
                    if q % 2:
                        P.op('act', lambda e, q=q, b=b, pq=pq: e.copy(oT[b][:, q * 4:(q + 1) * 4, :], pq[:, 0:512].rearrange("p (a c) -> p a c", a=4)), [kp], ['oT%d' % b])
                    else:
                        P.op('dve', lambda e, q=q, b=b, pq=pq: e.tensor_copy(oT[b][:, q * 4:(q + 1) * 4, :], pq[:, 0:512].rearrange("p (a c) -> p a c", a=4)), [kp], ['oT%d' % b])
                for cc in range(2):
                    pyy = py[npy % 4]; kpy = 'py%d' % (npy % 4); npy += 1
                    for k in range(32):
                        P.mm(pyy[:], oT[b][:, k, :], wo[:, k, cc * 512:(cc + 1) * 512], k == 0, k == 31, ['oT%d' % b, 'wo'], [kpy])
                    P.op('dve', lambda e, b=b, cc=cc, pyy=pyy: e.scalar_tensor_tensor(xh[b][:, cc * 512:(cc + 1) * 512], xh[b][:, cc * 512:(cc + 1) * 512], ALPHA, pyy[:], ALU.mult, ALU.add),
                         [kpy, 'xh%d' % b], ['xh%d' % b])
                P.ld(r_d[t * 128:(t + 1) * 128, ch * 1024:(ch + 1) * 1024], xh[b][:], reads=['xh%d' % b], writes=['r_d'])
        P.barrier()
        P.flush()
    with ExitStack() as ph:
        def sb(shape, dt, name):
            return ph.enter_context(nc.sbuf_tensor(name, list(shape), dt))
        xt = [sb([128, D], F32, 'op2_xt%d' % i) for i in range(2)]
        ot = [sb([128, D], F32, 'op2_ot%d' % i) for i in range(2)]
        gt = sb([128, D], F32, 'op2_g'); bt = sb([128, D], F32, 'op2_b')
        st = sb([128, 24], F32, 'op2_st'); ag = sb([128, 2], F32, 'op2_ag'); rstd = sb([128, 1], F32, 'op2_rstd')
        P.ld(gt[:], ln_g.partition_broadcast(128), writes=['lng'])
        P.ld(bt[:], ln_b.partition_broadcast(128), writes=['lnb'])
        for t in range(NT):
            b = t % 2
            P.ld(xt[b][:], r_d[t * 128:(t + 1) * 128, :], writes=['xt%d' % b])
            emit_ln(P, xt[b], ot[b], gt, bt, 'op2_', ['xt%d' % b], ['ot%d' % b], st, ag, rstd)
            P.ld(xm_d[t * 128:(t + 1) * 128, :], ot[b][:], reads=['ot%d' % b], writes=['xm_d'])
        P.barrier()
        P.flush()


def _moe_W(nc):
    W = {}
    W['router_w'] = nc.dram_tensor("router_w", [D, NE], F32, kind="ExternalInput").ap()
    W['router_b'] = nc.dram_tensor("router_b", [NE], F32, kind="ExternalInput").ap()
    W['w_gate_up'] = nc.dram_tensor("w_gate_up", [NE, D, 2 * FF], F32, kind="ExternalInput").ap()
    W['bgu_l'] = nc.dram_tensor("bgu_l", [128, NE * 32], F32, kind="ExternalInput").ap()
    W['w_down'] = nc.dram_tensor("w_down", [NE, FF, D], F32, kind="ExternalInput").ap()
    W['b_down'] = nc.dram_tensor("b_down", [NE, D], F32, kind="ExternalInput").ap()
    W['ln_g'] = nc.dram_tensor("ln2_g", [D], F32, kind="ExternalInput").ap()
    W['ln_b'] = nc.dram_tensor("ln2_b", [D], F32, kind="ExternalInput").ap()
    return W


def build_L1(NQ, C):
    nc = bass.Bass("TRN2", target_bir_lowering=False)
    xc = nc.dram_tensor("xc", [(16 + NQ) * 128, D], F32, kind="ExternalInput").ap()
    hv_d = nc.dram_tensor("hv", [128, 1], F32, kind="ExternalInput").ap()
    out_d = nc.dram_tensor("out", [NQ * 128, D], F32, kind="ExternalOutput").ap()
    xm_d = nc.dram_tensor("xmid", [NQ * 128, D], F32, kind="Internal").ap()
    WA = {}
    WA['rel_bias'] = nc.dram_tensor("rel_bias", [32, 24], F32, kind="ExternalInput").ap()
    WA['attn_w_in'] = nc.dram_tensor("attn_w_in", [D, 9216], F32, kind="ExternalInput").ap()
    WA['attn_w_out'] = nc.dram_tensor("attn_w_out", [1024, D], F32, kind="ExternalInput").ap()
    WA['ln_g'] = nc.dram_tensor("ln1_g", [D], F32, kind="ExternalInput").ap()
    WA['ln_b'] = nc.dram_tensor("ln1_b", [D], F32, kind="ExternalInput").ap()
    WM = _moe_W(nc)
    with ExitStack() as es:
        P = Prog(nc, es)
        cst = load_consts(P, nc, ['c_ident', 'c_uts', 'c_ones'])
        with ExitStack() as aes:
            emit_attn(P, nc, aes, NQ, xc, hv_d, WA, xm_d, cst)
        emit_moe(P, nc, es, NQ, C, xm_d, out_d, WM, cst)
        P.finish()
    return nc


def build_L3(NT, C):
    nc = bass.Bass("TRN2", target_bir_lowering=False)
    o_d = nc.dram_tensor("o_in", [NT * 128, 4096], BF16, kind="ExternalInput").ap()
    x_d = nc.dram_tensor("x_res", [NT * 128, D], F32, kind="ExternalInput").ap()
    out_d = nc.dram_tensor("out", [NT * 128, D], F32, kind="ExternalOutput").ap()
    xm_d = nc.dram_tensor("xmid", [NT * 128, D], F32, kind="Internal").ap()
    w_out = nc.dram_tensor("dn_w_out", [4096, D], F32, kind="ExternalInput").ap()
    ln_g = nc.dram_tensor("ln1_g", [D], F32, kind="ExternalInput").ap()
    ln_b = nc.dram_tensor("ln1_b", [D], F32, kind="ExternalInput").ap()
    WM = _moe_W(nc)
    with ExitStack() as es:
        P = Prog(nc, es)
        cst = load_consts(P, nc, ['c_ident', 'c_uts', 'c_ones'])
        emit_outproj(P, nc, NT, o_d, x_d, w_out, ln_g, ln_b, xm_d, cst)
        emit_moe(P, nc, es, NT, C, xm_d, out_d, WM, cst)
        P.finish()
    return nc


def _moe_in(layer, router_w, router_b, w_gate_up, b_gate_up, w_down, b_down, ln2_g, ln2_b):
    bgu_l = np.ascontiguousarray(b_gate_up[layer].reshape(NE, 32, 128).transpose(2, 0, 1).reshape(128, NE * 32))
    return {'router_w': router_w[layer], 'router_b': router_b[layer], 'w_gate_up': w_gate_up[layer], 'bgu_l': bgu_l,
            'w_down': w_down[layer], 'b_down': b_down[layer], 'ln2_g': ln2_g[layer], 'ln2_b': ln2_b[layer]}


NCORES = 8
SEQ = 16384
TPC = 4096
CAP = 640


def kernel(x, rel_bias, attn_w_in, attn_w_out, dn_w_in, dn_conv_w, dn_a_log, dn_dt_bias,
           dn_norm_g, dn_w_out, ln1_g, ln1_b, router_w, router_b, w_gate_up, b_gate_up,
           w_down, b_down, ln2_g, ln2_b):
    f32 = np.float32
    args = [x, rel_bias, attn_w_in, attn_w_out, dn_w_in, dn_conv_w, dn_a_log, dn_dt_bias, dn_norm_g, dn_w_out,
            ln1_g, ln1_b, router_w, router_b, w_gate_up, b_gate_up, w_down, b_down, ln2_g, ln2_b]
    (x, rel_bias, attn_w_in, attn_w_out, dn_w_in, dn_conv_w, dn_a_log, dn_dt_bias, dn_norm_g, dn_w_out,
     ln1_g, ln1_b, router_w, router_b, w_gate_up, b_gate_up, w_down, b_down, ln2_g, ln2_b) = [np.asarray(a, dtype=f32) for a in args]
    cst = consts_np()
    cst.update(dn_consts_np())
    NQ = TPC // 128
    cores = list(range(NCORES))
    c3 = {k: cst[k] for k in ('c_ident', 'c_uts', 'c_ones')}
    com = dict(c3, **attn_static(), rel_bias=rel_bias, attn_w_in=attn_w_in[0], attn_w_out=attn_w_out[0],
               ln1_g=ln1_g[0], ln1_b=ln1_b[0],
               **_moe_in(0, router_w, router_b, w_gate_up, b_gate_up, w_down, b_down, ln2_g, ln2_b))
    ims = []
    for c in cores:
        b, sg = c // 4, c % 4
        t0 = sg * TPC
        if sg == 0:
            xc = np.concatenate([np.zeros((HALO, D), f32), x[b, 0:TPC]], axis=0)
            hv = np.zeros((128, 1), f32)
        else:
            xc = np.ascontiguousarray(x[b, t0 - HALO:t0 + TPC])
            hv = np.ones((128, 1), f32)
        ims.append(dict(com, xc=xc, hv=hv))
    nc1 = build_L1(NQ, CAP)
    r1 = run_bass_kernel_spmd(nc1, ims, core_ids=cores)
    x1 = np.concatenate([np.asarray(r1.results[c]['out']) for c in cores], axis=0)
    del ims, r1
    c6 = {k: cst[k] for k in ('c_ident', 'c_uti', 'c_ones', 'c_negu', 'c_negl', 'c_strl')}
    ims = [dict(c6, x1=x1, **dn_inputs(c, dn_w_in[0], dn_conv_w[0], dn_a_log[0], dn_dt_bias[0], dn_norm_g[0])) for c in cores]
    nc2 = build_dn(2, SEQ // 128)
    r2 = run_bass_kernel_spmd(nc2, ims, core_ids=cores)
    o_full = np.concatenate([np.asarray(r2.results[c]['out']) for c in cores], axis=1)
    del ims, r2
    com = dict(c3, dn_w_out=dn_w_out[0], ln1_g=ln1_g[1], ln1_b=ln1_b[1],
               **_moe_in(1, router_w, router_b, w_gate_up, b_gate_up, w_down, b_down, ln2_g, ln2_b))
    ims = [dict(com, o_in=np.ascontiguousarray(o_full[c * TPC:(c + 1) * TPC]), x_res=np.ascontiguousarray(x1[c * TPC:(c + 1) * TPC])) for c in cores]
    nc3 = build_L3(NQ, CAP)
    r3 = run_bass_kernel_spmd(nc3, ims, core_ids=cores)
    out = np.concatenate([np.asarray(r3.results[c]['out']) for c in cores], axis=0)
    return out.reshape(2, SEQ, D).astype(f32)
```

```python
import numpy as np
import concourse.bass as bass
import concourse.mybir as mybir
from contextlib import ExitStack
from concourse.bass_utils import run_bass_kernel_spmd

F32 = mybir.dt.float32
BF16 = mybir.dt.bfloat16
I32 = mybir.dt.int32
ALU = mybir.AluOpType
AF = mybir.ActivationFunctionType
AX = mybir.AxisListType


class Prog:
    ENG = ('pe', 'dve', 'act', 'pool', 'sp')
    EPOCH = 20000
    ND = 40
    NDI = 4

    def __init__(self, nc, es):
        self.nc, self.es = nc, es
        self.q = {e: [] for e in self.ENG}
        self.cnt = {e: 0 for e in self.ENG}
        self.epoch = {e: 0 for e in self.ENG}
        self.sems = {}
        self.seen = {e: {} for e in self.ENG}
        self.lastw = {}
        self.reads = {}
        self.dcnt = [0] * (self.ND + self.NDI)
        self.dnext = 0
        self.inext = 0
        self.nsem = 0
        self.ntile = 0

    def sem(self, key):
        if key not in self.sems:
            self.nsem += 1
            self.sems[key] = self.es.enter_context(self.nc.semaphore('s%d' % self.nsem))
        return self.sems[key]

    def sb(self, shape, dt, name=None):
        self.ntile += 1
        return self.es.enter_context(self.nc.sbuf_tensor(name or 't%d' % self.ntile, list(shape), dt))

    def ps(self, shape, dt, name=None):
        self.ntile += 1
        return self.es.enter_context(self.nc.psum_tensor(name or 'p%d' % self.ntile, list(shape), dt))

    def _wait(self, eng, tok):
        key, val = tok
        if eng == 'pe' and key[0] == 'e' and key[1] == 'pe':
            return
        if self.seen[eng].get(key, 0) >= val:
            return
        self.seen[eng][key] = val
        s = self.sem(key)
        self.q[eng].append(lambda e, s=s, val=val: e.wait_ge(s, val))

    def _deps(self, eng, reads, writes):
        for k in reads:
            t = self.lastw.get(k)
            if t:
                self._wait(eng, t)
        for k in writes:
            t = self.lastw.get(k)
            if t:
                self._wait(eng, t)
            for t in self.reads.get(k, ()):
                self._wait(eng, t)

    def _commit(self, tok, reads, writes):
        for k in reads:
            lst = self.reads.setdefault(k, [])
            for i, t in enumerate(lst):
                if t[0] == tok[0]:
                    lst[i] = tok
                    break
            else:
                lst.append(tok)
        for k in writes:
            self.lastw[k] = tok
            self.reads[k] = []

    def op(self, eng, fn, reads=(), writes=()):
        self._deps(eng, reads, writes)
        self.cnt[eng] += 1
        if self.cnt[eng] > self.EPOCH:
            self.epoch[eng] += 1
            self.cnt[eng] = 1
        key = ('e', eng, self.epoch[eng])
        s = self.sem(key)
        self.q[eng].append(lambda e, s=s, fn=fn: fn(e).then_inc(s, 1))
        self._commit((key, self.cnt[eng]), reads, writes)

    def dma(self, eng, fn, reads=(), writes=(), ind=False):
        if ind:
            i = self.ND + self.inext
            self.inext = (self.inext + 1) % self.NDI
        else:
            i = self.dnext
            self.dnext = (i + 1) % self.ND
        key = ('d', i)
        if self.dcnt[i]:
            self._wait(eng, (key, self.dcnt[i]))
        self._deps(eng, reads, writes)
        self.dcnt[i] += 16
        s = self.sem(key)
        self.q[eng].append(lambda e, s=s, fn=fn: fn(e).then_inc(s, 16))
        self._commit((key, self.dcnt[i]), reads, writes)

    def finish(self):
        for i in range(self.ND + self.NDI):
            if self.dcnt[i]:
                self._wait('sp', (('d', i), self.dcnt[i]))
        for e in ('pe', 'dve', 'act', 'pool'):
            if self.cnt[e]:
                self._wait('sp', (('e', e, self.epoch[e]), self.cnt[e]))
        self.flush()

    def flush(self):
        q = self.q
        self.q = {e: [] for e in self.ENG}
        with self.nc.Block() as block:
            @block.tensor
            def _(e):
                for f in q['pe']:
                    f(e)

            @block.vector
            def _(e):
                for f in q['dve']:
                    f(e)

            @block.scalar
            def _(e):
                for f in q['act']:
                    f(e)

            @block.gpsimd
            def _(e):
                for f in q['pool']:
                    f(e)

            @block.sync
            def _(e):
                for f in q['sp']:
                    f(e)

    def mm(self, out, lhsT, rhs, start=True, stop=True, reads=(), writes=()):
        self.op('pe', lambda e: e.matmul(out, lhsT, rhs, start=start, stop=stop), reads, writes)

    def tr(self, out, in_, ident, reads=(), writes=()):
        self.op('pe', lambda e: e.transpose(out, in_, ident), reads, writes)

    def ld(self, out, in_, reads=(), writes=(), eng='sp'):
        self.dma(eng, lambda e: e.dma_start(out=out, in_=in_), reads, writes)

    def barrier(self):
        toks = []
        for i in range(self.ND + self.NDI):
            if self.dcnt[i]:
                toks.append((('d', i), self.dcnt[i]))
        for e in ('pe', 'dve', 'act', 'pool'):
            for ep in range(self.epoch[e] + 1):
                c = self.cnt[e] if ep == self.epoch[e] else self.EPOCH
                if c:
                    toks.append((('e', e, ep), c))
        for eng in self.ENG:
            for t in toks:
                self._wait(eng, t)
        self.lastw = {}
        self.reads = {}


D = 2048
ALPHA = 2.0 ** 0.5
LN_EPS = 1e-5
NE = 32
FF = 2048


def consts_np():
    c = {}
    c['c_ident'] = np.eye(128, dtype=np.float32)
    c['c_uts'] = np.triu(np.ones((128, 128), np.float32), 1)
    c['c_uti'] = np.triu(np.ones((128, 128), np.float32), 0)
    c['c_ones'] = np.ones((128, 128), np.float32)
    return c


def load_consts(P, nc, names):
    out = {}
    for n in names:
        d = nc.dram_tensor(n, [128, 128], F32, kind="ExternalInput").ap()
        t = P.sb([128, 128], F32, name='sb_' + n)
        P.ld(t[:], d, writes=[n])
        out[n] = t
    return out


def emit_ln(P, src, dst, gt, bt, tmpk, keys_r, keys_w, st, ag, rstd):
    for i in range(4):
        P.op('dve', lambda e, i=i: e.bn_stats(st[:, i * 6:(i + 1) * 6], src[:, i * 512:(i + 1) * 512]), keys_r, [tmpk + 'st'])
    P.op('dve', lambda e: e.bn_aggr(ag[:], st[:]), [tmpk + 'st'], [tmpk + 'ag'])
    P.op('dve', lambda e: e.tensor_scalar(rstd[:], ag[:, 1:2], LN_EPS, None, ALU.add), [tmpk + 'ag'], [tmpk + 'rs'])
    P.op('act', lambda e: e.activation(rstd[:], rstd[:], AF.Sqrt), [tmpk + 'rs'], [tmpk + 'rs'])
    P.op('dve', lambda e: e.reciprocal(rstd[:], rstd[:]), [tmpk + 'rs'], [tmpk + 'rs'])
    P.op('dve', lambda e: e.tensor_scalar(dst[:], src[:], ag[:, 0:1], rstd[:, 0:1], ALU.subtract, ALU.mult),
         list(keys_r) + [tmpk + 'ag', tmpk + 'rs'], keys_w)
    P.op('pool', lambda e: e.tensor_tensor(dst[:], dst[:], gt[:], ALU.mult), list(keys_w) + ['lng'], keys_w)
    P.op('pool', lambda e: e.tensor_tensor(dst[:], dst[:], bt[:], ALU.add), list(keys_w) + ['lnb'], keys_w)


def emit_moe(P, nc, es, NT, C, x_in, out_d, W, cst, dbg=False):
    NSLOT = NE * C
    NB = C // 128
    H = C // 2
    BIG = float(NSLOT + 4096)
    kind = "ExternalOutput" if dbg else "Internal"
    xg = nc.dram_tensor("moe_xg", [NSLOT + 1, D], BF16, kind=kind).ap()
    yd = nc.dram_tensor("moe_y", [NSLOT + 1, D], F32, kind=kind).ap()
    if dbg:
        d_sl = nc.dram_tensor("dbg_slots", [128, NT * 4], I32, kind="ExternalOutput").ap()
        d_ga = nc.dram_tensor("dbg_gates", [128, NT * 4], F32, kind="ExternalOutput").ap()
    ident, uts, ones = cst['c_ident'], cst['c_uts'], cst['c_ones']
    slots_all = P.sb([128, NT, 4], I32, name='slots_all')
    gates_all = P.sb([128, NT, 4], F32, name='gates_all')
    identb = P.sb([128, 128], BF16, name='identb')
    P.op('dve', lambda e: e.tensor_copy(identb[:], ident[:]), ['c_ident'], ['identb'])
    scat_keys = []
    with ExitStack() as ph:
        def sb(shape, dt, name):
            return ph.enter_context(nc.sbuf_tensor(name, list(shape), dt))

        def ps(name):
            return ph.enter_context(nc.psum_tensor(name, [128, 512], F32))
        xt = [sb([128, D], F32, 'r_xt%d' % i) for i in range(2)]
        xb = [sb([128, D], BF16, 'r_xb%d' % i) for i in range(2)]
        xT = sb([128, 16, 128], F32, 'r_xT')
        rw = sb([128, 16, NE], F32, 'r_rw')
        rb = sb([128, NE], F32, 'r_rb')
        eoff = sb([128, NE], F32, 'r_eoff')
        base = sb([128, NE], F32, 'r_base')
        zt = sb([128, D], F32, 'r_zero')
        pt = [ps('r_pt%d' % i) for i in range(2)]
        pl = ps('r_pl'); pr = ps('r_pr'); pb = ps('r_pb')
        sm = {n: sb([128, NE], F32, 'r_' + n) for n in ('lg', 'mask', 'ex', 'G', 'rank', 'ov', 'slot', 'v', 'oh')}
        t8 = sb([128, 8], F32, 'r_t8'); v8 = sb([128, 8], F32, 'r_v8')
        s1 = {n: sb([128, 1], F32, 'r_' + n) for n in ('negm', 'ssum', 'rs')}
        slotf = sb([128, 4], F32, 'r_slotf')
        P.ld(rw[:], W['router_w'].rearrange("(k p) e -> p k e", p=128), writes=['rw'])
        P.ld(rb[:], W['router_b'].partition_broadcast(128), writes=['rb'])
        P.op('pool', lambda e: e.iota(eoff[:], [[C, NE]], base=0, channel_multiplier=0, allow_small_or_imprecise_dtypes=True), [], ['eoff'])
        P.op('pool', lambda e: e.memset(base[:], 0.0), [], ['base'])
        P.op('pool', lambda e: e.memset(zt[:], 0.0), [], ['zt'])
        P.ld(yd[NSLOT:NSLOT + 1, :], zt[0:1, :], reads=['zt'], writes=['ydummy'])
        for t in range(NT):
            b = t % 2
            kx, kb = 'xt%d' % b, 'xb%d' % b
            P.ld(xt[b][:], x_in[t * 128:(t + 1) * 128, :], writes=[kx])
            P.op('act', lambda e, b=b: e.copy(xb[b][:], xt[b][:]), [kx], [kb])
            for q in range(4):
                pq = pt[q % 2]
                kp = 'pt%d' % (q % 2)
                for j in range(4):
                    k = q * 4 + j
                    P.tr(pq[:, j * 128:(j + 1) * 128], xt[b][:, k * 128:(k + 1) * 128], ident[:], [kx, 'c_ident'], [kp])
                eng = 'dve' if q % 2 == 0 else 'act'
                if eng == 'dve':
                    P.op('dve', lambda e, q=q, pq=pq: e.tensor_copy(xT[:, q * 4:(q + 1) * 4, :], pq[:].rearrange("p (a b) -> p a b", a=4)), [kp], ['xT%d' % q])
                else:
                    P.op('act', lambda e, q=q, pq=pq: e.copy(xT[:, q * 4:(q + 1) * 4, :], pq[:].rearrange("p (a b) -> p a b", a=4)), [kp], ['xT%d' % q])
            for k in range(16):
                P.mm(pl[:, 0:NE], xT[:, k, :], rw[:, k, :], k == 0, k == 15, ['xT%d' % (k // 4), 'rw'], ['pl'])
            lg, mask, ex, G, rank, ov, slot, v, oh = (sm[n] for n in ('lg', 'mask', 'ex', 'G', 'rank', 'ov', 'slot', 'v', 'oh'))
            P.op('dve', lambda e: e.tensor_tensor(lg[:], pl[:, 0:NE], rb[:], ALU.add), ['pl', 'rb'], ['lg'])
            P.op('dve', lambda e: e.max(t8[:], lg[:]), ['lg'], ['t8'])
            P.op('dve', lambda e: e.tensor_scalar(mask[:], lg[:], t8[:, 3:4], None, ALU.is_ge), ['lg', 't8'], ['mask'])
            P.op('dve', lambda e: e.tensor_scalar(s1['negm'][:], t8[:, 0:1], -1.0, None, ALU.mult), ['t8'], ['negm'])
            P.op('act', lambda e: e.activation(ex[:], lg[:], AF.Exp, bias=s1['negm'][:, 0:1], scale=1.0), ['lg', 'negm'], ['ex'])
            P.op('dve', lambda e: e.tensor_tensor(ex[:], ex[:], mask[:], ALU.mult), ['ex', 'mask'], ['ex'])
            P.op('dve', lambda e: e.reduce_sum(s1['ssum'][:], ex[:], AX.X), ['ex'], ['ssum'])
            P.op('dve', lambda e: e.reciprocal(s1['rs'][:], s1['ssum'][:]), ['ssum'], ['rs'])
            P.op('dve', lambda e: e.tensor_scalar(G[:], ex[:], s1['rs'][:, 0:1], None, ALU.mult), ['ex', 'rs'], ['G'])
            P.mm(pr[:, 0:NE], uts[:], mask[:], True, True, ['c_uts', 'mask'], ['pr'])
            P.mm(pb[:, 0:NE], ones[:], mask[:], True, True, ['c_ones', 'mask'], ['pb'])
            P.op('dve', lambda e: e.tensor_tensor(rank[:], pr[:, 0:NE], base[:], ALU.add), ['pr', 'base'], ['rank'])
            P.op('dve', lambda e: e.tensor_tensor(base[:], pb[:, 0:NE], base[:], ALU.add), ['pb', 'base', 'rank'], ['base'])
            P.op('dve', lambda e: e.tensor_scalar(ov[:], rank[:], float(C), None, ALU.is_lt), ['rank'], ['ov'])
            P.op('dve', lambda e: e.tensor_tensor(slot[:], rank[:], eoff[:], ALU.add), ['rank', 'eoff'], ['slot'])
            P.op('dve', lambda e: e.tensor_scalar(slot[:], slot[:], float(-NSLOT), None, ALU.add), ['slot'], ['slot'])
            P.op('dve', lambda e: e.tensor_tensor(slot[:], slot[:], ov[:], ALU.mult), ['slot', 'ov'], ['slot'])
            P.op('dve', lambda e: e.tensor_scalar(slot[:], slot[:], float(NSLOT), None, ALU.add), ['slot'], ['slot'])
            P.op('dve', lambda e: e.tensor_tensor(G[:], G[:], ov[:], ALU.mult), ['G', 'ov'], ['G'])
            P.op('dve', lambda e: e.tensor_scalar(v[:], slot[:], -1.0, BIG, ALU.mult, ALU.add), ['slot'], ['v'])
            P.op('dve', lambda e: e.tensor_tensor(v[:], v[:], mask[:], ALU.mult), ['v', 'mask'], ['v'])
            P.op('dve', lambda e: e.max(v8[:], v[:]), ['v'], ['v8'])
            P.op('dve', lambda e: e.tensor_scalar(slotf[:], v8[:, 0:4], -1.0, BIG, ALU.mult, ALU.add), ['v8'], ['slotf'])
            ks = 'slots%d' % t
            P.op('dve', lambda e, t=t: e.tensor_copy(slots_all[:, t, :], slotf[:]), ['slotf'], [ks])
            for c in range(4):
                P.op('dve', lambda e, c=c: e.tensor_scalar(oh[:], v[:], v8[:, c:c + 1], None, ALU.is_equal), ['v', 'v8'], ['oh'])
                P.op('dve', lambda e: e.tensor_tensor(oh[:], oh[:], G[:], ALU.mult), ['oh', 'G'], ['oh'])
                P.op('dve', lambda e, t=t, c=c: e.reduce_sum(gates_all[:, t, c:c + 1], oh[:], AX.X), ['oh'], ['gates%d_%d' % (t, c)])
            for c in range(4):
                sk = 'scat%d_%d' % (t, c)
                scat_keys.append(sk)
                P.dma('pool', lambda e, t=t, c=c, b=b: e.indirect_dma_start(
                    out=xg, out_offset=bass.IndirectOffsetOnAxis(ap=slots_all[:, t, c:c + 1], axis=0),
                    in_=xb[b][:], in_offset=None), [kb, ks], [sk], ind=True)
        if dbg:
            d_lg = nc.dram_tensor("dbg_lg", [128, NE], F32, kind="ExternalOutput").ap()
            d_xT = nc.dram_tensor("dbg_xT", [128, 16 * 128], F32, kind="ExternalOutput").ap()
            P.barrier()
            P.ld(d_lg, sm['lg'][:])
            P.ld(d_xT, xT[:].rearrange("p a b -> p (a b)"))
            P.ld(d_sl, slots_all[:].rearrange("p t c -> p (t c)"))
            P.ld(d_ga, gates_all[:].rearrange("p t c -> p (t c)"))
        P.barrier()
        P.flush()
    with ExitStack() as ph:
        def sb(shape, dt, name):
            return ph.enter_context(nc.sbuf_tensor(name, list(shape), dt))

        def ps(name, dt=F32, w=512):
            return ph.enter_context(nc.psum_tensor(name, [128, w], dt))
        xgt = sb([128, NB, D], BF16, 'e_xgt')
        xT = sb([128, 16, C], BF16, 'e_xT')
        wgu = [sb([128, 16, 512], BF16, 'e_wgu%d' % i) for i in range(2)]
        wd = [sb([128, 16, 512], BF16, 'e_wd%d' % i) for i in range(2)]
        actT = sb([128, 16, C], BF16, 'e_actT')
        bgu = sb([128, NE * 32], F32, 'e_bgu')
        bd = sb([128, D], F32, 'e_bd')
        gl = [sb([128, H], F32, 'e_gl%d' % i) for i in range(2)]
        sg = [sb([128, H], F32, 'e_sg%d' % i) for i in range(2)]
        ln = [sb([128, H], F32, 'e_ln%d' % i) for i in range(2)]
        ysb = [sb([128, 512], F32, 'e_ysb%d' % i) for i in range(2)]
        ptr = [ps('e_ptr%d' % i, BF16, 1024) for i in range(2)]
        pg = [ps('e_pg%d' % i) for i in range(2)]
        pn = [ps('e_pn%d' % i) for i in range(2)]
        py = [ps('e_py%d' % i) for i in range(2)]
        P.ld(bgu[:], W['bgu_l'], writes=['bgu'])
        nw = 0
        nd = 0
        ny = 0
        nel = 0
        for ex_ in range(NE):
            P.ld(xgt[:], xg[ex_ * C:(ex_ + 1) * C, :].rearrange("(b p) d -> p b d", p=128), writes=['xgt'])
            P.ld(bd[:], W['b_down'][ex_, :].partition_broadcast(128), writes=['bd'])
            n4 = 0
            for bk in range(NB):
                for q in range(4):
                    pq = ptr[n4 % 2]; kp = 'ptr%d' % (n4 % 2)
                    for j in range(4):
                        k = q * 4 + j
                        P.tr(pq[:, j * 128:(j + 1) * 128], xgt[:, bk, k * 128:(k + 1) * 128], identb[:], ['xgt', 'identb'], [kp])
                    if n4 % 2 == 0:
                        P.op('dve', lambda e, q=q, bk=bk, pq=pq: e.tensor_copy(xT[:, q * 4:(q + 1) * 4, bk * 128:(bk + 1) * 128], pq[:, 0:512].rearrange("p (a b) -> p a b", a=4)), [kp], ['xT'])
                    else:
                        P.op('act', lambda e, q=q, bk=bk, pq=pq: e.copy(xT[:, q * 4:(q + 1) * 4, bk * 128:(bk + 1) * 128], pq[:, 0:512].rearrange("p (a b) -> p a b", a=4)), [kp], ['xT'])
                    n4 += 1
            for g in range(8):
                wb_ = wgu[nw % 2]; kw = 'wgu%d' % (nw % 2); nw += 1
                wv = W['w_gate_up'][ex_].rearrange("(k p) f -> p k f", p=128)
                P.ld(wb_[:, :, 0:256], wv[:, :, g * 256:(g + 1) * 256], writes=[kw + 'a'], eng='pool')
                P.ld(wb_[:, :, 256:512], wv[:, :, FF + g * 256:FF + (g + 1) * 256], writes=[kw + 'b'], eng='pool')
                for j in range(2):
                    fj = g * 2 + j
                    for s in range(2):
                        i2 = nel % 2; nel += 1
                        kg, kn = 'pg%d' % i2, 'pn%d' % i2
                        for k in range(16):
                            P.mm(pg[i2][:, 0:H], wb_[:, k, j * 128:(j + 1) * 128], xT[:, k, s * H:(s + 1) * H], k == 0, k == 15, [kw + 'a', 'xT'], [kg])
                        for k in range(16):
                            P.mm(pn[i2][:, 0:H], wb_[:, k, 256 + j * 128:256 + (j + 1) * 128], xT[:, k, s * H:(s + 1) * H], k == 0, k == 15, [kw + 'b', 'xT'], [kn])
                        cg = ex_ * 32 + fj
                        cl = ex_ * 32 + 16 + fj
                        P.op('dve', lambda e, i2=i2, cg=cg: e.tensor_scalar(gl[i2][:], pg[i2][:, 0:H], bgu[:, cg:cg + 1], 7.0, ALU.add, ALU.min), [kg, 'bgu'], ['gl%d' % i2])
                        P.op('act', lambda e, i2=i2: e.activation(sg[i2][:], gl[i2][:], AF.Sigmoid, scale=1.702), ['gl%d' % i2], ['sg%d' % i2])
                        P.op('dve', lambda e, i2=i2, cl=cl: e.tensor_scalar(ln[i2][:], pn[i2][:, 0:H], bgu[:, cl:cl + 1], 7.0, ALU.add, ALU.min), [kn, 'bgu'], ['ln%d' % i2])
                        P.op('pool', lambda e, i2=i2: e.tensor_scalar(ln[i2][:], ln[i2][:], -7.0, 1.0, ALU.max, ALU.add), ['ln%d' % i2], ['ln%d' % i2])
                        P.op('pool', lambda e, i2=i2: e.tensor_tensor(gl[i2][:], gl[i2][:], sg[i2][:], ALU.mult), ['gl%d' % i2, 'sg%d' % i2], ['gl%d' % i2])
                        P.op('dve', lambda e, i2=i2, fj=fj, s=s: e.tensor_tensor(actT[:, fj, s * H:(s + 1) * H], gl[i2][:], ln[i2][:], ALU.mult), ['gl%d' % i2, 'ln%d' % i2], ['actT'])
            for c in range(4):
                wb_ = wd[nd % 2]; kw = 'wd%d' % (nd % 2); nd += 1
                P.ld(wb_[:], W['w_down'][ex_].rearrange("(k p) d -> p k d", p=128)[:, :, c * 512:(c + 1) * 512], writes=[kw], eng='pool')
                for bk in range(NB):
                    i2 = ny % 2; ny += 1
                    for k in range(16):
                        P.mm(py[i2][:], actT[:, k, bk * 128:(bk + 1) * 128], wb_[:, k, :], k == 0, k == 15, ['actT', kw], ['py%d' % i2])
                    P.op('dve', lambda e, i2=i2, c=c: e.tensor_tensor(ysb[i2][:], py[i2][:], bd[:, c * 512:(c + 1) * 512], ALU.add), ['py%d' % i2, 'bd'], ['ysb%d' % i2])
                    P.ld(yd[ex_ * C + bk * 128:ex_ * C + (bk + 1) * 128, c * 512:(c + 1) * 512], ysb[i2][:], reads=['ysb%d' % i2], writes=['yd'], eng='sp')
        P.barrier()
        P.flush()
    with ExitStack() as ph:
        def sb(shape, dt, name):
            return ph.enter_context(nc.sbuf_tensor(name, list(shape), dt))
        xt = [sb([128, D], F32, 'c_xt%d' % i) for i in range(2)]
        yg = [sb([128, D], F32, 'c_yg%d' % i) for i in range(4)]
        ot = [sb([128, D], F32, 'c_ot%d' % i) for i in range(2)]
        gt = sb([128, D], F32, 'c_g'); bt = sb([128, D], F32, 'c_b')
        st = sb([128, 24], F32, 'c_st'); ag = sb([128, 2], F32, 'c_ag'); rstd = sb([128, 1], F32, 'c_rstd')
        P.ld(gt[:], W['ln_g'].partition_broadcast(128), writes=['lng'])
        P.ld(bt[:], W['ln_b'].partition_broadcast(128), writes=['lnb'])
        for t in range(NT):
            b = t % 2
            kx = 'cxt%d' % b
            P.ld(xt[b][:], x_in[t * 128:(t + 1) * 128, :], writes=[kx])
            for c in range(4):
                P.dma('pool', lambda e, t=t, c=c: e.indirect_dma_start(
                    out=yg[c][:], out_offset=None, in_=yd,
                    in_offset=bass.IndirectOffsetOnAxis(ap=slots_all[:, t, c:c + 1], axis=0),
                    ), [], ['yg%d' % c], ind=True)
            P.op('act', lambda e, b=b: e.mul(xt[b][:], xt[b][:], ALPHA), [kx], [kx])
            for c in range(4):
                P.op('dve', lambda e, b=b, c=c, t=t: e.scalar_tensor_tensor(xt[b][:], yg[c][:], gates_all[:, t, c:c + 1], xt[b][:], ALU.mult, ALU.add), ['yg%d' % c, kx], [kx])
            ko = 'cot%d' % b
            emit_ln(P, xt[b], ot[b], gt, bt, 'c_', [kx], [ko], st, ag, rstd)
            P.ld(out_d[t * 128:(t + 1) * 128, :], ot[b][:], reads=[ko])
        P.barrier()
        P.flush()


def build_moe(NT, C, dbg=False):
    nc = bass.Bass("TRN2", target_bir_lowering=False)
    x_in = nc.dram_tensor("x_in", [NT * 128, D], F32, kind="ExternalInput").ap()
    out_d = nc.dram_tensor("out", [NT * 128, D], F32, kind="ExternalOutput").ap()
    W = {}
    W['router_w'] = nc.dram_tensor("router_w", [D, NE], F32, kind="ExternalInput").ap()
    W['router_b'] = nc.dram_tensor("router_b", [NE], F32, kind="ExternalInput").ap()
    W['w_gate_up'] = nc.dram_tensor("w_gate_up", [NE, D, 2 * FF], F32, kind="ExternalInput").ap()
    W['bgu_l'] = nc.dram_tensor("bgu_l", [128, NE * 32], F32, kind="ExternalInput").ap()
    W['w_down'] = nc.dram_tensor("w_down", [NE, FF, D], F32, kind="ExternalInput").ap()
    W['b_down'] = nc.dram_tensor("b_down", [NE, D], F32, kind="ExternalInput").ap()
    W['ln_g'] = nc.dram_tensor("ln_g", [D], F32, kind="ExternalInput").ap()
    W['ln_b'] = nc.dram_tensor("ln_b", [D], F32, kind="ExternalInput").ap()
    with ExitStack() as es:
        P = Prog(nc, es)
        cst = load_consts(P, nc, ['c_ident', 'c_uts', 'c_ones'])
        emit_moe(P, nc, es, NT, C, x_in, out_d, W, cst, dbg)
        P.finish()
    return nc


def moe_inputs(x_sh, router_w, router_b, w_gate_up, b_gate_up, w_down, b_down, ln_g, ln_b):
    c = consts_np()
    bgu_l = np.ascontiguousarray(b_gate_up.reshape(NE, 32, 128).transpose(2, 0, 1).reshape(128, NE * 32))
    com = {'router_w': router_w, 'router_b': router_b, 'w_gate_up': w_gate_up, 'bgu_l': bgu_l,
           'w_down': w_down, 'b_down': b_down, 'ln_g': ln_g, 'ln_b': ln_b,
           'c_ident': c['c_ident'], 'c_uts': c['c_uts'], 'c_ones': c['c_ones']}
    return [dict(com, x_in=np.ascontiguousarray(xs)) for xs in x_sh]


GROUPS = ((128, 1), (512, 4), (2048, 16))
NEG = -30000.0
HALO = 2048


def attn_static():
    import math
    out = {}
    for g, (w, d) in enumerate(GROUPS):
        L = w + 256
        S = np.zeros((33, L), np.float32)
        for n in range(L):
            dl = n - 127
            if dl >= 0 and dl % d == 0 and dl <= w:
                if dl < 16:
                    bk = dl
                else:
                    v = np.log(np.float32(max(dl, 1)) / np.float32(16)) / np.float32(math.log(2048 / 16)) * np.float32(16)
                    bk = min(16 + int(np.float32(v).astype(np.int32)), 31)
                S[bk, n] = 1.0
            else:
                S[32, n] = NEG
        out['c_S%d' % g] = S
    return out


def emit_attn(P, nc, es, NQ, xc, hv_d, W, out_d, cst):
    NTT = 16 + NQ
    NTOK = NTT * 128
    ident = cst['c_ident']
    xT_d = nc.dram_tensor("a_xT", [16, 128, NTOK], BF16, kind="Internal").ap()
    qT_d = nc.dram_tensor("a_qT", [3, 8, 128, NQ * 128], BF16, kind="Internal").ap()
    kT_d = nc.dram_tensor("a_kT", [3, 8, 128, NTOK], BF16, kind="Internal").ap()
    v_d = nc.dram_tensor("a_v", [3, 8, NTOK, 128], BF16, kind="Internal").ap()
    Zs = [[nc.dram_tensor("a_Z%d_%d" % (g, h), [128, GROUPS[g][0] + 256], F32, kind="Internal") for h in range(8)] for g in range(3)]
    identb = es.enter_context(nc.sbuf_tensor('a_identb', [128, 128], BF16))
    P.op('dve', lambda e: e.tensor_copy(identb[:], ident[:]), ['c_ident'], ['a_identb'])
    o_all = es.enter_context(nc.sbuf_tensor('a_oall', [128, NQ, 1024], BF16))
    with ExitStack() as ph:
        def sb(shape, dt, name):
            return ph.enter_context(nc.sbuf_tensor(name, list(shape), dt))
        rbT = sb([33, 24], F32, 'a0_rbT')
        lb = sb([33, 128], F32, 'a0_lb')
        Ssb = [sb([33, GROUPS[g][0] + 256], F32, 'a0_S%d' % g) for g in range(3)]
        fb = [sb([128, 2304], F32, 'a0_fb%d' % i) for i in range(2)]
        pz = [ph.enter_context(nc.psum_tensor('a0_pz%d' % i, [128, 512], F32)) for i in range(2)]
        P.op('pool', lambda e: e.memset(rbT[:], 1.0), [], ['rbT'])
        P.ld(rbT[0:32, :], W['rel_bias'], writes=['rbT'])
        for g in range(3):
            Sd = nc.dram_tensor('c_S%d' % g, [33, GROUPS[g][0] + 256], F32, kind="ExternalInput").ap()
            P.ld(Ssb[g][:], Sd, writes=['S%d' % g])
        n = 0
        for g in range(3):
            L = GROUPS[g][0] + 256
            for h in range(8):
                c = g * 8 + h
                P.op('dve', lambda e, c=c: e.tensor_copy(lb[:], rbT[:, c:c + 1].to_broadcast([33, 128])), ['rbT'], ['lb'])
                f = fb[n % 2]; kf = 'fb%d' % (n % 2); n += 1
                for ci, c0 in enumerate(range(0, L, 512)):
                    cw = min(512, L - c0)
                    pzz = pz[ci % 2]
                    P.mm(pzz[:, 0:cw], lb[:], Ssb[g][:, c0:c0 + cw], True, True, ['lb', 'S%d' % g], ['pz%d' % (ci % 2)])
                    P.op('act', lambda e, pzz=pzz, f=f, c0=c0, cw=cw: e.copy(f[:, c0:c0 + cw], pzz[:, 0:cw]), ['pz%d' % (ci % 2)], [kf])
                P.ld(Zs[g][h].ap(), f[:, 0:L], reads=[kf], writes=['Z'])
        P.barrier()
        P.flush()
    with ExitStack() as ph:
        def sb(shape, dt, name):
            return ph.enter_context(nc.sbuf_tensor(name, list(shape), dt))

        def ps(name, dt=F32, w=512):
            return ph.enter_context(nc.psum_tensor(name, [128, w], dt))
        xt = [sb([128, D], F32, 'a1_xt%d' % i) for i in range(2)]
        xb = [sb([128, D], BF16, 'a1_xb%d' % i) for i in range(2)]
        xTt = [sb([128, 16, 128], BF16, 'a1_xTt%d' % i) for i in range(2)]
        wblk = [sb([128, 16, 512], BF16, 'a1_w%d' % i) for i in range(2)]
        xTg = [sb([128, 16, 512], BF16, 'a1_xTg%d' % i) for i in range(2)]
        stg = [sb([128, 512], BF16, 'a1_stg%d' % i) for i in range(4)]
        ptr = [ps('a1_ptr%d' % i, BF16, 1024) for i in range(2)]
        pm = [ps('a1_pm%d' % i) for i in range(4)]
        n4 = 0
        for t in range(NTT):
            b = t % 2
            P.ld(xt[b][:], xc[t * 128:(t + 1) * 128, :], writes=['xt%d' % b])
            P.op('act', lambda e, b=b: e.copy(xb[b][:], xt[b][:]), ['xt%d' % b], ['xb%d' % b])
            for q in range(4):
                pq = ptr[n4 % 2]; kp = 'ptr%d' % (n4 % 2)
                for j in range(4):
                    k = q * 4 + j
                    P.tr(pq[:, j * 128:(j + 1) * 128], xb[b][:, k * 128:(k + 1) * 128], identb[:], ['xb%d' % b, 'a_identb'], [kp])
                P.op('dve', lambda e, q=q, b=b, pq=pq: e.tensor_copy(xTt[b][:, q * 4:(q + 1) * 4, :], pq[:, 0:512].rearrange("p (a b) -> p a b", a=4)), [kp], ['xTt%d' % b])
                n4 += 1
            P.ld(xT_d[:, :, t * 128:(t + 1) * 128].rearrange("k p t -> p k t"), xTt[b][:], reads=['xTt%d' % b], writes=['xT_d'])
        P.barrier()
        ns = 0
        npm = 0
        for cb in range(18):
            g, part, hh = cb // 6, (cb % 6) // 2, cb % 2
            wb_ = wblk[cb % 2]; kw = 'w%d' % (cb % 2)
            P.ld(wb_[:], W['attn_w_in'].rearrange("(k p) f -> p k f", p=128)[:, :, cb * 512:(cb + 1) * 512], writes=[kw], eng='pool')
            tg0 = 4 if part == 0 else 0
            for tg in range(tg0, NTT // 4):
                xg_ = xTg[tg % 2]; kx = 'xTg%d' % (tg % 2)
                P.ld(xg_[:], xT_d[:, :, tg * 512:(tg + 1) * 512].rearrange("k p t -> p k t"), writes=[kx])
                for j in range(4):
                    pmm = pm[npm % 4]; kpm = 'pm%d' % (npm % 4); npm += 1
                    st_ = stg[ns % 4]; kst = 'stg%d' % (ns % 4); ns += 1
                    if part < 2:
                        h = hh * 4 + j
                        for k in range(16):
                            P.mm(pmm[:], wb_[:, k, j * 128:(j + 1) * 128], xg_[:, k, :], k == 0, k == 15, [kw, kx], [kpm])
                        if ns % 2:
                            P.op('act', lambda e, st_=st_, pmm=pmm: e.copy(st_[:], pmm[:]), [kpm], [kst])
                        else:
                            P.op('dve', lambda e, st_=st_, pmm=pmm: e.tensor_copy(st_[:], pmm[:]), [kpm], [kst])
                        if part == 0:
                            P.ld(qT_d[g, h, :, (tg - 4) * 512:(tg - 3) * 512], st_[:], reads=[kst], writes=['qT_d'])
                        else:
                            P.ld(kT_d[g, h, :, tg * 512:(tg + 1) * 512], st_[:], reads=[kst], writes=['kT_d'])
                    else:
                        for k in range(16):
                            P.mm(pmm[:], xg_[:, k, j * 128:(j + 1) * 128], wb_[:, k, :], k == 0, k == 15, [kw, kx], [kpm])
                        if ns % 2:
                            P.op('act', lambda e, st_=st_, pmm=pmm: e.copy(st_[:], pmm[:]), [kpm], [kst])
                        else:
                            P.op('dve', lambda e, st_=st_, pmm=pmm: e.tensor_copy(st_[:], pmm[:]), [kpm], [kst])
                        r0 = tg * 512 + j * 128
                        P.ld(v_d[g, hh * 4:(hh + 1) * 4, r0:r0 + 128, :].rearrange("h t d -> t h d"),
                             st_[:].rearrange("p (h d) -> p h d", h=4), reads=[kst], writes=['v_d'])
        P.barrier()
        P.flush()
    scale = 128.0 ** -0.5
    with ExitStack() as ph:
        def sb(shape, dt, name):
            return ph.enter_context(nc.sbuf_tensor(name, list(shape), dt))

        def ps(name, dt=F32, w=512):
            return ph.enter_context(nc.psum_tensor(name, [128, w], dt))
        kT = [sb([128, NTOK], BF16, 'a2_kT%d' % g) for g in range(3)]
        qT = [sb([128, NQ * 128], BF16, 'a2_qT%d' % g) for g in range(3)]
        vs = [sb([128, NTT, 129], BF16, 'a2_v%d' % g) for g in range(3)]
        Bt = sb([128, 24, 128], F32, 'a2_Bt')
        tmp = [sb([128, 512], F32, 'a2_tmp%d' % i) for i in range(2)]
        pT = [sb([128, 512], BF16, 'a2_pT%d' % i) for i in range(3)]
        rc = [sb([128, 1], F32, 'a2_rc%d' % i) for i in range(2)]
        hv = sb([128, 1], F32, 'a2_hv')
        pS = [ps('a2_pS%d' % i) for i in range(3)]
        pO = [ps('a2_pO%d' % i) for i in range(2)]
        P.ld(hv[:], hv_d, writes=['hv'])
        for g in range(3):
            P.op('pool', lambda e, g=g: e.memset(vs[g][:, :, 128:129], 1.0), [], ['vs%d' % g])
            P.op('dve', lambda e, g=g: e.tensor_scalar(vs[g][:, 0:16, 128:129], vs[g][:, 0:16, 128:129], hv[:, 0:1], None, ALU.mult), ['vs%d' % g, 'hv'], ['vs%d' % g])
        chunks = []
        bi = 0
        for g in range(3):
            nm = GROUPS[g][0] // 128 + 1
            for m0 in range(0, nm, 4):
                ms = list(range(m0, min(m0 + 4, nm)))
                chunks.append((g, ms, bi))
                bi += len(ms)
        nS = 0
        nO = 0
        for h in range(8):
            for g in range(3):
                P.ld(kT[g][:], kT_d[g, h], writes=['kT%d' % g])
                P.ld(qT[g][:], qT_d[g, h], writes=['qT%d' % g])
                P.ld(vs[g][:, :, 0:128], v_d[g, h].rearrange("(t p) d -> p t d", p=128), writes=['vs%d' % g])
            bi = 0
            for g in range(3):
                L = GROUPS[g][0] + 256
                for m in range(GROUPS[g][0] // 128 + 1):
                    tap = bass.AP(tensor=Zs[g][h], offset=m * 128 + 127, ap=[[L - 1, 128], [1, 128]])
                    P.ld(Bt[:, bi, :], tap, writes=['Bt'])
                    bi += 1
            for qt in range(NQ):
                T = 16 + qt
                io = nO % 2; nO += 1
                first = True
                for (g, ms, b0) in chunks:
                    i3 = nS % 3; nS += 1
                    n = len(ms)
                    for mi, m in enumerate(ms):
                        KT = T - m
                        P.mm(pS[i3][:, mi * 128:(mi + 1) * 128], kT[g][:, KT * 128:(KT + 1) * 128], qT[g][:, qt * 128:(qt + 1) * 128], True, True,
                             ['kT%d' % g, 'qT%d' % g], ['pS%d' % i3])
                    i2 = nS % 2
                    P.op('dve', lambda e, i3=i3, i2=i2, n=n, b0=b0: e.scalar_tensor_tensor(
                        tmp[i2][:, 0:n * 128], pS[i3][:, 0:n * 128], scale, Bt[:, b0:b0 + n, :].rearrange("p a b -> p (a b)"), ALU.mult, ALU.add),
                        ['pS%d' % i3, 'Bt'], ['tmp%d' % i2])
                    P.op('act', lambda e, i3=i3, i2=i2, n=n: e.activation(pT[i3][:, 0:n * 128], tmp[i2][:, 0:n * 128], AF.Exp), ['tmp%d' % i2], ['pT%d' % i3])
                    for mi, m in enumerate(ms):
                        KT = T - m
                        last = (g == 2 and m == 16)
                        P.mm(pO[io][:, 0:129], pT[i3][:, mi * 128:(mi + 1) * 128], vs[g][:, KT, :], first, last, ['pT%d' % i3, 'vs%d' % g], ['pO%d' % io])
                        first = False
                P.op('dve', lambda e, io=io: e.reciprocal(rc[io][:], pO[io][:, 128:129]), ['pO%d' % io], ['rc%d' % io])
                P.op('act', lambda e, io=io, qt=qt, h=h: e.activation(o_all[:, qt, h * 128:(h + 1) * 128], pO[io][:, 0:128], AF.Copy, scale=rc[io][:, 0:1]),
                     ['pO%d' % io, 'rc%d' % io], ['oall%d' % qt])
        P.barrier()
        P.flush()
    with ExitStack() as ph:
        def sb(shape, dt, name):
            return ph.enter_context(nc.sbuf_tensor(name, list(shape), dt))

        def ps(name, dt=F32, w=512):
            return ph.enter_context(nc.psum_tensor(name, [128, w], dt))
        wo = sb([128, 8, D], BF16, 'a3_wo')
        oT = [sb([128, 8, 128], BF16, 'a3_oT%d' % i) for i in range(2)]
        xt = [sb([128, D], F32, 'a3_xt%d' % i) for i in range(2)]
        ot = [sb([128, D], F32, 'a3_ot%d' % i) for i in range(2)]
        gt = sb([128, D], F32, 'a3_g'); bt = sb([128, D], F32, 'a3_b')
        st = sb([128, 24], F32, 'a3_st'); ag = sb([128, 2], F32, 'a3_ag'); rstd = sb([128, 1], F32, 'a3_rstd')
        ptr = [ps('a3_ptr%d' % i, BF16, 1024) for i in range(2)]
        py = [ps('a3_py%d' % i) for i in range(4)]
        P.ld(wo[:], W['attn_w_out'].rearrange("(k p) d -> p k d", p=128), writes=['wo'], eng='pool')
        P.ld(gt[:], W['ln_g'].partition_broadcast(128), writes=['lng'])
        P.ld(bt[:], W['ln_b'].partition_broadcast(128), writes=['lnb'])
        for qt in range(NQ):
            b = qt % 2
            P.ld(xt[b][:], xc[(16 + qt) * 128:(17 + qt) * 128, :], writes=['xt%d' % b])
            for q in range(2):
                pq = ptr[q]; kp = 'ptr%d' % q
                for j in range(4):
                    k = q * 4 + j
                    P.tr(pq[:, j * 128:(j + 1) * 128], o_all[:, qt, k * 128:(k + 1) * 128], identb[:], ['oall%d' % qt, 'a_identb'], [kp])
                P.op('dve', lambda e, q=q, b=b, pq=pq: e.tensor_copy(oT[b][:, q * 4:(q + 1) * 4, :], pq[:, 0:512].rearrange("p (a b) -> p a b", a=4)), [kp], ['oT%d' % b])
            for c in range(4):
                for k in range(8):
                    P.mm(py[c][:], oT[b][:, k, :], wo[:, k, c * 512:(c + 1) * 512], k == 0, k == 7, ['oT%d' % b, 'wo'], ['py%d' % c])
                P.op('dve', lambda e, b=b, c=c: e.scalar_tensor_tensor(xt[b][:, c * 512:(c + 1) * 512], xt[b][:, c * 512:(c + 1) * 512], ALPHA, py[c][:], ALU.mult, ALU.add),
                     ['py%d' % c, 'xt%d' % b], ['xt%d' % b])
            emit_ln(P, xt[b], ot[b], gt, bt, 'a3_', ['xt%d' % b], ['ot%d' % b], st, ag, rstd)
            P.ld(out_d[qt * 128:(qt + 1) * 128, :], ot[b][:], reads=['ot%d' % b])
        P.barrier()
        P.flush()


def build_attn(NQ):
    nc = bass.Bass("TRN2", target_bir_lowering=False)
    xc = nc.dram_tensor("xc", [(16 + NQ) * 128, D], F32, kind="ExternalInput").ap()
    hv_d = nc.dram_tensor("hv", [128, 1], F32, kind="ExternalInput").ap()
    out_d = nc.dram_tensor("out", [NQ * 128, D], F32, kind="ExternalOutput").ap()
    W = {}
    W['rel_bias'] = nc.dram_tensor("rel_bias", [32, 24], F32, kind="ExternalInput").ap()
    W['attn_w_in'] = nc.dram_tensor("attn_w_in", [D, 9216], F32, kind="ExternalInput").ap()
    W['attn_w_out'] = nc.dram_tensor("attn_w_out", [1024, D], F32, kind="ExternalInput").ap()
    W['ln_g'] = nc.dram_tensor("ln_g", [D], F32, kind="ExternalInput").ap()
    W['ln_b'] = nc.dram_tensor("ln_b", [D], F32, kind="ExternalInput").ap()
    with ExitStack() as es:
        P = Prog(nc, es)
        cst = load_consts(P, nc, ['c_ident'])
        emit_attn(P, nc, es, NQ, xc, hv_d, W, out_d, cst)
        P.finish()
    return nc


class RR:
    def __init__(self, items):
        self.items = items
        self.i = 0

    def get(self):
        it = self.items[self.i % len(self.items)]
        self.i += 1
        return it


def dn_consts_np():
    c = {}
    iu = np.triu(np.ones((128, 128), np.float32), 1)
    c['c_negu'] = (-1e4 * iu).astype(np.float32)
    c['c_negl'] = (-1e4 * iu.T).astype(np.float32)
    c['c_strl'] = iu.T.copy()
    return c


def emit_dn(P, nc, es, NB_, NTL, x_d, W, out_d, cst, dbg=False):
    ident, uti, ones = cst['c_ident'], cst['c_uti'], cst['c_ones']
    negu, negl, strl = cst['c_negu'], cst['c_negl'], cst['c_strl']
    scale = 128.0 ** -0.5

    def sb(shape, dt, name):
        return es.enter_context(nc.sbuf_tensor(name, list(shape), dt))

    identb = sb([128, 128], BF16, 'd_identb')
    P.op('dve', lambda e: e.tensor_copy(identb[:], ident[:]), ['c_ident'], ['d_identb'])
    wsl = sb([128, 16, 1544], BF16, 'd_wsl')
    P.ld(wsl[:], W['w_sl'].rearrange("(k p) f -> p k f", p=128), writes=['wsl'], eng='pool')
    cw = sb([128, 8, 4], F32, 'd_cw')
    P.ld(cw[:], W['cw_l'], writes=['cw'])
    ng = sb([128, 128], F32, 'd_ng')
    P.ld(ng[:], W['norm_g'].partition_broadcast(128), writes=['ng'])
    negA = sb([128, 4], F32, 'd_negA')
    dtb = sb([128, 4], F32, 'd_dtb')
    P.ld(negA[:], W['alog_l'].partition_broadcast(128), writes=['negA'])
    P.ld(dtb[:], W['dtb_l'].partition_broadcast(128), writes=['dtb'])
    P.op('act', lambda e: e.activation(negA[:], negA[:], AF.Exp), ['negA'], ['negA'])
    P.op('dve', lambda e: e.tensor_scalar(negA[:], negA[:], -1.0, None, ALU.mult), ['negA'], ['negA'])
    xt = [sb([128, D], F32, 'd_xt%d' % i) for i in range(2)]
    xb = [sb([128, D], BF16, 'd_xb%d' % i) for i in range(2)]
    xT = [sb([128, 16, 128], BF16, 'd_xT%d' % i) for i in range(2)]
    raw = [[sb([128, 131], F32, 'd_raw%d_%d' % (i, c)) for c in range(8)] for i in range(2)]
    qk = [[sb([128, 128], BF16, 'd_qk%d_%d' % (i, c)) for c in range(4)] for i in range(2)]
    vT = [[sb([128, 128], F32, 'd_vT%d_%d' % (i, c)) for c in range(4)] for i in range(2)]
    qf = [[sb([128, 128], F32, 'd_qf%d_%d' % (i, c)) for c in range(2)] for i in range(2)]
    zs = [sb([128, 512], F32, 'd_zs%d' % i) for i in range(2)]
    outt = [sb([128, 512], BF16, 'd_out%d' % i) for i in range(2)]
    sm4 = {n: [sb([128, 4], F32, 'd_%s%d' % (n, i)) for i in range(2)] for n in ('beta', 'nbeta', 'g', 'gc', 'ngc', 'xa')}
    ded = {n: [[sb([128, 128], F32 if n in ('Erow', 'vb') else BF16, 'd_%s%d_%d' % (n, i, j)) for j in range(4)] for i in range(2)]
           for n in ('Erow', 'TT', 'QKdT', 'kdec', 'vb', 'qtil')}
    c1 = [[sb([128, 1], F32, 'd_c1%d_%d' % (i, j)) for j in range(4)] for i in range(2)]
    growl = [[sb([128, 1], F32, 'd_gl%d_%d' % (i, j)) for j in range(4)] for i in range(2)]
    Sst = [sb([128, 128], F32, 'd_S%d' % j) for j in range(4)]
    Sbf = [sb([128, 128], BF16, 'd_Sbf%d' % j) for j in range(4)]
    tf = RR([(sb([128, 128], F32, 'd_tf%d' % i), 'tf%d' % i) for i in range(56)])
    tb = RR([(sb([128, 128], BF16, 'd_tb%d' % i), 'tb%d' % i) for i in range(8)])
    tn = RR([(sb([128, 128], BF16, 'd_tn%d' % i), 'tn%d' % i) for i in range(8)])
    tn4 = RR([(sb([128, 512], BF16, 'd_tn4_%d' % i), 'tn4_%d' % i) for i in range(14)])
    identb4 = sb([128, 512], BF16, 'd_identb4')
    for j4 in range(4):
        P.op('dve', lambda e, j4=j4: e.tensor_copy(identb4[:, j4 * 128:(j4 + 1) * 128], ident[:]), ['c_ident'], ['d_identb4'])
    TT4 = [sb([128, 512], BF16, 'd_TT4_%d' % i) for i in range(2)]
    identb2 = identb
    t1 = RR([(sb([128, 1], F32, 'd_t1%d' % i), 't1%d' % i) for i in range(16)])
    pxt = es.enter_context(nc.psum_tensor('d_pxt', [128, 1024], BF16))
    pkt = es.enter_context(nc.psum_tensor('d_pkt', [128, 1024], BF16))
    pz = es.enter_context(nc.psum_tensor('d_pz', [128, 512], F32))
    pm0b = es.enter_context(nc.psum_tensor('d_pm0b', [128, 1024], BF16))
    pbk = RR([(es.enter_context(nc.psum_tensor('d_pb%d' % i, [128, 512], F32)), 'pb%d' % i) for i in range(4)])

    def sl(t, j):
        return t[:, j * 128:(j + 1) * 128]

    tfP = RR([(sb([128, 128], F32, 'd_tfP%d' % i), 'tfP%d' % i) for i in range(24)])
    pbP = RR([pbk.items[3]])
    pbU = RR(pbk.items[0:3])

    def genP(b_, n):
        par = n % 2
        row0 = (b_ * NTL + n) * 128
        kx, kb_, kT_ = 'xt%d' % par, 'xb%d' % par, 'xT%d' % par
        P.ld(xt[par][:], x_d[row0:row0 + 128, :], writes=[kx])
        P.op('act', lambda e, par=par: e.copy(xb[par][:], xt[par][:]), [kx], [kb_])
        for q in range(4):
            half = q % 2
            for jj in range(4):
                k = q * 4 + jj
                P.tr(pxt[:, half * 512 + jj * 128:half * 512 + (jj + 1) * 128], xb[par][:, k * 128:(k + 1) * 128], identb[:], [kb_, 'd_identb'], ['pxt'])
            P.op('dve', lambda e, q=q, par=par, half=half: e.tensor_copy(xT[par][:, q * 4:(q + 1) * 4, :], pxt[:, half * 512:(half + 1) * 512].rearrange("p (a b) -> p a b", a=4)), [], ['pxt', kT_])
        yield
        ys = [None] * 4
        for grp in range(2):
            bk_, kbk = pbP.get()
            for ci in range(4):
                c = grp * 4 + ci
                for k in range(16):
                    P.mm(sl(bk_, ci), wsl[:, k, c * 128:(c + 1) * 128], xT[par][:, k, :], k == 0, k == 15, ['wsl', kT_], [kbk])
            for ci in range(4):
                c = grp * 4 + ci
                kr = 'raw%d_%d' % (par, c)
                P.op('act', lambda e, c=c, par=par, bk_=bk_, ci=ci: e.copy(raw[par][c][:, 3:131], sl(bk_, ci)), [], [kbk, kr])
                P.op('pool', lambda e, c=c, par=par: e.tensor_copy(raw[1 - par][c][:, 0:3], raw[par][c][:, 128:131]), [kr], ['raw%d_%d' % (1 - par, c)])
                acc, ka = tfP.get()
                P.op('dve', lambda e, c=c, par=par, acc=acc: e.tensor_scalar(acc[:], raw[par][c][:, 0:128], cw[:, c, 0:1], None, ALU.mult), [kr, 'cw'], [ka])
                for jt in range(1, 4):
                    P.op('dve', lambda e, c=c, par=par, acc=acc, jt=jt: e.scalar_tensor_tensor(acc[:], raw[par][c][:, jt:jt + 128], cw[:, c, jt:jt + 1], acc[:], ALU.mult, ALU.add), [kr, 'cw', ka], [ka])
                if c >= 4:
                    P.op('act', lambda e, c=c, par=par, acc=acc: e.activation(vT[par][c - 4][:], acc[:], AF.Silu), [ka], ['vT%d_%d' % (par, c - 4)])
                else:
                    y, ky = tfP.get()
                    P.op('act', lambda e, acc=acc, y=y: e.activation(y[:], acc[:], AF.Silu), [ka], [ky])
                    ys[c] = (y, ky)
                yield
        bk_, kbk = pbP.get()
        sqs = []
        for c in range(4):
            y, ky = ys[c]
            sq, ksq = tfP.get()
            P.op('pool', lambda e, y=y, sq=sq: e.tensor_tensor(sq[:], y[:], y[:], ALU.mult), [ky], [ksq])
            sqs.append((sq, ksq))
        for c in range(4):
            P.mm(sl(bk_, c), ones[:], sqs[c][0][:], True, True, ['c_ones', sqs[c][1]], [kbk])
        for c in range(4):
            y, ky = ys[c]
            rn, krn = tfP.get()
            P.op('dve', lambda e, rn=rn, bk_=bk_, c=c: e.tensor_scalar(rn[:], sl(bk_, c), 1e-6, None, ALU.add), [], [kbk, krn])
            P.op('act', lambda e, rn=rn: e.activation(rn[:], rn[:], AF.Sqrt), [krn], [krn])
            P.op('dve', lambda e, rn=rn: e.reciprocal(rn[:], rn[:]), [krn], [krn])
            P.op('dve', lambda e, rn=rn, y=y, c=c, par=par: e.tensor_tensor(qk[par][c][:], y[:], rn[:], ALU.mult), [krn, ky], ['qk%d_%d' % (par, c)])
            if c < 2:
                P.op('pool', lambda e, rn=rn, y=y, c=c, par=par: e.tensor_tensor(qf[par][c][:], y[:], rn[:], ALU.mult), [krn, ky], ['qf%d_%d' % (par, c)])
        yield
        for k in range(16):
            P.mm(pz[:], xT[par][:, k, :], wsl[:, k, 1024:1536], k == 0, k == 15, [kT_, 'wsl'], ['pz'])
        P.op('act', lambda e, par=par: e.activation(zs[par][:], pz[:], AF.Silu), [], ['pz', 'zs%d' % par])
        yield
        pba, kpba = pbP.get()
        for k in range(16):
            P.mm(pba[:, 0:8], xT[par][:, k, :], wsl[:, k, 1536:1544], k == 0, k == 15, [kT_, 'wsl'], [kpba])
        beta, nbeta, g_, gc, ngc, xa = (sm4[n_][par] for n_ in ('beta', 'nbeta', 'g', 'gc', 'ngc', 'xa'))
        ksm = {n_: 'sm_%s%d' % (n_, par) for n_ in sm4}
        P.op('act', lambda e, beta=beta, pba=pba: e.activation(beta[:], pba[:, 0:4], AF.Sigmoid), [], [kpba, ksm['beta']])
        P.op('dve', lambda e, beta=beta, nbeta=nbeta: e.tensor_scalar(nbeta[:], beta[:], -1.0, None, ALU.mult), [ksm['beta']], [ksm['nbeta']])
        P.op('dve', lambda e, xa=xa, pba=pba: e.tensor_tensor(xa[:], pba[:, 4:8], dtb[:], ALU.add), ['dtb'], [kpba, ksm['xa']])
        P.op('act', lambda e, xa=xa: e.activation(xa[:], xa[:], AF.Exp), [ksm['xa']], [ksm['xa']])
        P.op('dve', lambda e, xa=xa: e.tensor_scalar(xa[:], xa[:], 1.0, None, ALU.add), [ksm['xa']], [ksm['xa']])
        P.op('act', lambda e, xa=xa: e.activation(xa[:], xa[:], AF.Ln), [ksm['xa']], [ksm['xa']])
        P.op('dve', lambda e, xa=xa, g_=g_: e.tensor_tensor(g_[:], xa[:], negA[:], ALU.mult), [ksm['xa'], 'negA'], [ksm['g']])
        P.mm(pba[:, 128:132], uti[:], g_[:], True, True, ['c_uti', ksm['g']], [kpba])
        P.op('dve', lambda e, gc=gc, pba=pba: e.tensor_copy(gc[:], pba[:, 128:132]), [], [kpba, ksm['gc']])
        P.op('dve', lambda e, gc=gc, ngc=ngc: e.tensor_scalar(ngc[:], gc[:], -1.0, None, ALU.mult), [ksm['gc']], [ksm['ngc']])

    def stageUS(b_, n, step):
        par = n % 2
        row0 = (b_ * NTL + n) * 128
        beta, nbeta, g_, gc, ngc, xa = (sm4[n_][par] for n_ in ('beta', 'nbeta', 'g', 'gc', 'ngc', 'xa'))
        ksm = {n_: 'sm_%s%d' % (n_, par) for n_ in sm4}
        dkk = [{n_: 'ded_%s%d_%d' % (n_, par, j) for n_ in ded} for j in range(4)]
        kq_ = ['qk%d_%d' % (par, j // 2) for j in range(4)]
        kk_ = ['qk%d_%d' % (par, 2 + j // 2) for j in range(4)]
        qTs = [qk[par][j // 2] for j in range(4)]
        kTs = [qk[par][2 + j // 2] for j in range(4)]
        bgr, kbgr = pbU.get()
        for j in range(4):
            gbc, kgbc = tf.get()
            P.op('dve', lambda e, gbc=gbc, g_=g_, j=j: e.tensor_scalar(gbc[:], ones[:], g_[:, j:j + 1], None, ALU.mult), ['c_ones', ksm['g']], [kgbc])
            P.mm(sl(bgr, j), gbc[:], uti[:], True, True, [kgbc, 'c_uti'], [kbgr])
        tDs = []; tDTs = []
        for j in range(4):
            Erow = ded['Erow'][par][j]
            P.op('act', lambda e, bgr=bgr, Erow=Erow, j=j: e.activation(Erow[:], sl(bgr, j), AF.Exp), [], [kbgr, dkk[j]['Erow']])
        for j in range(4):
            gl_ = growl[par][j]
            P.op('dve', lambda e, bgr=bgr, gl_=gl_, j=j: e.tensor_copy(gl_[:], sl(bgr, j)[:, 127:128]), [], [kbgr, 'gl%d_%d' % (par, j)])
            tD, ktD = tf.get()
            P.op('dve', lambda e, bgr=bgr, tD=tD, j=j: e.scalar_tensor_tensor(tD[:], sl(bgr, j), -1.0, negu[:], ALU.mult, ALU.add), ['c_negu'], [kbgr, ktD])
            tDT, ktDT = tf.get()
            P.op('dve', lambda e, bgr=bgr, tDT=tDT, j=j: e.tensor_tensor(tDT[:], sl(bgr, j), negl[:], ALU.add), ['c_negl'], [kbgr, ktDT])
            tDs.append((tD, ktD)); tDTs.append((tDT, ktDT))
        Dss = []
        for j in range(4):
            tD, ktD = tDs[j]; tDT, ktDT = tDTs[j]
            P.op('act', lambda e, tD=tD, gc=gc, j=j: e.activation(tD[:], tD[:], AF.Exp, bias=gc[:, j:j + 1], scale=1.0), [ktD, ksm['gc']], [ktD])
            Ds, kDs = tf.get()
            P.op('pool', lambda e, Ds=Ds, tD=tD: e.tensor_tensor(Ds[:], tD[:], strl[:], ALU.mult), [ktD, 'c_strl'], [kDs])
            Dss.append((Ds, kDs))
            P.op('act', lambda e, tDT=tDT, ngc=ngc, j=j: e.activation(tDT[:], tDT[:], AF.Exp, bias=ngc[:, j:j + 1], scale=1.0), [ktDT, ksm['ngc']], [ktDT])
        step()
        bkk, kbkk = pbU.get()
        for j in range(4):
            P.mm(sl(bkk, j), kTs[j][:], kTs[j][:], True, True, [kk_[j]], [kbkk])
        B4, kB4 = tn4.get()
        for j in range(4):
            Ds, kDs = Dss[j]
            P.op('dve', lambda e, bkk=bkk, B4=B4, nbeta=nbeta, Ds=Ds, j=j: e.scalar_tensor_tensor(sl(B4, j), sl(bkk, j), nbeta[:, j:j + 1], Ds[:], ALU.mult, ALU.mult), [ksm['nbeta'], kDs], [kbkk, kB4])
        step()
        bm0, kbm0 = pm0b, 'pm0b'
        for j in range(4):
            P.tr(sl(bm0, j), sl(B4, j), identb[:], [kB4, 'd_identb'], [kbm0])
        M4, kM4 = tn4.get()
        P.op('dve', lambda e, M4=M4: e.tensor_copy(M4[:], pm0b[:, 0:512]), [], [kbm0, kM4])
        P4, kP4 = tn4.get()
        P.op('dve', lambda e, P4=P4: e.tensor_tensor(P4[:], pm0b[:, 0:512], identb4[:], ALU.add), ['d_identb4'], [kbm0, kP4])
        step()
        bkq, kbkq = pbU.get()
        for j in range(4):
            P.mm(sl(bkq, j), kTs[j][:], qTs[j][:], True, True, [kk_[j], kq_[j]], [kbkq])
        for j in range(4):
            QKdT = ded['QKdT'][par][j]
            tDT, ktDT = tDTs[j]
            P.op('dve', lambda e, bkq=bkq, QKdT=QKdT, tDT=tDT, j=j: e.scalar_tensor_tensor(QKdT[:], sl(bkq, j), scale, tDT[:], ALU.mult, ALU.mult), [ktDT], [kbkq, dkk[j]['QKdT']])
        step()
        for j in range(4):
            P.tr(pkt[:, j * 128:(j + 1) * 128], kTs[j][:], identb[:], [kk_[j], 'd_identb'], ['pkt'])
        for j in range(4):
            gl_ = growl[par][j]
            decf, kdf = t1.get()
            P.op('act', lambda e, decf=decf, gc=gc, gl_=gl_, j=j: e.activation(decf[:], gc[:, j:j + 1], AF.Exp, bias=gl_[:, 0:1], scale=-1.0), [ksm['gc'], 'gl%d_%d' % (par, j)], [kdf])
            kdec = ded['kdec'][par][j]
            P.op('dve', lambda e, kdec=kdec, decf=decf, j=j: e.tensor_scalar(kdec[:], pkt[:, j * 128:(j + 1) * 128], decf[:, 0:1], None, ALU.mult), [kdf], ['pkt', dkk[j]['kdec']])
        step()
        bvt, kbvt = pbU.get()
        for j in range(4):
            P.tr(sl(bvt, j), vT[par][j][:], ident[:], ['vT%d_%d' % (par, j), 'c_ident'], [kbvt])
        for j in range(4):
            vb = ded['vb'][par][j]
            P.op('dve', lambda e, bvt=bvt, vb=vb, beta=beta, j=j: e.tensor_scalar(vb[:], sl(bvt, j), beta[:, j:j + 1], None, ALU.mult), [ksm['beta']], [kbvt, dkk[j]['vb']])
            c1_ = c1[par][j]; kc1 = 'c1%d_%d' % (par, j)
            P.op('act', lambda e, c1_=c1_, gc=gc, j=j: e.activation(c1_[:], gc[:, j:j + 1], AF.Exp), [ksm['gc']], [kc1])
            P.op('dve', lambda e, c1_=c1_, nbeta=nbeta, j=j: e.tensor_tensor(c1_[:], c1_[:], nbeta[:, j:j + 1], ALU.mult), [kc1, ksm['nbeta']], [kc1])
            qtil = ded['qtil'][par][j]
            Erow = ded['Erow'][par][j]
            P.op('dve', lambda e, par=par, qtil=qtil, j=j, Erow=Erow: e.scalar_tensor_tensor(qtil[:], qf[par][j // 2][:], scale, Erow[:], ALU.mult, ALU.mult), ['qf%d_%d' % (par, j // 2), dkk[j]['Erow']], [dkk[j]['qtil']])
        step()
        for lv in range(1, 7):
            if lv < 6:
                bmn, kbmn = pbU.get()
                for j in range(4):
                    P.mm(sl(bmn, j), sl(B4, j), sl(M4, j), True, True, [kB4, kM4], [kbmn])
            bbn, kbbn = pbU.get()
            for j in range(4):
                P.mm(sl(bbn, j), sl(M4, j), sl(B4, j), True, True, [kB4, kM4], [kbbn])
            Bn4, kBn4 = tn4.get()
            P.op('act', lambda e, Bn4=Bn4, bbn=bbn: e.copy(Bn4[:], bbn[:]), [], [kbbn, kBn4])
            if lv < 6:
                Mn4, kMn4 = tn4.get()
                P.op('dve', lambda e, Mn4=Mn4, bmn=bmn: e.tensor_copy(Mn4[:], bmn[:]), [], [kbmn, kMn4])
            else:
                Mn4, kMn4 = None, None
            step()
            bpn, kbpn = pbU.get()
            for j in range(4):
                P.mm(sl(bpn, j), sl(Bn4, j), sl(P4, j), True, True, [kBn4, kP4], [kbpn])
            if lv < 6:
                Pn4, kPn4 = tn4.get()
            else:
                Pn4, kPn4 = TT4[par], 'TT4_%d' % par
            P.op('dve', lambda e, Pn4=Pn4, P4=P4, bpn=bpn: e.tensor_tensor(Pn4[:], bpn[:], P4[:], ALU.add), [kP4], [kbpn, kPn4])
            P4, kP4 = Pn4, kPn4
            B4, kB4 = Bn4, kBn4
            M4, kM4 = Mn4, kMn4
            step()
        step()
        bks, kbks = pbU.get()
        for j in range(4):
            P.mm(sl(bks, j), kTs[j][:], Sbf[j][:], True, True, [kk_[j], 'Sbf%d' % j], [kbks])
        rr = [tn.get() for j in range(4)]
        for j in range(4):
            P.op('dve', lambda e, par=par, j=j, r=rr[j][0], bks=bks: e.scalar_tensor_tensor(r[:], sl(bks, j), c1[par][j][:, 0:1], ded['vb'][par][j][:], ALU.mult, ALU.add),
                 ['c1%d_%d' % (par, j), dkk[j]['vb']], [kbks, rr[j][1]])
        step()
        bvn, kbvn = pbU.get()
        for j in range(4):
            P.mm(sl(bvn, j), sl(TT4[par], j), rr[j][0][:], True, True, ['TT4_%d' % par, rr[j][1]], [kbvn])
        vn4, kvn4 = tn4.get()
        P.op('act', lambda e, vn4=vn4, bvn=bvn: e.copy(vn4[:], bvn[:]), [], [kbvn, kvn4])
        vn = [(sl(vn4, j), kvn4) for j in range(4)]
        step()
        bo, kbo = pbU.get()
        bst, kbst = pbU.get()
        for j in range(4):
            P.mm(sl(bo, j), ded['qtil'][par][j][:], Sbf[j][:], True, False, [dkk[j]['qtil'], 'Sbf%d' % j], [kbo])
            P.mm(sl(bo, j), ded['QKdT'][par][j][:], vn[j][0], False, True, [dkk[j]['QKdT'], vn[j][1]], [kbo])
            P.mm(sl(bst, j), ded['kdec'][par][j][:], vn[j][0], True, True, [dkk[j]['kdec'], vn[j][1]], [kbst])
        for j in range(4):
            P.op('dve', lambda e, par=par, j=j, bst=bst: e.scalar_tensor_tensor(Sst[j][:], Sst[j][:], ded['Erow'][par][j][:, 127:128], sl(bst, j), ALU.mult, ALU.add),
                 [dkk[j]['Erow']], [kbst, 'S%d' % j])
            P.op('act', lambda e, j=j: e.copy(Sbf[j][:], Sst[j][:]), ['S%d' % j], ['Sbf%d' % j])
        step()
        Oss = []
        for j in range(4):
            Os, kOs = tf.get()
            P.op('dve', lambda e, Os=Os, j=j, bo=bo: e.tensor_copy(Os[:], sl(bo, j)), [], [kbo, kOs])
            Oss.append((Os, kOs))
        for j in range(4):
            Os, kOs = Oss[j]
            ssq, kss = t1.get()
            junk, kj = tf.get()
            P.op('pool', lambda e, ssq=ssq: e.memset(ssq[:], 0.0), [], [kss])
            P.op('act', lambda e, junk=junk, Os=Os, ssq=ssq: e.activation(junk[:], Os[:], AF.Square, accum_out=ssq[:]), [kOs], [kj, kss])
            P.op('dve', lambda e, ssq=ssq: e.tensor_scalar(ssq[:], ssq[:], 1.0 / 128, 1e-6, ALU.mult, ALU.add), [kss], [kss])
            P.op('act', lambda e, ssq=ssq: e.activation(ssq[:], ssq[:], AF.Sqrt), [kss], [kss])
            P.op('dve', lambda e, ssq=ssq: e.reciprocal(ssq[:], ssq[:]), [kss], [kss])
            P.op('dve', lambda e, Os=Os, ssq=ssq: e.scalar_tensor_tensor(Os[:], Os[:], ssq[:, 0:1], ng[:], ALU.mult, ALU.mult), [kss, 'ng'], [kOs])
            P.op('pool', lambda e, Os=Os, j=j, par=par: e.tensor_tensor(outt[par][:, j * 128:(j + 1) * 128], Os[:], zs[par][:, j * 128:(j + 1) * 128], ALU.mult),
                 [kOs, 'zs%d' % par], ['out%d' % par])
        P.ld(out_d[row0:row0 + 128, :], outt[par][:], reads=['out%d' % par])
        if dbg and n == 0 and b_ == 0:
            def dump(name, ap, key, shape, dt=F32):
                dd = nc.dram_tensor(name, shape, dt, kind="ExternalOutput").ap()
                P.ld(dd, ap, reads=[key])
            for c in range(4):
                dump('g_qk%d' % c, qk[par][c][:], 'qk%d_%d' % (par, c), [128, 128], BF16)
                dump('g_vT%d' % c, vT[par][c][:], 'vT%d_%d' % (par, c), [128, 128])
                dump('g_raw%d' % c, raw[par][c][:], 'raw%d_%d' % (par, c), [128, 131])
            for n_ in ('beta', 'g', 'gc'):
                dump('g_' + n_, sm4[n_][par][:], ksm[n_], [128, 4])
            for n_ in ded:
                dump('g_' + n_, ded[n_][par][0][:], dkk[0][n_], [128, 128], F32 if n_ in ('Erow', 'vb') else BF16)
            dump('g_zs', zs[par][:], 'zs%d' % par, [128, 512])
            dump('g_c1', c1[par][0][:], 'c1%d_0' % par, [128, 1])
            dump('g_r', rr[0][0][:], rr[0][1], [128, 128], BF16)
            dump('g_vn', vn[0][0], vn[0][1], [128, 128], BF16)
            dump('g_Os', Oss[0][0][:], Oss[0][1], [128, 128])
            dump('g_out', outt[par][:], 'out%d' % par, [128, 512], BF16)


    for b_ in range(NB_):
        for j in range(4):
            P.op('pool', lambda e, j=j: e.memset(Sst[j][:], 0.0), [], ['S%d' % j])
            P.op('pool', lambda e, j=j: e.memset(Sbf[j][:], 0.0), [], ['Sbf%d' % j])
        for c in range(8):
            P.op('pool', lambda e, c=c: e.memset(raw[0][c][:, 0:3], 0.0), [], ['raw0_%d' % c])
        for _ in genP(b_, 0):
            pass
        for n in range(NTL):
            gen = genP(b_, n + 1) if n + 1 < NTL else None

            def step(gen=gen):
                if gen is not None:
                    next(gen, None)
            stageUS(b_, n, step)
            if gen is not None:
                for _ in gen:
                    pass

def build_dn(NB_, NTL, dbg=False):
    nc = bass.Bass("TRN2", target_bir_lowering=False)
    x_d = nc.dram_tensor("x1", [NB_ * NTL * 128, D], F32, kind="ExternalInput").ap()
    out_d = nc.dram_tensor("out", [NB_ * NTL * 128, 512], BF16, kind="ExternalOutput").ap()
    W = {}
    W['w_sl'] = nc.dram_tensor("w_sl", [D, 1544], F32, kind="ExternalInput").ap()
    W['cw_l'] = nc.dram_tensor("cw_l", [128, 8, 4], F32, kind="ExternalInput").ap()
    W['norm_g'] = nc.dram_tensor("norm_g", [128], F32, kind="ExternalInput").ap()
    W['alog_l'] = nc.dram_tensor("alog_l", [4], F32, kind="ExternalInput").ap()
    W['dtb_l'] = nc.dram_tensor("dtb_l", [4], F32, kind="ExternalInput").ap()
    with ExitStack() as es:
        P = Prog(nc, es)
        cst = load_consts(P, nc, ['c_ident', 'c_uti', 'c_ones', 'c_negu', 'c_negl', 'c_strl'])
        emit_dn(P, nc, es, NB_, NTL, x_d, W, out_d, cst, dbg)
        P.finish()
    return nc


def dn_inputs(core, dn_w_in, dn_conv_w, dn_a_log, dn_dt_bias, dn_norm_g):
    c = core
    qc = slice(2 * c * 128, (2 * c + 2) * 128)
    cols = [dn_w_in[:, qc], dn_w_in[:, 2048 + 2 * c * 128:2048 + (2 * c + 2) * 128],
            dn_w_in[:, 4096 + 4 * c * 128:4096 + (4 * c + 4) * 128],
            dn_w_in[:, 8192 + 4 * c * 128:8192 + (4 * c + 4) * 128],
            dn_w_in[:, 12288 + 4 * c:12288 + 4 * c + 4], dn_w_in[:, 12320 + 4 * c:12320 + 4 * c + 4]]
    w_sl = np.ascontiguousarray(np.concatenate(cols, axis=1))
    cwc = np.concatenate([dn_conv_w[:, qc], dn_conv_w[:, 2048 + 2 * c * 128:2048 + (2 * c + 2) * 128],
                          dn_conv_w[:, 4096 + 4 * c * 128:4096 + (4 * c + 4) * 128]], axis=1)
    cw_l = np.ascontiguousarray(cwc.reshape(4, 8, 128).transpose(2, 1, 0))
    return {'w_sl': w_sl, 'cw_l': cw_l, 'norm_g': np.ascontiguousarray(dn_norm_g),
            'alog_l': np.ascontiguousarray(dn_a_log[4 * c:4 * c + 4]), 'dtb_l': np.ascontiguousarray(dn_dt_bias[4 * c:4 * c + 4])}


def emit_outproj(P, nc, NT, o_d, x_d, w_out, ln_g, ln_b, xm_d, cst):
    ident = cst['c_ident']
    r_d = nc.dram_tensor("op_r", [NT * 128, D], F32, kind="Internal").ap()
    with ExitStack() as ph:
        def sb(shape, dt, name):
            return ph.enter_context(nc.sbuf_tensor(name, list(shape), dt))

        def ps(name, dt=F32, w=512):
            return ph.enter_context(nc.psum_tensor(name, [128, w], dt))
        identb = sb([128, 128], BF16, 'op_identb')
        P.op('dve', lambda e: e.tensor_copy(identb[:], ident[:]), ['c_ident'], ['op_identb'])
        wo = sb([128, 32, 1024], BF16, 'op_wo')
        ob = [sb([128, 4096], BF16, 'op_ob%d' % i) for i in range(2)]
        oT = [sb([128, 32, 128], BF16, 'op_oT%d' % i) for i in range(2)]
        xh = [sb([128, 1024], F32, 'op_xh%d' % i) for i in range(2)]
        ptr = [ps('op_ptr%d' % i, BF16, 1024) for i in range(2)]
        py = [ps('op_py%d' % i) for i in range(4)]
        n4 = 0
        npy = 0
        for ch in range(2):
            P.ld(wo[:], w_out.rearrange("(k p) d -> p k d", p=128)[:, :, ch * 1024:(ch + 1) * 1024], writes=['wo'], eng='pool')
            for t in range(NT):
                b = t % 2
                P.ld(ob[b][:], o_d[t * 128:(t + 1) * 128, :], writes=['ob%d' % b])
                P.ld(xh[b][:], x_d[t * 128:(t + 1) * 128, ch * 1024:(ch + 1) * 1024], writes=['xh%d' % b])
                for q in range(8):
                    pq = ptr[n4 % 2]; kp = 'ptr%d' % (n4 % 2); n4 += 1
                    for j in range(4):
                        k = q * 4 + j
                        P.tr(pq[:, j * 128:(j + 1) * 128], ob[b][:, k * 128:(k + 1) * 128], identb[:], ['ob%d' % b, 'op_identb'], [kp])
                    if q % 2:
                        P.op('act', lambda e, q=q, b=b, pq=pq: e.copy(oT[b][:, q * 4:(q + 1) * 4, :], pq[:, 0:512].rearrange("p (a c) -> p a c", a=4)), [kp], ['oT%d' % b])
                    else:
                        P.op('dve', lambda e, q=q, b=b, pq=pq: e.tensor_copy(oT[b][:, q * 4:(q + 1) * 4, :], pq[:, 0:512].rearrange("p (a c) -> p a c", a=4)), [kp], ['oT%d' % b])
                for cc in range(2):
                    pyy = py[npy % 4]; kpy = 'py%d' % (npy % 4); npy += 1
                    for k in range(32):
                        P.mm(pyy[:], oT[b][:, k, :], wo[:, k, cc * 512:(cc + 1) * 512], k == 0, k == 31, ['oT%d' % b, 'wo'], [kpy])
                    P.op('dve', lambda e, b=b, cc=cc, pyy=pyy: e.scalar_tensor_tensor(xh[b][:, cc * 512:(cc + 1) * 512], xh[b][:, cc * 512:(cc + 1) * 512], ALPHA, pyy[:], ALU.mult, ALU.add),
                         [kpy, 'xh%d' % b], ['xh%d' % b])
                P.ld(r_d[t * 128:(t + 1) * 128, ch * 1024:(ch + 1) * 1024], xh[b][:], reads=['xh%d' % b], writes=['r_d'])
        P.barrier()
        P.flush()
    with ExitStack() as ph:
        def sb(shape, dt, name):
            return ph.enter_context(nc.sbuf_tensor(name, list(shape), dt))
        xt = [sb([128, D], F32, 'op2_xt%d' % i) for i in range(2)]
        ot = [sb([128, D], F32, 'op2_ot%d' % i) for i in range(2)]
        gt = sb([128, D], F32, 'op2_g'); bt = sb([128, D], F32, 'op2_b')
        st = sb([128, 24], F32, 'op2_st'); ag = sb([128, 2], F32, 'op2_ag'); rstd = sb([128, 1], F32, 'op2_rstd')
        P.ld(gt[:], ln_g.partition_broadcast(128), writes=['lng'])
        P.ld(bt[:], ln_b.partition_broadcast(128), writes=['lnb'])
        for t in range(NT):
            b = t % 2
            P.ld(xt[b][:], r_d[t * 128:(t + 1) * 128, :], writes=['xt%d' % b])
            emit_ln(P, xt[b], ot[b], gt, bt, 'op2_', ['xt%d' % b], ['ot%d' % b], st, ag, rstd)
            P.ld(xm_d[t * 128:(t + 1) * 128, :], ot[b][:], reads=['ot%d' % b], writes=['xm_d'])
        P.barrier()
        P.flush()


def _moe_W(nc):
    W = {}
    W['router_w'] = nc.dram_tensor("router_w", [D, NE], F32, kind="ExternalInput").ap()
    W['router_b'] = nc.dram_tensor("router_b", [NE], F32, kind="ExternalInput").ap()
    W['w_gate_up'] = nc.dram_tensor("w_gate_up", [NE, D, 2 * FF], F32, kind="ExternalInput").ap()
    W['bgu_l'] = nc.dram_tensor("bgu_l", [128, NE * 32], F32, kind="ExternalInput").ap()
    W['w_down'] = nc.dram_tensor("w_down", [NE, FF, D], F32, kind="ExternalInput").ap()
    W['b_down'] = nc.dram_tensor("b_down", [NE, D], F32, kind="ExternalInput").ap()
    W['ln_g'] = nc.dram_tensor("ln2_g", [D], F32, kind="ExternalInput").ap()
    W['ln_b'] = nc.dram_tensor("ln2_b", [D], F32, kind="ExternalInput").ap()
    return W


def build_L1(NQ, C):
    nc = bass.Bass("TRN2", target_bir_lowering=False)
    xc = nc.dram_tensor("xc", [(16 + NQ) * 128, D], F32, kind="ExternalInput").ap()
    hv_d = nc.dram_tensor("hv", [128, 1], F32, kind="ExternalInput").ap()
    out_d = nc.dram_tensor("out", [NQ * 128, D], F32, kind="ExternalOutput").ap()
    xm_d = nc.dram_tensor("xmid", [NQ * 128, D], F32, kind="Internal").ap()
    WA = {}
    WA['rel_bias'] = nc.dram_tensor("rel_bias", [32, 24], F32, kind="ExternalInput").ap()
    WA['attn_w_in'] = nc.dram_tensor("attn_w_in", [D, 9216], F32, kind="ExternalInput").ap()
    WA['attn_w_out'] = nc.dram_tensor("attn_w_out", [1024, D], F32, kind="ExternalInput").ap()
    WA['ln_g'] = nc.dram_tensor("ln1_g", [D], F32, kind="ExternalInput").ap()
    WA['ln_b'] = nc.dram_tensor("ln1_b", [D], F32, kind="ExternalInput").ap()
    WM = _moe_W(nc)
    with ExitStack() as es:
        P = Prog(nc, es)
        cst = load_consts(P, nc, ['c_ident', 'c_uts', 'c_ones'])
        with ExitStack() as aes:
            emit_attn(P, nc, aes, NQ, xc, hv_d, WA, xm_d, cst)
        emit_moe(P, nc, es, NQ, C, xm_d, out_d, WM, cst)
        P.finish()
    return nc


def build_L3(NT, C):
    nc = bass.Bass("TRN2", target_bir_lowering=False)
    o_d = nc.dram_tensor("o_in", [NT * 128, 4096], BF16, kind="ExternalInput").ap()
    x_d = nc.dram_tensor("x_res", [NT * 128, D], F32, kind="ExternalInput").ap()
    out_d = nc.dram_tensor("out", [NT * 128, D], F32, kind="ExternalOutput").ap()
    xm_d = nc.dram_tensor("xmid", [NT * 128, D], F32, kind="Internal").ap()
    w_out = nc.dram_tensor("dn_w_out", [4096, D], F32, kind="ExternalInput").ap()
    ln_g = nc.dram_tensor("ln1_g", [D], F32, kind="ExternalInput").ap()
    ln_b = nc.dram_tensor("ln1_b", [D], F32, kind="ExternalInput").ap()
    WM = _moe_W(nc)
    with ExitStack() as es:
        P = Prog(nc, es)
        cst = load_consts(P, nc, ['c_ident', 'c_uts', 'c_ones'])
        emit_outproj(P, nc, NT, o_d, x_d, w_out, ln_g, ln_b, xm_d, cst)
        emit_moe(P, nc, es, NT, C, xm_d, out_d, WM, cst)
        P.finish()
    return nc


def _moe_in(layer, router_w, router_b, w_gate_up, b_gate_up, w_down, b_down, ln2_g, ln2_b):
    bgu_l = np.ascontiguousarray(b_gate_up[layer].reshape(NE, 32, 128).transpose(2, 0, 1).reshape(128, NE * 32))
    return {'router_w': router_w[layer], 'router_b': router_b[layer], 'w_gate_up': w_gate_up[layer], 'bgu_l': bgu_l,
            'w_down': w_down[layer], 'b_down': b_down[layer], 'ln2_g': ln2_g[layer], 'ln2_b': ln2_b[layer]}


NCORES = 8
SEQ = 16384
TPC = 4096
CAP = 640


def kernel(x, rel_bias, attn_w_in, attn_w_out, dn_w_in, dn_conv_w, dn_a_log, dn_dt_bias,
           dn_norm_g, dn_w_out, ln1_g, ln1_b, router_w, router_b, w_gate_up, b_gate_up,
           w_down, b_down, ln2_g, ln2_b):
    f32 = np.float32
    args = [x, rel_bias, attn_w_in, attn_w_out, dn_w_in, dn_conv_w, dn_a_log, dn_dt_bias, dn_norm_g, dn_w_out,
            ln1_g, ln1_b, router_w, router_b, w_gate_up, b_gate_up, w_down, b_down, ln2_g, ln2_b]
    (x, rel_bias, attn_w_in, attn_w_out, dn_w_in, dn_conv_w, dn_a_log, dn_dt_bias, dn_norm_g, dn_w_out,
     ln1_g, ln1_b, router_w, router_b, w_gate_up, b_gate_up, w_down, b_down, ln2_g, ln2_b) = [np.asarray(a, dtype=f32) for a in args]
    cst = consts_np()
    cst.update(dn_consts_np())
    NQ = TPC // 128
    cores = list(range(NCORES))
    c3 = {k: cst[k] for k in ('c_ident', 'c_uts', 'c_ones')}
    com = dict(c3, **attn_static(), rel_bias=rel_bias, attn_w_in=attn_w_in[0], attn_w_out=attn_w_out[0],
               ln1_g=ln1_g[0], ln1_b=ln1_b[0],
               **_moe_in(0, router_w, router_b, w_gate_up, b_gate_up, w_down, b_down, ln2_g, ln2_b))
    ims = []
    for c in cores:
        b, sg = c // 4, c % 4
        t0 = sg * TPC
        if sg == 0:
            xc = np.concatenate([np.zeros((HALO, D), f32), x[b, 0:TPC]], axis=0)
            hv = np.zeros((128, 1), f32)
        else:
            xc = np.ascontiguousarray(x[b, t0 - HALO:t0 + TPC])
            hv = np.ones((128, 1), f32)
        ims.append(dict(com, xc=xc, hv=hv))
    nc1 = build_L1(NQ, CAP)
    r1 = run_bass_kernel_spmd(nc1, ims, core_ids=cores)
    x1 = np.concatenate([np.asarray(r1.results[c]['out']) for c in cores], axis=0)
    del ims, r1
    c6 = {k: cst[k] for k in ('c_ident', 'c_uti', 'c_ones', 'c_negu', 'c_negl', 'c_strl')}
    ims = [dict(c6, x1=x1, **dn_inputs(c, dn_w_in[0], dn_conv_w[0], dn_a_log[0], dn_dt_bias[0], dn_norm_g[0])) for c in cores]
    nc2 = build_dn(2, SEQ // 128)
    r2 = run_bass_kernel_spmd(nc2, ims, core_ids=cores)
    o_full = np.concatenate([np.asarray(r2.results[c]['out']) for c in cores], axis=1)
    del ims, r2
    com = dict(c3, dn_w_out=dn_w_out[0], ln1_g=ln1_g[1], ln1_b=ln1_b[1],
               **_moe_in(1, router_w, router_b, w_gate_up, b_gate_up, w_down, b_down, ln2_g, ln2_b))
    ims = [dict(com, o_in=np.ascontiguousarray(o_full[c * TPC:(c + 1) * TPC]), x_res=np.ascontiguousarray(x1[c * TPC:(c + 1) * TPC])) for c in cores]
    nc3 = build_L3(NQ, CAP)
    r3 = run_bass_kernel_spmd(nc3, ims, core_ids=cores)
    out = np.concatenate([np.asarray(r3.results[c]['out']) for c in cores], axis=0)
    return out.reshape(2, SEQ, D).astype(f32)
```

```python
import numpy as np
import concourse.bass as bass
import concourse.mybir as mybir
from contextlib import ExitStack
from concourse.bass_utils import run_bass_kernel_spmd

F32 = mybir.dt.float32
BF16 = mybir.dt.bfloat16
I32 = mybir.dt.int32
ALU = mybir.AluOpType
AF = mybir.ActivationFunctionType
AX = mybir.AxisListType


class Prog:
    ENG = ('pe', 'dve', 'act', 'pool', 'sp')
    EPOCH = 20000
    ND = 40
    NDI = 4

    def __init__(self, nc, es):
        self.nc, self.es = nc, es
        self.q = {e: [] for e in self.ENG}
        self.cnt = {e: 0 for e in self.ENG}
        self.epoch = {e: 0 for e in self.ENG}
        self.sems = {}
        self.seen = {e: {} for e in self.ENG}
        self.lastw = {}
        self.reads = {}
        self.dcnt = [0] * (self.ND + self.NDI)
        self.dnext = 0
        self.inext = 0
        self.nsem = 0
        self.ntile = 0

    def sem(self, key):
        if key not in self.sems:
            self.nsem += 1
            self.sems[key] = self.es.enter_context(self.nc.semaphore('s%d' % self.nsem))
        return self.sems[key]

    def sb(self, shape, dt, name=None):
        self.ntile += 1
        return self.es.enter_context(self.nc.sbuf_tensor(name or 't%d' % self.ntile, list(shape), dt))

    def ps(self, shape, dt, name=None):
        self.ntile += 1
        return self.es.enter_context(self.nc.psum_tensor(name or 'p%d' % self.ntile, list(shape), dt))

    def _wait(self, eng, tok):
        key, val = tok
        if eng == 'pe' and key[0] == 'e' and key[1] == 'pe':
            return
        if self.seen[eng].get(key, 0) >= val:
            return
        self.seen[eng][key] = val
        s = self.sem(key)
        self.q[eng].append(lambda e, s=s, val=val: e.wait_ge(s, val))

    def _deps(self, eng, reads, writes):
        for k in reads:
            t = self.lastw.get(k)
            if t:
                self._wait(eng, t)
        for k in writes:
            t = self.lastw.get(k)
            if t:
                self._wait(eng, t)
            for t in self.reads.get(k, ()):
                self._wait(eng, t)

    def _commit(self, tok, reads, writes):
        for k in reads:
            lst = self.reads.setdefault(k, [])
            for i, t in enumerate(lst):
                if t[0] == tok[0]:
                    lst[i] = tok
                    break
            else:
                lst.append(tok)
        for k in writes:
            self.lastw[k] = tok
            self.reads[k] = []

    def op(self, eng, fn, reads=(), writes=()):
        self._deps(eng, reads, writes)
        self.cnt[eng] += 1
        if self.cnt[eng] > self.EPOCH:
            self.epoch[eng] += 1
            self.cnt[eng] = 1
        key = ('e', eng, self.epoch[eng])
        s = self.sem(key)
        self.q[eng].append(lambda e, s=s, fn=fn: fn(e).then_inc(s, 1))
        self._commit((key, self.cnt[eng]), reads, writes)

    def dma(self, eng, fn, reads=(), writes=(), ind=False):
        if ind:
            i = self.ND + self.inext
            self.inext = (self.inext + 1) % self.NDI
        else:
            i = self.dnext
            self.dnext = (i + 1) % self.ND
        key = ('d', i)
        if self.dcnt[i]:
            self._wait(eng, (key, self.dcnt[i]))
        self._deps(eng, reads, writes)
        self.dcnt[i] += 16
        s = self.sem(key)
        self.q[eng].append(lambda e, s=s, fn=fn: fn(e).then_inc(s, 16))
        self._commit((key, self.dcnt[i]), reads, writes)

    def finish(self):
        for i in range(self.ND + self.NDI):
            if self.dcnt[i]:
                self._wait('sp', (('d', i), self.dcnt[i]))
        for e in ('pe', 'dve', 'act', 'pool'):
            if self.cnt[e]:
                self._wait('sp', (('e', e, self.epoch[e]), self.cnt[e]))
        self.flush()

    def flush(self):
        q = self.q
        self.q = {e: [] for e in self.ENG}
        with self.nc.Block() as block:
            @block.tensor
            def _(e):
                for f in q['pe']:
                    f(e)

            @block.vector
            def _(e):
                for f in q['dve']:
                    f(e)

            @block.scalar
            def _(e):
                for f in q['act']:
                    f(e)

            @block.gpsimd
            def _(e):
                for f in q['pool']:
                    f(e)

            @block.sync
            def _(e):
                for f in q['sp']:
                    f(e)

    def mm(self, out, lhsT, rhs, start=True, stop=True, reads=(), writes=()):
        self.op('pe', lambda e: e.matmul(out, lhsT, rhs, start=start, stop=stop), reads, writes)

    def tr(self, out, in_, ident, reads=(), writes=()):
        self.op('pe', lambda e: e.transpose(out, in_, ident), reads, writes)

    def ld(self, out, in_, reads=(), writes=(), eng='sp'):
        self.dma(eng, lambda e: e.dma_start(out=out, in_=in_), reads, writes)

    def barrier(self):
        toks = []
        for i in range(self.ND + self.NDI):
            if self.dcnt[i]:
                toks.append((('d', i), self.dcnt[i]))
        for e in ('pe', 'dve', 'act', 'pool'):
            for ep in range(self.epoch[e] + 1):
                c = self.cnt[e] if ep == self.epoch[e] else self.EPOCH
                if c:
                    toks.append((('e', e, ep), c))
        for eng in self.ENG:
            for t in toks:
                self._wait(eng, t)
        self.lastw = {}
        self.reads = {}


D = 2048
ALPHA = 2.0 ** 0.5
LN_EPS = 1e-5
NE = 32
FF = 2048


def consts_np():
    c = {}
    c['c_ident'] = np.eye(128, dtype=np.float32)
    c['c_uts'] = np.triu(np.ones((128, 128), np.float32), 1)
    c['c_uti'] = np.triu(np.ones((128, 128), np.float32), 0)
    c['c_ones'] = np.ones((128, 128), np.float32)
    return c


def load_consts(P, nc, names):
    out = {}
    for n in names:
        d = nc.dram_tensor(n, [128, 128], F32, kind="ExternalInput").ap()
        t = P.sb([128, 128], F32, name='sb_' + n)
        P.ld(t[:], d, writes=[n])
        out[n] = t
    return out


def emit_ln(P, src, dst, gt, bt, tmpk, keys_r, keys_w, st, ag, rstd):
    for i in range(4):
        P.op('dve', lambda e, i=i: e.bn_stats(st[:, i * 6:(i + 1) * 6], src[:, i * 512:(i + 1) * 512]), keys_r, [tmpk + 'st'])
    P.op('dve', lambda e: e.bn_aggr(ag[:], st[:]), [tmpk + 'st'], [tmpk + 'ag'])
    P.op('dve', lambda e: e.tensor_scalar(rstd[:], ag[:, 1:2], LN_EPS, None, ALU.add), [tmpk + 'ag'], [tmpk + 'rs'])
    P.op('act', lambda e: e.activation(rstd[:], rstd[:], AF.Sqrt), [tmpk + 'rs'], [tmpk + 'rs'])
    P.op('dve', lambda e: e.reciprocal(rstd[:], rstd[:]), [tmpk + 'rs'], [tmpk + 'rs'])
    P.op('dve', lambda e: e.tensor_scalar(dst[:], src[:], ag[:, 0:1], rstd[:, 0:1], ALU.subtract, ALU.mult),
         list(keys_r) + [tmpk + 'ag', tmpk + 'rs'], keys_w)
    P.op('pool', lambda e: e.tensor_tensor(dst[:], dst[:], gt[:], ALU.mult), list(keys_w) + ['lng'], keys_w)
    P.op('pool', lambda e: e.tensor_tensor(dst[:], dst[:], bt[:], ALU.add), list(keys_w) + ['lnb'], keys_w)


def emit_moe(P, nc, es, NT, C, x_in, out_d, W, cst, dbg=False):
    NSLOT = NE * C
    NB = C // 128
    H = C // 2
    BIG = float(NSLOT + 4096)
    kind = "ExternalOutput" if dbg else "Internal"
    xg = nc.dram_tensor("moe_xg", [NSLOT + 1, D], BF16, kind=kind).ap()
    yd = nc.dram_tensor("moe_y", [NSLOT + 1, D], F32, kind=kind).ap()
    if dbg:
        d_sl = nc.dram_tensor("dbg_slots", [128, NT * 4], I32, kind="ExternalOutput").ap()
        d_ga = nc.dram_tensor("dbg_gates", [128, NT * 4], F32, kind="ExternalOutput").ap()
    ident, uts, ones = cst['c_ident'], cst['c_uts'], cst['c_ones']
    slots_all = P.sb([128, NT, 4], I32, name='slots_all')
    gates_all = P.sb([128, NT, 4], F32, name='gates_all')
    identb = P.sb([128, 128], BF16, name='identb')
    P.op('dve', lambda e: e.tensor_copy(identb[:], ident[:]), ['c_ident'], ['identb'])
    scat_keys = []
    with ExitStack() as ph:
        def sb(shape, dt, name):
            return ph.enter_context(nc.sbuf_tensor(name, list(shape), dt))

        def ps(name):
            return ph.enter_context(nc.psum_tensor(name, [128, 512], F32))
        xt = [sb([128, D], F32, 'r_xt%d' % i) for i in range(2)]
        xb = [sb([128, D], BF16, 'r_xb%d' % i) for i in range(2)]
        xT = sb([128, 16, 128], F32, 'r_xT')
        rw = sb([128, 16, NE], F32, 'r_rw')
        rb = sb([128, NE], F32, 'r_rb')
        eoff = sb([128, NE], F32, 'r_eoff')
        base = sb([128, NE], F32, 'r_base')
        zt = sb([128, D], F32, 'r_zero')
        pt = [ps('r_pt%d' % i) for i in range(2)]
        pl = ps('r_pl'); pr = ps('r_pr'); pb = ps('r_pb')
        sm = {n: sb([128, NE], F32, 'r_' + n) for n in ('lg', 'mask', 'ex', 'G', 'rank', 'ov', 'slot', 'v', 'oh')}
        t8 = sb([128, 8], F32, 'r_t8'); v8 = sb([128, 8], F32, 'r_v8')
        s1 = {n: sb([128, 1], F32, 'r_' + n) for n in ('negm', 'ssum', 'rs')}
        slotf = sb([128, 4], F32, 'r_slotf')
        P.ld(rw[:], W['router_w'].rearrange("(k p) e -> p k e", p=128), writes=['rw'])
        P.ld(rb[:], W['router_b'].partition_broadcast(128), writes=['rb'])
        P.op('pool', lambda e: e.iota(eoff[:], [[C, NE]], base=0, channel_multiplier=0, allow_small_or_imprecise_dtypes=True), [], ['eoff'])
        P.op('pool', lambda e: e.memset(base[:], 0.0), [], ['base'])
        P.op('pool', lambda e: e.memset(zt[:], 0.0), [], ['zt'])
        P.ld(yd[NSLOT:NSLOT + 1, :], zt[0:1, :], reads=['zt'], writes=['ydummy'])
        for t in range(NT):
            b = t % 2
            kx, kb = 'xt%d' % b, 'xb%d' % b
            P.ld(xt[b][:], x_in[t * 128:(t + 1) * 128, :], writes=[kx])
            P.op('act', lambda e, b=b: e.copy(xb[b][:], xt[b][:]), [kx], [kb])
            for q in range(4):
                pq = pt[q % 2]
                kp = 'pt%d' % (q % 2)
                for j in range(4):
                    k = q * 4 + j
                    P.tr(pq[:, j * 128:(j + 1) * 128], xt[b][:, k * 128:(k + 1) * 128], ident[:], [kx, 'c_ident'], [kp])
                eng = 'dve' if q % 2 == 0 else 'act'
                if eng == 'dve':
                    P.op('dve', lambda e, q=q, pq=pq: e.tensor_copy(xT[:, q * 4:(q + 1) * 4, :], pq[:].rearrange("p (a b) -> p a b", a=4)), [kp], ['xT%d' % q])
                else:
                    P.op('act', lambda e, q=q, pq=pq: e.copy(xT[:, q * 4:(q + 1) * 4, :], pq[:].rearrange("p (a b) -> p a b", a=4)), [kp], ['xT%d' % q])
            for k in range(16):
                P.mm(pl[:, 0:NE], xT[:, k, :], rw[:, k, :], k == 0, k == 15, ['xT%d' % (k // 4), 'rw'], ['pl'])
            lg, mask, ex, G, rank, ov, slot, v, oh = (sm[n] for n in ('lg', 'mask', 'ex', 'G', 'rank', 'ov', 'slot', 'v', 'oh'))
            P.op('dve', lambda e: e.tensor_tensor(lg[:], pl[:, 0:NE], rb[:], ALU.add), ['pl', 'rb'], ['lg'])
            P.op('dve', lambda e: e.max(t8[:], lg[:]), ['lg'], ['t8'])
            P.op('dve', lambda e: e.tensor_scalar(mask[:], lg[:], t8[:, 3:4], None, ALU.is_ge), ['lg', 't8'], ['mask'])
            P.op('dve', lambda e: e.tensor_scalar(s1['negm'][:], t8[:, 0:1], -1.0, None, ALU.mult), ['t8'], ['negm'])
            P.op('act', lambda e: e.activation(ex[:], lg[:], AF.Exp, bias=s1['negm'][:, 0:1], scale=1.0), ['lg', 'negm'], ['ex'])
            P.op('dve', lambda e: e.tensor_tensor(ex[:], ex[:], mask[:], ALU.mult), ['ex', 'mask'], ['ex'])
            P.op('dve', lambda e: e.reduce_sum(s1['ssum'][:], ex[:], AX.X), ['ex'], ['ssum'])
            P.op('dve', lambda e: e.reciprocal(s1['rs'][:], s1['ssum'][:]), ['ssum'], ['rs'])
            P.op('dve', lambda e: e.tensor_scalar(G[:], ex[:], s1['rs'][:, 0:1], None, ALU.mult), ['ex', 'rs'], ['G'])
            P.mm(pr[:, 0:NE], uts[:], mask[:], True, True, ['c_uts', 'mask'], ['pr'])
            P.mm(pb[:, 0:NE], ones[:], mask[:], True, True, ['c_ones', 'mask'], ['pb'])
            P.op('dve', lambda e: e.tensor_tensor(rank[:], pr[:, 0:NE], base[:], ALU.add), ['pr', 'base'], ['rank'])
            P.op('dve', lambda e: e.tensor_tensor(base[:], pb[:, 0:NE], base[:], ALU.add), ['pb', 'base', 'rank'], ['base'])
            P.op('dve', lambda e: e.tensor_scalar(ov[:], rank[:], float(C), None, ALU.is_lt), ['rank'], ['ov'])
            P.op('dve', lambda e: e.tensor_tensor(slot[:], rank[:], eoff[:], ALU.add), ['rank', 'eoff'], ['slot'])
            P.op('dve', lambda e: e.tensor_scalar(slot[:], slot[:], float(-NSLOT), None, ALU.add), ['slot'], ['slot'])
            P.op('dve', lambda e: e.tensor_tensor(slot[:], slot[:], ov[:], ALU.mult), ['slot', 'ov'], ['slot'])
            P.op('dve', lambda e: e.tensor_scalar(slot[:], slot[:], float(NSLOT), None, ALU.add), ['slot'], ['slot'])
            P.op('dve', lambda e: e.tensor_tensor(G[:], G[:], ov[:], ALU.mult), ['G', 'ov'], ['G'])
            P.op('dve', lambda e: e.tensor_scalar(v[:], slot[:], -1.0, BIG, ALU.mult, ALU.add), ['slot'], ['v'])
            P.op('dve', lambda e: e.tensor_tensor(v[:], v[:], mask[:], ALU.mult), ['v', 'mask'], ['v'])
            P.op('dve', lambda e: e.max(v8[:], v[:]), ['v'], ['v8'])
            P.op('dve', lambda e: e.tensor_scalar(slotf[:], v8[:, 0:4], -1.0, BIG, ALU.mult, ALU.add), ['v8'], ['slotf'])
            ks = 'slots%d' % t
            P.op('dve', lambda e, t=t: e.tensor_copy(slots_all[:, t, :], slotf[:]), ['slotf'], [ks])
            for c in range(4):
                P.op('dve', lambda e, c=c: e.tensor_scalar(oh[:], v[:], v8[:, c:c + 1], None, ALU.is_equal), ['v', 'v8'], ['oh'])
                P.op('dve', lambda e: e.tensor_tensor(oh[:], oh[:], G[:], ALU.mult), ['oh', 'G'], ['oh'])
                P.op('dve', lambda e, t=t, c=c: e.reduce_sum(gates_all[:, t, c:c + 1], oh[:], AX.X), ['oh'], ['gates%d_%d' % (t, c)])
            for c in range(4):
                sk = 'scat%d_%d' % (t, c)
                scat_keys.append(sk)
                P.dma('pool', lambda e, t=t, c=c, b=b: e.indirect_dma_start(
                    out=xg, out_offset=bass.IndirectOffsetOnAxis(ap=slots_all[:, t, c:c + 1], axis=0),
                    in_=xb[b][:], in_offset=None), [kb, ks], [sk], ind=True)
        if dbg:
            d_lg = nc.dram_tensor("dbg_lg", [128, NE], F32, kind="ExternalOutput").ap()
            d_xT = nc.dram_tensor("dbg_xT", [128, 16 * 128], F32, kind="ExternalOutput").ap()
            P.barrier()
            P.ld(d_lg, sm['lg'][:])
            P.ld(d_xT, xT[:].rearrange("p a b -> p (a b)"))
            P.ld(d_sl, slots_all[:].rearrange("p t c -> p (t c)"))
            P.ld(d_ga, gates_all[:].rearrange("p t c -> p (t c)"))
        P.barrier()
        P.flush()
    with ExitStack() as ph:
        def sb(shape, dt, name):
            return ph.enter_context(nc.sbuf_tensor(name, list(shape), dt))

        def ps(name, dt=F32, w=512):
            return ph.enter_context(nc.psum_tensor(name, [128, w], dt))
        xgt = sb([128, NB, D], BF16, 'e_xgt')
        xT = sb([128, 16, C], BF16, 'e_xT')
        wgu = [sb([128, 16, 512], BF16, 'e_wgu%d' % i) for i in range(2)]
        wd = [sb([128, 16, 512], BF16, 'e_wd%d' % i) for i in range(2)]
        actT = sb([128, 16, C], BF16, 'e_actT')
        bgu = sb([128, NE * 32], F32, 'e_bgu')
        bd = sb([128, D], F32, 'e_bd')
        gl = [sb([128, H], F32, 'e_gl%d' % i) for i in range(2)]
        sg = [sb([128, H], F32, 'e_sg%d' % i) for i in range(2)]
        ln = [sb([128, H], F32, 'e_ln%d' % i) for i in range(2)]
        ysb = [sb([128, 512], F32, 'e_ysb%d' % i) for i in range(2)]
        ptr = [ps('e_ptr%d' % i, BF16, 1024) for i in range(2)]
        pg = [ps('e_pg%d' % i) for i in range(2)]
        pn = [ps('e_pn%d' % i) for i in range(2)]
        py = [ps('e_py%d' % i) for i in range(2)]
        P.ld(bgu[:], W['bgu_l'], writes=['bgu'])
        nw = 0
        nd = 0
        ny = 0
        nel = 0
        for ex_ in range(NE):
            P.ld(xgt[:], xg[ex_ * C:(ex_ + 1) * C, :].rearrange("(b p) d -> p b d", p=128), writes=['xgt'])
            P.ld(bd[:], W['b_down'][ex_, :].partition_broadcast(128), writes=['bd'])
            n4 = 0
            for bk in range(NB):
                for q in range(4):
                    pq = ptr[n4 % 2]; kp = 'ptr%d' % (n4 % 2)
                    for j in range(4):
                        k = q * 4 + j
                        P.tr(pq[:, j * 128:(j + 1) * 128], xgt[:, bk, k * 128:(k + 1) * 128], identb[:], ['xgt', 'identb'], [kp])
                    if n4 % 2 == 0:
                        P.op('dve', lambda e, q=q, bk=bk, pq=pq: e.tensor_copy(xT[:, q * 4:(q + 1) * 4, bk * 128:(bk + 1) * 128], pq[:, 0:512].rearrange("p (a b) -> p a b", a=4)), [kp], ['xT'])
                    else:
                        P.op('act', lambda e, q=q, bk=bk, pq=pq: e.copy(xT[:, q * 4:(q + 1) * 4, bk * 128:(bk + 1) * 128], pq[:, 0:512].rearrange("p (a b) -> p a b", a=4)), [kp], ['xT'])
                    n4 += 1
            for g in range(8):
                wb_ = wgu[nw % 2]; kw = 'wgu%d' % (nw % 2); nw += 1
                wv = W['w_gate_up'][ex_].rearrange("(k p) f -> p k f", p=128)
                P.ld(wb_[:, :, 0:256], wv[:, :, g * 256:(g + 1) * 256], writes=[kw + 'a'], eng='pool')
                P.ld(wb_[:, :, 256:512], wv[:, :, FF + g * 256:FF + (g + 1) * 256], writes=[kw + 'b'], eng='pool')
                for j in range(2):
                    fj = g * 2 + j
                    for s in range(2):
                        i2 = nel % 2; nel += 1
                        kg, kn = 'pg%d' % i2, 'pn%d' % i2
                        for k in range(16):
                            P.mm(pg[i2][:, 0:H], wb_[:, k, j * 128:(j + 1) * 128], xT[:, k, s * H:(s + 1) * H], k == 0, k == 15, [kw + 'a', 'xT'], [kg])
                        for k in range(16):
                            P.mm(pn[i2][:, 0:H], wb_[:, k, 256 + j * 128:256 + (j + 1) * 128], xT[:, k, s * H:(s + 1) * H], k == 0, k == 15, [kw + 'b', 'xT'], [kn])
                        cg = ex_ * 32 + fj
                        cl = ex_ * 32 + 16 + fj
                        P.op('dve', lambda e, i2=i2, cg=cg: e.tensor_scalar(gl[i2][:], pg[i2][:, 0:H], bgu[:, cg:cg + 1], 7.0, ALU.add, ALU.min), [kg, 'bgu'], ['gl%d' % i2])
                        P.op('act', lambda e, i2=i2: e.activation(sg[i2][:], gl[i2][:], AF.Sigmoid, scale=1.702), ['gl%d' % i2], ['sg%d' % i2])
                        P.op('dve', lambda e, i2=i2, cl=cl: e.tensor_scalar(ln[i2][:], pn[i2][:, 0:H], bgu[:, cl:cl + 1], 7.0, ALU.add, ALU.min), [kn, 'bgu'], ['ln%d' % i2])
                        P.op('pool', lambda e, i2=i2: e.tensor_scalar(ln[i2][:], ln[i2][:], -7.0, 1.0, ALU.max, ALU.add), ['ln%d' % i2], ['ln%d' % i2])
                        P.op('pool', lambda e, i2=i2: e.tensor_tensor(gl[i2][:], gl[i2][:], sg[i2][:], ALU.mult), ['gl%d' % i2, 'sg%d' % i2], ['gl%d' % i2])
                        P.op('dve', lambda e, i2=i2, fj=fj, s=s: e.tensor_tensor(actT[:, fj, s * H:(s + 1) * H], gl[i2][:], ln[i2][:], ALU.mult), ['gl%d' % i2, 'ln%d' % i2], ['actT'])
            for c in range(4):
                wb_ = wd[nd % 2]; kw = 'wd%d' % (nd % 2); nd += 1
                P.ld(wb_[:], W['w_down'][ex_].rearrange("(k p) d -> p k d", p=128)[:, :, c * 512:(c + 1) * 512], writes=[kw], eng='pool')
                for bk in range(NB):
                    i2 = ny % 2; ny += 1
                    for k in range(16):
                        P.mm(py[i2][:], actT[:, k, bk * 128:(bk + 1) * 128], wb_[:, k, :], k == 0, k == 15, ['actT', kw], ['py%d' % i2])
                    P.op('dve', lambda e, i2=i2, c=c: e.tensor_tensor(ysb[i2][:], py[i2][:], bd[:, c * 512:(c + 1) * 512], ALU.add), ['py%d' % i2, 'bd'], ['ysb%d' % i2])
                    P.ld(yd[ex_ * C + bk * 128:ex_ * C + (bk + 1) * 128, c * 512:(c + 1) * 512], ysb[i2][:], reads=['ysb%d' % i2], writes=['yd'], eng='sp')
        P.barrier()
        P.flush()
    with ExitStack() as ph:
        def sb(shape, dt, name):
            return ph.enter_context(nc.sbuf_tensor(name, list(shape), dt))
        xt = [sb([128, D], F32, 'c_xt%d' % i) for i in range(2)]
        yg = [sb([128, D], F32, 'c_yg%d' % i) for i in range(4)]
        ot = [sb([128, D], F32, 'c_ot%d' % i) for i in range(2)]
        gt = sb([128, D], F32, 'c_g'); bt = sb([128, D], F32, 'c_b')
        st = sb([128, 24], F32, 'c_st'); ag = sb([128, 2], F32, 'c_ag'); rstd = sb([128, 1], F32, 'c_rstd')
        P.ld(gt[:], W['ln_g'].partition_broadcast(128), writes=['lng'])
        P.ld(bt[:], W['ln_b'].partition_broadcast(128), writes=['lnb'])
        for t in range(NT):
            b = t % 2
            kx = 'cxt%d' % b
            P.ld(xt[b][:], x_in[t * 128:(t + 1) * 128, :], writes=[kx])
            for c in range(4):
                P.dma('pool', lambda e, t=t, c=c: e.indirect_dma_start(
                    out=yg[c][:], out_offset=None, in_=yd,
                    in_offset=bass.IndirectOffsetOnAxis(ap=slots_all[:, t, c:c + 1], axis=0),
                    ), [], ['yg%d' % c], ind=True)
            P.op('act', lambda e, b=b: e.mul(xt[b][:], xt[b][:], ALPHA), [kx], [kx])
            for c in range(4):
                P.op('dve', lambda e, b=b, c=c, t=t: e.scalar_tensor_tensor(xt[b][:], yg[c][:], gates_all[:, t, c:c + 1], xt[b][:], ALU.mult, ALU.add), ['yg%d' % c, kx], [kx])
            ko = 'cot%d' % b
            emit_ln(P, xt[b], ot[b], gt, bt, 'c_', [kx], [ko], st, ag, rstd)
            P.ld(out_d[t * 128:(t + 1) * 128, :], ot[b][:], reads=[ko])
        P.barrier()
        P.flush()


def build_moe(NT, C, dbg=False):
    nc = bass.Bass("TRN2", target_bir_lowering=False)
    x_in = nc.dram_tensor("x_in", [NT * 128, D], F32, kind="ExternalInput").ap()
    out_d = nc.dram_tensor("out", [NT * 128, D], F32, kind="ExternalOutput").ap()
    W = {}
    W['router_w'] = nc.dram_tensor("router_w", [D, NE], F32, kind="ExternalInput").ap()
    W['router_b'] = nc.dram_tensor("router_b", [NE], F32, kind="ExternalInput").ap()
    W['w_gate_up'] = nc.dram_tensor("w_gate_up", [NE, D, 2 * FF], F32, kind="ExternalInput").ap()
    W['bgu_l'] = nc.dram_tensor("bgu_l", [128, NE * 32], F32, kind="ExternalInput").ap()
    W['w_down'] = nc.dram_tensor("w_down", [NE, FF, D], F32, kind="ExternalInput").ap()
    W['b_down'] = nc.dram_tensor("b_down", [NE, D], F32, kind="ExternalInput").ap()
    W['ln_g'] = nc.dram_tensor("ln_g", [D], F32, kind="ExternalInput").ap()
    W['ln_b'] = nc.dram_tensor("ln_b", [D], F32, kind="ExternalInput").ap()
    with ExitStack() as es:
        P = Prog(nc, es)
        cst = load_consts(P, nc, ['c_ident', 'c_uts', 'c_ones'])
        emit_moe(P, nc, es, NT, C, x_in, out_d, W, cst, dbg)
        P.finish()
    return nc


def moe_inputs(x_sh, router_w, router_b, w_gate_up, b_gate_up, w_down, b_down, ln_g, ln_b):
    c = consts_np()
    bgu_l = np.ascontiguousarray(b_gate_up.reshape(NE, 32, 128).transpose(2, 0, 1).reshape(128, NE * 32))
    com = {'router_w': router_w, 'router_b': router_b, 'w_gate_up': w_gate_up, 'bgu_l': bgu_l,
           'w_down': w_down, 'b_down': b_down, 'ln_g': ln_g, 'ln_b': ln_b,
           'c_ident': c['c_ident'], 'c_uts': c['c_uts'], 'c_ones': c['c_ones']}
    return [dict(com, x_in=np.ascontiguousarray(xs)) for xs in x_sh]


GROUPS = ((128, 1), (512, 4), (2048, 16))
NEG = -30000.0
HALO = 2048


def attn_static():
    import math
    out = {}
    for g, (w, d) in enumerate(GROUPS):
        L = w + 256
        S = np.zeros((33, L), np.float32)
        for n in range(L):
            dl = n - 127
            if dl >= 0 and dl % d == 0 and dl <= w:
                if dl < 16:
                    bk = dl
                else:
                    v = np.log(np.float32(max(dl, 1)) / np.float32(16)) / np.float32(math.log(2048 / 16)) * np.float32(16)
                    bk = min(16 + int(np.float32(v).astype(np.int32)), 31)
                S[bk, n] = 1.0
            else:
                S[32, n] = NEG
        out['c_S%d' % g] = S
    return out


def emit_attn(P, nc, es, NQ, xc, hv_d, W, out_d, cst):
    NTT = 16 + NQ
    NTOK = NTT * 128
    ident = cst['c_ident']
    xT_d = nc.dram_tensor("a_xT", [16, 128, NTOK], BF16, kind="Internal").ap()
    qT_d = nc.dram_tensor("a_qT", [3, 8, 128, NQ * 128], BF16, kind="Internal").ap()
    kT_d = nc.dram_tensor("a_kT", [3, 8, 128, NTOK], BF16, kind="Internal").ap()
    v_d = nc.dram_tensor("a_v", [3, 8, NTOK, 128], BF16, kind="Internal").ap()
    Zs = [[nc.dram_tensor("a_Z%d_%d" % (g, h), [128, GROUPS[g][0] + 256], F32, kind="Internal") for h in range(8)] for g in range(3)]
    identb = es.enter_context(nc.sbuf_tensor('a_identb', [128, 128], BF16))
    P.op('dve', lambda e: e.tensor_copy(identb[:], ident[:]), ['c_ident'], ['a_identb'])
    o_all = es.enter_context(nc.sbuf_tensor('a_oall', [128, NQ, 1024], BF16))
    with ExitStack() as ph:
        def sb(shape, dt, name):
            return ph.enter_context(nc.sbuf_tensor(name, list(shape), dt))
        rbT = sb([33, 24], F32, 'a0_rbT')
        lb = sb([33, 128], F32, 'a0_lb')
        Ssb = [sb([33, GROUPS[g][0] + 256], F32, 'a0_S%d' % g) for g in range(3)]
        fb = [sb([128, 2304], F32, 'a0_fb%d' % i) for i in range(2)]
        pz = [ph.enter_context(nc.psum_tensor('a0_pz%d' % i, [128, 512], F32)) for i in range(2)]
        P.op('pool', lambda e: e.memset(rbT[:], 1.0), [], ['rbT'])
        P.ld(rbT[0:32, :], W['rel_bias'], writes=['rbT'])
        for g in range(3):
            Sd = nc.dram_tensor('c_S%d' % g, [33, GROUPS[g][0] + 256], F32, kind="ExternalInput").ap()
            P.ld(Ssb[g][:], Sd, writes=['S%d' % g])
        n = 0
        for g in range(3):
            L = GROUPS[g][0] + 256
            for h in range(8):
                c = g * 8 + h
                P.op('dve', lambda e, c=c: e.tensor_copy(lb[:], rbT[:, c:c + 1].to_broadcast([33, 128])), ['rbT'], ['lb'])
                f = fb[n % 2]; kf = 'fb%d' % (n % 2); n += 1
                for ci, c0 in enumerate(range(0, L, 512)):
                    cw = min(512, L - c0)
                    pzz = pz[ci % 2]
                    P.mm(pzz[:, 0:cw], lb[:], Ssb[g][:, c0:c0 + cw], True, True, ['lb', 'S%d' % g], ['pz%d' % (ci % 2)])
                    P.op('act', lambda e, pzz=pzz, f=f, c0=c0, cw=cw: e.copy(f[:, c0:c0 + cw], pzz[:, 0:cw]), ['pz%d' % (ci % 2)], [kf])
                P.ld(Zs[g][h].ap(), f[:, 0:L], reads=[kf], writes=['Z'])
        P.barrier()
        P.flush()
    with ExitStack() as ph:
        def sb(shape, dt, name):
            return ph.enter_context(nc.sbuf_tensor(name, list(shape), dt))

        def ps(name, dt=F32, w=512):
            return ph.enter_context(nc.psum_tensor(name, [128, w], dt))
        xt = [sb([128, D], F32, 'a1_xt%d' % i) for i in range(2)]
        xb = [sb([128, D], BF16, 'a1_xb%d' % i) for i in range(2)]
        xTt = [sb([128, 16, 128], BF16, 'a1_xTt%d' % i) for i in range(2)]
        wblk = [sb([128, 16, 512], BF16, 'a1_w%d' % i) for i in range(2)]
        xTg = [sb([128, 16, 512], BF16, 'a1_xTg%d' % i) for i in range(2)]
        stg = [sb([128, 512], BF16, 'a1_stg%d' % i) for i in range(4)]
        ptr = [ps('a1_ptr%d' % i, BF16, 1024) for i in range(2)]
        pm = [ps('a1_pm%d' % i) for i in range(4)]
        n4 = 0
        for t in range(NTT):
            b = t % 2
            P.ld(xt[b][:], xc[t * 128:(t + 1) * 128, :], writes=['xt%d' % b])
            P.op('act', lambda e, b=b: e.copy(xb[b][:], xt[b][:]), ['xt%d' % b], ['xb%d' % b])
            for q in range(4):
                pq = ptr[n4 % 2]; kp = 'ptr%d' % (n4 % 2)
                for j in range(4):
                    k = q * 4 + j
                    P.tr(pq[:, j * 128:(j + 1) * 128], xb[b][:, k * 128:(k + 1) * 128], identb[:], ['xb%d' % b, 'a_identb'], [kp])
                P.op('dve', lambda e, q=q, b=b, pq=pq: e.tensor_copy(xTt[b][:, q * 4:(q + 1) * 4, :], pq[:, 0:512].rearrange("p (a b) -> p a b", a=4)), [kp], ['xTt%d' % b])
                n4 += 1
            P.ld(xT_d[:, :, t * 128:(t + 1) * 128].rearrange("k p t -> p k t"), xTt[b][:], reads=['xTt%d' % b], writes=['xT_d'])
        P.barrier()
        ns = 0
        npm = 0
        for cb in range(18):
            g, part, hh = cb // 6, (cb % 6) // 2, cb % 2
            wb_ = wblk[cb % 2]; kw = 'w%d' % (cb % 2)
            P.ld(wb_[:], W['attn_w_in'].rearrange("(k p) f -> p k f", p=128)[:, :, cb * 512:(cb + 1) * 512], writes=[kw], eng='pool')
            tg0 = 4 if part == 0 else 0
            for tg in range(tg0, NTT // 4):
                xg_ = xTg[tg % 2]; kx = 'xTg%d' % (tg % 2)
                P.ld(xg_[:], xT_d[:, :, tg * 512:(tg + 1) * 512].rearrange("k p t -> p k t"), writes=[kx])
                for j in range(4):
                    pmm = pm[npm % 4]; kpm = 'pm%d' % (npm % 4); npm += 1
                    st_ = stg[ns % 4]; kst = 'stg%d' % (ns % 4); ns += 1
                    if part < 2:
                        h = hh * 4 + j
                        for k in range(16):
                            P.mm(pmm[:], wb_[:, k, j * 128:(j + 1) * 128], xg_[:, k, :], k == 0, k == 15, [kw, kx], [kpm])
                        if ns % 2:
                            P.op('act', lambda e, st_=st_, pmm=pmm: e.copy(st_[:], pmm[:]), [kpm], [kst])
                        else:
                            P.op('dve', lambda e, st_=st_, pmm=pmm: e.tensor_copy(st_[:], pmm[:]), [kpm], [kst])
                        if part == 0:
                            P.ld(qT_d[g, h, :, (tg - 4) * 512:(tg - 3) * 512], st_[:], reads=[kst], writes=['qT_d'])
                        else:
                            P.ld(kT_d[g, h, :, tg * 512:(tg + 1) * 512], st_[:], reads=[kst], writes=['kT_d'])
                    else:
                        for k in range(16):
                            P.mm(pmm[:], xg_[:, k, j * 128:(j + 1) * 128], wb_[:, k, :], k == 0, k == 15, [kw, kx], [kpm])
                        if ns % 2:
                            P.op('act', lambda e, st_=st_, pmm=pmm: e.copy(st_[:], pmm[:]), [kpm], [kst])
                        else:
                            P.op('dve', lambda e, st_=st_, pmm=pmm: e.tensor_copy(st_[:], pmm[:]), [kpm], [kst])
                        r0 = tg * 512 + j * 128
                        P.ld(v_d[g, hh * 4:(hh + 1) * 4, r0:r0 + 128, :].rearrange("h t d -> t h d"),
                             st_[:].rearrange("p (h d) -> p h d", h=4), reads=[kst], writes=['v_d'])
        P.barrier()
        P.flush()
    scale = 128.0 ** -0.5
    with ExitStack() as ph:
        def sb(shape, dt, name):
            return ph.enter_context(nc.sbuf_tensor(name, list(shape), dt))

        def ps(name, dt=F32, w=512):
            return ph.enter_context(nc.psum_tensor(name, [128, w], dt))
        kT = [sb([128, NTOK], BF16, 'a2_kT%d' % g) for g in range(3)]
        qT = [sb([128, NQ * 128], BF16, 'a2_qT%d' % g) for g in range(3)]
        vs = [sb([128, NTT, 129], BF16, 'a2_v%d' % g) for g in range(3)]
        Bt = sb([128, 24, 128], F32, 'a2_Bt')
        tmp = [sb([128, 512], F32, 'a2_tmp%d' % i) for i in range(2)]
        pT = [sb([128, 512], BF16, 'a2_pT%d' % i) for i in range(3)]
        rc = [sb([128, 1], F32, 'a2_rc%d' % i) for i in range(2)]
        hv = sb([128, 1], F32, 'a2_hv')
        pS = [ps('a2_pS%d' % i) for i in range(3)]
        pO = [ps('a2_pO%d' % i) for i in range(2)]
        P.ld(hv[:], hv_d, writes=['hv'])
        for g in range(3):
            P.op('pool', lambda e, g=g: e.memset(vs[g][:, :, 128:129], 1.0), [], ['vs%d' % g])
            P.op('dve', lambda e, g=g: e.tensor_scalar(vs[g][:, 0:16, 128:129], vs[g][:, 0:16, 128:129], hv[:, 0:1], None, ALU.mult), ['vs%d' % g, 'hv'], ['vs%d' % g])
        chunks = []
        bi = 0
        for g in range(3):
            nm = GROUPS[g][0] // 128 + 1
            for m0 in range(0, nm, 4):
                ms = list(range(m0, min(m0 + 4, nm)))
                chunks.append((g, ms, bi))
                bi += len(ms)
        nS = 0
        nO = 0
        for h in range(8):
            for g in range(3):
                P.ld(kT[g][:], kT_d[g, h], writes=['kT%d' % g])
                P.ld(qT[g][:], qT_d[g, h], writes=['qT%d' % g])
                P.ld(vs[g][:, :, 0:128], v_d[g, h].rearrange("(t p) d -> p t d", p=128), writes=['vs%d' % g])
            bi = 0
            for g in range(3):
                L = GROUPS[g][0] + 256
                for m in range(GROUPS[g][0] // 128 + 1):
                    tap = bass.AP(tensor=Zs[g][h], offset=m * 128 + 127, ap=[[L - 1, 128], [1, 128]])
                    P.ld(Bt[:, bi, :], tap, writes=['Bt'])
                    bi += 1
            for qt in range(NQ):
                T = 16 + qt
                io = nO % 2; nO += 1
                first = True
                for (g, ms, b0) in chunks:
                    i3 = nS % 3; nS += 1
                    n = len(ms)
                    for mi, m in enumerate(ms):
                        KT = T - m
                        P.mm(pS[i3][:, mi * 128:(mi + 1) * 128], kT[g][:, KT * 128:(KT + 1) * 128], qT[g][:, qt * 128:(qt + 1) * 128], True, True,
                             ['kT%d' % g, 'qT%d' % g], ['pS%d' % i3])
                    i2 = nS % 2
                    P.op('dve', lambda e, i3=i3, i2=i2, n=n, b0=b0: e.scalar_tensor_tensor(
                        tmp[i2][:, 0:n * 128], pS[i3][:, 0:n * 128], scale, Bt[:, b0:b0 + n, :].rearrange("p a b -> p (a b)"), ALU.mult, ALU.add),
                        ['pS%d' % i3, 'Bt'], ['tmp%d' % i2])
                    P.op('act', lambda e, i3=i3, i2=i2, n=n: e.activation(pT[i3][:, 0:n * 128], tmp[i2][:, 0:n * 128], AF.Exp), ['tmp%d' % i2], ['pT%d' % i3])
                    for mi, m in enumerate(ms):
                        KT = T - m
                        last = (g == 2 and m == 16)
                        P.mm(pO[io][:, 0:129], pT[i3][:, mi * 128:(mi + 1) * 128], vs[g][:, KT, :], first, last, ['pT%d' % i3, 'vs%d' % g], ['pO%d' % io])
                        first = False
                P.op('dve', lambda e, io=io: e.reciprocal(rc[io][:], pO[io][:, 128:129]), ['pO%d' % io], ['rc%d' % io])
                P.op('act', lambda e, io=io, qt=qt, h=h: e.activation(o_all[:, qt, h * 128:(h + 1) * 128], pO[io][:, 0:128], AF.Copy, scale=rc[io][:, 0:1]),
                     ['pO%d' % io, 'rc%d' % io], ['oall%d' % qt])
        P.barrier()
        P.flush()
    with ExitStack() as ph:
        def sb(shape, dt, name):
            return ph.enter_context(nc.sbuf_tensor(name, list(shape), dt))

        def ps(name, dt=F32, w=512):
            return ph.enter_context(nc.psum_tensor(name, [128, w], dt))
        wo = sb([128, 8, D], BF16, 'a3_wo')
        oT = [sb([128, 8, 128], BF16, 'a3_oT%d' % i) for i in range(2)]
        xt = [sb([128, D], F32, 'a3_xt%d' % i) for i in range(2)]
        ot = [sb([128, D], F32, 'a3_ot%d' % i) for i in range(2)]
        gt = sb([128, D], F32, 'a3_g'); bt = sb([128, D], F32, 'a3_b')
        st = sb([128, 24], F32, 'a3_st'); ag = sb([128, 2], F32, 'a3_ag'); rstd = sb([128, 1], F32, 'a3_rstd')
        ptr = [ps('a3_ptr%d' % i, BF16, 1024) for i in range(2)]
        py = [ps('a3_py%d' % i) for i in range(4)]
        P.ld(wo[:], W['attn_w_out'].rearrange("(k p) d -> p k d", p=128), writes=['wo'], eng='pool')
        P.ld(gt[:], W['ln_g'].partition_broadcast(128), writes=['lng'])
        P.ld(bt[:], W['ln_b'].partition_broadcast(128), writes=['lnb'])
        for qt in range(NQ):
            b = qt % 2
            P.ld(xt[b][:], xc[(16 + qt) * 128:(17 + qt) * 128, :], writes=['xt%d' % b])
            for q in range(2):
                pq = ptr[q]; kp = 'ptr%d' % q
                for j in range(4):
                    k = q * 4 + j
                    P.tr(pq[:, j * 128:(j + 1) * 128], o_all[:, qt, k * 128:(k + 1) * 128], identb[:], ['oall%d' % qt, 'a_identb'], [kp])
                P.op('dve', lambda e, q=q, b=b, pq=pq: e.tensor_copy(oT[b][:, q * 4:(q + 1) * 4, :], pq[:, 0:512].rearrange("p (a b) -> p a b", a=4)), [kp], ['oT%d' % b])
            for c in range(4):
                for k in range(8):
                    P.mm(py[c][:], oT[b][:, k, :], wo[:, k, c * 512:(c + 1) * 512], k == 0, k == 7, ['oT%d' % b, 'wo'], ['py%d' % c])
                P.op('dve', lambda e, b=b, c=c: e.scalar_tensor_tensor(xt[b][:, c * 512:(c + 1) * 512], xt[b][:, c * 512:(c + 1) * 512], ALPHA, py[c][:], ALU.mult, ALU.add),
                     ['py%d' % c, 'xt%d' % b], ['xt%d' % b])
            emit_ln(P, xt[b], ot[b], gt, bt, 'a3_', ['xt%d' % b], ['ot%d' % b], st, ag, rstd)
            P.ld(out_d[qt * 128:(qt + 1) * 128, :], ot[b][:], reads=['ot%d' % b])
        P.barrier()
        P.flush()


def build_attn(NQ):
    nc = bass.Bass("TRN2", target_bir_lowering=False)
    xc = nc.dram_tensor("xc", [(16 + NQ) * 128, D], F32, kind="ExternalInput").ap()
    hv_d = nc.dram_tensor("hv", [128, 1], F32, kind="ExternalInput").ap()
    out_d = nc.dram_tensor("out", [NQ * 128, D], F32, kind="ExternalOutput").ap()
    W = {}
    W['rel_bias'] = nc.dram_tensor("rel_bias", [32, 24], F32, kind="ExternalInput").ap()
    W['attn_w_in'] = nc.dram_tensor("attn_w_in", [D, 9216], F32, kind="ExternalInput").ap()
    W['attn_w_out'] = nc.dram_tensor("attn_w_out", [1024, D], F32, kind="ExternalInput").ap()
    W['ln_g'] = nc.dram_tensor("ln_g", [D], F32, kind="ExternalInput").ap()
    W['ln_b'] = nc.dram_tensor("ln_b", [D], F32, kind="ExternalInput").ap()
    with ExitStack() as es:
        P = Prog(nc, es)
        cst = load_consts(P, nc, ['c_ident'])
        emit_attn(P, nc, es, NQ, xc, hv_d, W, out_d, cst)
        P.finish()
    return nc


class RR:
    def __init__(self, items):
        self.items = items
        self.i = 0

    def get(self):
        it = self.items[self.i % len(self.items)]
        self.i += 1
        return it


def dn_consts_np():
    c = {}
    iu = np.triu(np.ones((128, 128), np.float32), 1)
    c['c_negu'] = (-1e4 * iu).astype(np.float32)
    c['c_negl'] = (-1e4 * iu.T).astype(np.float32)
    c['c_strl'] = iu.T.copy()
    return c


def emit_dn(P, nc, es, NB_, NTL, x_d, W, out_d, cst, dbg=False):
    ident, uti, ones = cst['c_ident'], cst['c_uti'], cst['c_ones']
    negu, negl, strl = cst['c_negu'], cst['c_negl'], cst['c_strl']
    scale = 128.0 ** -0.5

    def sb(shape, dt, name):
        return es.enter_context(nc.sbuf_tensor(name, list(shape), dt))

    identb = sb([128, 128], BF16, 'd_identb')
    P.op('dve', lambda e: e.tensor_copy(identb[:], ident[:]), ['c_ident'], ['d_identb'])
    wsl = sb([128, 16, 1544], BF16, 'd_wsl')
    P.ld(wsl[:], W['w_sl'].rearrange("(k p) f -> p k f", p=128), writes=['wsl'], eng='pool')
    cw = sb([128, 8, 4], F32, 'd_cw')
    P.ld(cw[:], W['cw_l'], writes=['cw'])
    ng = sb([128, 128], F32, 'd_ng')
    P.ld(ng[:], W['norm_g'].partition_broadcast(128), writes=['ng'])
    negA = sb([128, 4], F32, 'd_negA')
    dtb = sb([128, 4], F32, 'd_dtb')
    P.ld(negA[:], W['alog_l'].partition_broadcast(128), writes=['negA'])
    P.ld(dtb[:], W['dtb_l'].partition_broadcast(128), writes=['dtb'])
    P.op('act', lambda e: e.activation(negA[:], negA[:], AF.Exp), ['negA'], ['negA'])
    P.op('dve', lambda e: e.tensor_scalar(negA[:], negA[:], -1.0, None, ALU.mult), ['negA'], ['negA'])
    xt = [sb([128, D], F32, 'd_xt0')]
    xb = [sb([128, D], BF16, 'd_xb%d' % i) for i in range(2)]
    xT = [sb([128, 16, 128], BF16, 'd_xT%d' % i) for i in range(2)]
    raw = [[sb([128, 131], F32, 'd_raw%d_%d' % (i, c)) for c in range(8)] for i in range(2)]
    qk = [[sb([128, 128], BF16, 'd_qk%d_%d' % (i, c)) for c in range(4)] for i in range(4)]
    vT = [[sb([128, 128], F32, 'd_vT%d_%d' % (i, c)) for c in range(4)] for i in range(4)]
    qf = [[sb([128, 128], F32, 'd_qf%d_%d' % (i, c)) for c in range(2)] for i in range(4)]
    zs = [sb([128, 512], F32, 'd_zs%d' % i) for i in range(4)]
    outt = [sb([128, 512], BF16, 'd_out%d' % i) for i in range(2)]
    sm4 = {n: [sb([128, 4], F32, 'd_%s%d' % (n, i)) for i in range(4)] for n in ('beta', 'nbeta', 'g', 'gc', 'ngc', 'xa')}
    ded = {n: [[sb([128, 128], F32 if n in ('Erow', 'vb') else BF16, 'd_%s%d_%d' % (n, i, j)) for j in range(4)] for i in range(2)]
           for n in ('kdec', 'vb', 'qtil')}
    ded['Erow'] = [[None] * 4 for _ in range(2)]
    ded['QKdT'] = [[None] * 4 for _ in range(2)]
    c1 = [[sb([128, 1], F32, 'd_c1%d_%d' % (i, j)) for j in range(4)] for i in range(2)]
    growl = [[sb([128, 1], F32, 'd_gl%d_%d' % (i, j)) for j in range(4)] for i in range(2)]
    S4 = sb([128, 512], F32, 'd_S4')
    Sbf4 = sb([128, 512], BF16, 'd_Sbf4')
    Sst = [S4[:, j * 128:(j + 1) * 128] for j in range(4)]
    Sbf = [Sbf4[:, j * 128:(j + 1) * 128] for j in range(4)]
    E4 = [sb([128, 512], F32, 'd_E4_%d' % i) for i in range(2)]
    Q4 = [sb([128, 512], BF16, 'd_Q4_%d' % i) for i in range(2)]
    for i in range(2):
        for j in range(4):
            ded['Erow'][i][j] = E4[i][:, j * 128:(j + 1) * 128]
            ded['QKdT'][i][j] = Q4[i][:, j * 128:(j + 1) * 128]
    c4 = {}
    for nm, src_ in (('negu4', negu), ('negl4', negl), ('strl4', strl)):
        c4[nm] = sb([128, 512], F32, 'd_' + nm)
        for j in range(4):
            P.op('pool', lambda e, t_=c4[nm], src_=src_, j=j: e.tensor_copy(t_[:, j * 128:(j + 1) * 128], src_[:]), ['c_' + nm[:4]], ['d_' + nm])
    tf4 = RR([(sb([128, 512], F32, 'd_tf4_%d' % i), 'tf4_%d' % i) for i in range(10)])
    growl4 = [sb([128, 4], F32, 'd_gl4_%d' % i) for i in range(2)]
    tf = RR([(sb([128, 128], F32, 'd_tf%d' % i), 'tf%d' % i) for i in range(16)])
    tb = RR([(sb([128, 128], BF16, 'd_tb%d' % i), 'tb%d' % i) for i in range(8)])
    tn = RR([(sb([128, 128], BF16, 'd_tn%d' % i), 'tn%d' % i) for i in range(8)])
    tn4 = RR([(sb([128, 512], BF16, 'd_tn4_%d' % i), 'tn4_%d' % i) for i in range(16)])
    identb4 = sb([128, 512], BF16, 'd_identb4')
    for j4 in range(4):
        P.op('dve', lambda e, j4=j4: e.tensor_copy(identb4[:, j4 * 128:(j4 + 1) * 128], ident[:]), ['c_ident'], ['d_identb4'])
    TT4 = [sb([128, 512], BF16, 'd_TT4_%d' % i) for i in range(2)]
    identb2 = identb
    t1 = RR([(sb([128, 1], F32, 'd_t1%d' % i), 't1%d' % i) for i in range(16)])
    pxt = es.enter_context(nc.psum_tensor('d_pxt', [128, 1024], BF16))
    pkt = es.enter_context(nc.psum_tensor('d_pkt', [128, 1024], BF16))
    pz = es.enter_context(nc.psum_tensor('d_pz', [128, 512], F32))
    pm0b = es.enter_context(nc.psum_tensor('d_pm0b', [128, 1024], BF16))
    pbk = RR([(es.enter_context(nc.psum_tensor('d_pb%d' % i, [128, 512], F32)), 'pb%d' % i) for i in range(4)])

    def sl(t, j):
        return t[:, j * 128:(j + 1) * 128]

    rtm = [sb([128, 512], BF16, 'd_rtm%d' % i) for i in range(2)]
    tfP = RR([(sb([128, 128], F32, 'd_tfP%d' % i), 'tfP%d' % i) for i in range(24)])
    pbP = RR([pbk.items[3]])
    pbU = RR(pbk.items[0:3])

    def genP(b_, n):
        par = n % 2
        p4 = n % 4
        row0 = (b_ * NTL + n) * 128
        kx, kb_, kT_ = 'xt0', 'xb%d' % par, 'xT%d' % par
        P.ld(xt[0][:], x_d[row0:row0 + 128, :], writes=[kx])
        P.op('act', lambda e, par=par, p4=p4: e.copy(xb[par][:], xt[0][:]), [kx], [kb_])
        for q in range(4):
            half = q % 2
            for jj in range(4):
                k = q * 4 + jj
                P.tr(pxt[:, half * 512 + jj * 128:half * 512 + (jj + 1) * 128], xb[par][:, k * 128:(k + 1) * 128], identb[:], [kb_, 'd_identb'], ['pxt'])
            P.op('dve', lambda e, q=q, par=par, p4=p4, half=half: e.tensor_copy(xT[par][:, q * 4:(q + 1) * 4, :], pxt[:, half * 512:(half + 1) * 512].rearrange("p (a b) -> p a b", a=4)), [], ['pxt', kT_])
        yield
        ys = [None] * 4
        for grp in range(2):
            bk_, kbk = pbP.get()
            for k in range(16):
                P.mm(bk_[:, 0:512], xT[par][:, k, :], wsl[:, k, grp * 512:(grp + 1) * 512], k == 0, k == 15, ['wsl', kT_], [kbk])
            P.op('act', lambda e, bk_=bk_, grp=grp: e.copy(rtm[grp][:], bk_[:, 0:512]), [], [kbk, 'rtm%d' % grp])
            for ci in range(4):
                P.tr(pxt[:, grp * 512 + ci * 128:grp * 512 + (ci + 1) * 128], rtm[grp][:, ci * 128:(ci + 1) * 128], identb[:], ['rtm%d' % grp, 'd_identb'], ['pxt'])
            for ci in range(4):
                c = grp * 4 + ci
                kr = 'raw%d_%d' % (par, c)
                P.op('act', lambda e, c=c, par=par, p4=p4, grp=grp, ci=ci: e.copy(raw[par][c][:, 3:131], pxt[:, grp * 512 + ci * 128:grp * 512 + (ci + 1) * 128]), [], ['pxt', kr])
                P.op('pool', lambda e, c=c, par=par, p4=p4: e.tensor_copy(raw[1 - par][c][:, 0:3], raw[par][c][:, 128:131]), [kr], ['raw%d_%d' % (1 - par, c)])
                acc, ka = tfP.get()
                P.op('dve', lambda e, c=c, par=par, p4=p4, acc=acc: e.tensor_scalar(acc[:], raw[par][c][:, 0:128], cw[:, c, 0:1], None, ALU.mult), [kr, 'cw'], [ka])
                for jt in range(1, 4):
                    P.op('dve', lambda e, c=c, par=par, p4=p4, acc=acc, jt=jt: e.scalar_tensor_tensor(acc[:], raw[par][c][:, jt:jt + 128], cw[:, c, jt:jt + 1], acc[:], ALU.mult, ALU.add), [kr, 'cw', ka], [ka])
                if c >= 4:
                    P.op('act', lambda e, c=c, par=par, p4=p4, acc=acc: e.activation(vT[p4][c - 4][:], acc[:], AF.Silu), [ka], ['vT%d_%d' % (p4, c - 4)])
                else:
                    y, ky = tfP.get()
                    P.op('act', lambda e, acc=acc, y=y: e.activation(y[:], acc[:], AF.Silu), [ka], [ky])
                    ys[c] = (y, ky)
                yield
        bk_, kbk = pbP.get()
        sqs = []
        for c in range(4):
            y, ky = ys[c]
            sq, ksq = tfP.get()
            P.op('pool', lambda e, y=y, sq=sq: e.tensor_tensor(sq[:], y[:], y[:], ALU.mult), [ky], [ksq])
            sqs.append((sq, ksq))
        for c in range(4):
            P.mm(sl(bk_, c), ones[:], sqs[c][0][:], True, True, ['c_ones', sqs[c][1]], [kbk])
        for c in range(4):
            y, ky = ys[c]
            rn, krn = tfP.get()
            P.op('dve', lambda e, rn=rn, bk_=bk_, c=c: e.tensor_scalar(rn[:], sl(bk_, c), 1e-6, None, ALU.add), [], [kbk, krn])
            P.op('act', lambda e, rn=rn: e.activation(rn[:], rn[:], AF.Sqrt), [krn], [krn])
            P.op('dve', lambda e, rn=rn: e.reciprocal(rn[:], rn[:]), [krn], [krn])
            P.op('dve', lambda e, rn=rn, y=y, c=c, par=par, p4=p4: e.tensor_tensor(qk[p4][c][:], y[:], rn[:], ALU.mult), [krn, ky], ['qk%d_%d' % (p4, c)])
            if c < 2:
                P.op('pool', lambda e, rn=rn, y=y, c=c, par=par, p4=p4: e.tensor_tensor(qf[p4][c][:], y[:], rn[:], ALU.mult), [krn, ky], ['qf%d_%d' % (p4, c)])
        yield
        for k in range(16):
            P.mm(pz[:], xT[par][:, k, :], wsl[:, k, 1024:1536], k == 0, k == 15, [kT_, 'wsl'], ['pz'])
        P.op('act', lambda e, par=par, p4=p4: e.activation(zs[p4][:], pz[:], AF.Silu), [], ['pz', 'zs%d' % p4])
        yield
        pba, kpba = pbP.get()
        for k in range(16):
            P.mm(pba[:, 0:8], xT[par][:, k, :], wsl[:, k, 1536:1544], k == 0, k == 15, [kT_, 'wsl'], [kpba])
        beta, nbeta, g_, gc, ngc, xa = (sm4[n_][p4] for n_ in ('beta', 'nbeta', 'g', 'gc', 'ngc', 'xa'))
        ksm = {n_: 'sm_%s%d' % (n_, p4) for n_ in sm4}
        P.op('act', lambda e, beta=beta, pba=pba: e.activation(beta[:], pba[:, 0:4], AF.Sigmoid), [], [kpba, ksm['beta']])
        P.op('dve', lambda e, beta=beta, nbeta=nbeta: e.tensor_scalar(nbeta[:], beta[:], -1.0, None, ALU.mult), [ksm['beta']], [ksm['nbeta']])
        P.op('dve', lambda e, xa=xa, pba=pba: e.tensor_tensor(xa[:], pba[:, 4:8], dtb[:], ALU.add), ['dtb'], [kpba, ksm['xa']])
        P.op('act', lambda e, xa=xa: e.activation(xa[:], xa[:], AF.Exp), [ksm['xa']], [ksm['xa']])
        P.op('dve', lambda e, xa=xa: e.tensor_scalar(xa[:], xa[:], 1.0, None, ALU.add), [ksm['xa']], [ksm['xa']])
        P.op('act', lambda e, xa=xa: e.activation(xa[:], xa[:], AF.Ln), [ksm['xa']], [ksm['xa']])
        P.op('dve', lambda e, xa=xa, g_=g_: e.tensor_tensor(g_[:], xa[:], negA[:], ALU.mult), [ksm['xa'], 'negA'], [ksm['g']])
        P.mm(pba[:, 128:132], uti[:], g_[:], True, True, ['c_uti', ksm['g']], [kpba])
        P.op('dve', lambda e, gc=gc, pba=pba: e.tensor_copy(gc[:], pba[:, 128:132]), [], [kpba, ksm['gc']])
        P.op('dve', lambda e, gc=gc, ngc=ngc: e.tensor_scalar(ngc[:], gc[:], -1.0, None, ALU.mult), [ksm['gc']], [ksm['ngc']])

    def stageU(b_, n):
        par = n % 2
        p4 = n % 4
        row0 = (b_ * NTL + n) * 128
        beta, nbeta, g_, gc, ngc, xa = (sm4[n_][p4] for n_ in ('beta', 'nbeta', 'g', 'gc', 'ngc', 'xa'))
        ksm = {n_: 'sm_%s%d' % (n_, p4) for n_ in sm4}
        dkk = [{n_: 'ded_%s%d_%d' % (n_, par, j) for n_ in ded} for j in range(4)]
        kq_ = ['qk%d_%d' % (p4, j // 2) for j in range(4)]
        kk_ = ['qk%d_%d' % (p4, 2 + j // 2) for j in range(4)]
        qTs = [qk[p4][j // 2] for j in range(4)]
        kTs = [qk[p4][2 + j // 2] for j in range(4)]
        bgr, kbgr = pbU.get()
        for j in range(4):
            gbc, kgbc = tf.get()
            P.op('dve', lambda e, gbc=gbc, g_=g_, j=j: e.tensor_scalar(gbc[:], ones[:], g_[:, j:j + 1], None, ALU.mult), ['c_ones', ksm['g']], [kgbc])
            P.mm(sl(bgr, j), gbc[:], uti[:], True, True, [kgbc, 'c_uti'], [kbgr])
        P.op('act', lambda e, bgr=bgr, par=par, p4=p4: e.activation(E4[par][:], bgr[:], AF.Exp), [], [kbgr] + [dkk[j]['Erow'] for j in range(4)])
        P.op('dve', lambda e, bgr=bgr, par=par, p4=p4: e.tensor_copy(growl4[par][:], bgr[:, 127:512:128]), [], [kbgr] + ['gl%d_%d' % (par, j) for j in range(4)])
        tD4, ktD4 = tf4.get()
        P.op('dve', lambda e, bgr=bgr, tD4=tD4: e.scalar_tensor_tensor(tD4[:], bgr[:], -1.0, c4['negu4'][:], ALU.mult, ALU.add), ['d_negu4'], [kbgr, ktD4])
        tDT4, ktDT4 = tf4.get()
        P.op('dve', lambda e, bgr=bgr, tDT4=tDT4: e.tensor_tensor(tDT4[:], bgr[:], c4['negl4'][:], ALU.add), ['d_negl4'], [kbgr, ktDT4])
        for j in range(4):
            P.op('act', lambda e, tD4=tD4, gc=gc, j=j: e.activation(sl(tD4, j), sl(tD4, j), AF.Exp, bias=gc[:, j:j + 1], scale=1.0), [ktD4, ksm['gc']], [ktD4])
        for j in range(4):
            P.op('act', lambda e, tDT4=tDT4, ngc=ngc, j=j: e.activation(sl(tDT4, j), sl(tDT4, j), AF.Exp, bias=ngc[:, j:j + 1], scale=1.0), [ktDT4, ksm['ngc']], [ktDT4])
        Ds4, kDs4 = tf4.get()
        P.op('pool', lambda e, Ds4=Ds4, tD4=tD4: e.tensor_tensor(Ds4[:], tD4[:], c4['strl4'][:], ALU.mult), [ktD4, 'd_strl4'], [kDs4])
        Dss = [(sl(Ds4, j), kDs4) for j in range(4)]
        tDTs = [(sl(tDT4, j), ktDT4) for j in range(4)]
        yield
        bkk, kbkk = pbU.get()
        for j in range(4):
            P.mm(sl(bkk, j), kTs[j][:], kTs[j][:], True, True, [kk_[j]], [kbkk])
        B4, kB4 = tn4.get()
        for j in range(4):
            Ds, kDs = Dss[j]
            P.op('dve', lambda e, bkk=bkk, B4=B4, nbeta=nbeta, Ds=Ds, j=j: e.scalar_tensor_tensor(sl(B4, j), sl(bkk, j), nbeta[:, j:j + 1], Ds, ALU.mult, ALU.mult), [ksm['nbeta'], kDs], [kbkk, kB4])
        yield
        bm0, kbm0 = pm0b, 'pm0b'
        for j in range(4):
            P.tr(sl(bm0, j), sl(B4, j), identb[:], [kB4, 'd_identb'], [kbm0])
        M4, kM4 = tn4.get()
        P.op('dve', lambda e, M4=M4: e.tensor_copy(M4[:], pm0b[:, 0:512]), [], [kbm0, kM4])
        P4, kP4 = tn4.get()
        P.op('dve', lambda e, P4=P4: e.tensor_tensor(P4[:], pm0b[:, 0:512], identb4[:], ALU.add), ['d_identb4'], [kbm0, kP4])
        yield
        bkq, kbkq = pbU.get()
        for j in range(4):
            P.mm(sl(bkq, j), kTs[j][:], qTs[j][:], True, True, [kk_[j], kq_[j]], [kbkq])
        P.op('dve', lambda e, bkq=bkq, par=par, p4=p4, tDT4=tDT4: e.scalar_tensor_tensor(Q4[par][:], bkq[:], scale, tDT4[:], ALU.mult, ALU.mult), [ktDT4], [kbkq] + [dkk[j]['QKdT'] for j in range(4)])
        yield
        for j in range(4):
            P.tr(pkt[:, j * 128:(j + 1) * 128], kTs[j][:], identb[:], [kk_[j], 'd_identb'], ['pkt'])
        for j in range(4):
            gl_ = growl4[par][:, j:j + 1]
            decf, kdf = t1.get()
            P.op('act', lambda e, decf=decf, gc=gc, gl_=gl_, j=j: e.activation(decf[:], gc[:, j:j + 1], AF.Exp, bias=gl_, scale=-1.0), [ksm['gc'], 'gl%d_%d' % (par, j)], [kdf])
            kdec = ded['kdec'][par][j]
            P.op('dve', lambda e, kdec=kdec, decf=decf, j=j: e.tensor_scalar(kdec[:], pkt[:, j * 128:(j + 1) * 128], decf[:, 0:1], None, ALU.mult), [kdf], ['pkt', dkk[j]['kdec']])
        yield
        bvt, kbvt = pbU.get()
        for j in range(4):
            P.tr(sl(bvt, j), vT[p4][j][:], ident[:], ['vT%d_%d' % (p4, j), 'c_ident'], [kbvt])
        for j in range(4):
            vb = ded['vb'][par][j]
            P.op('dve', lambda e, bvt=bvt, vb=vb, beta=beta, j=j: e.tensor_scalar(vb[:], sl(bvt, j), beta[:, j:j + 1], None, ALU.mult), [ksm['beta']], [kbvt, dkk[j]['vb']])
            c1_ = c1[par][j]; kc1 = 'c1%d_%d' % (par, j)
            P.op('act', lambda e, c1_=c1_, gc=gc, j=j: e.activation(c1_[:], gc[:, j:j + 1], AF.Exp), [ksm['gc']], [kc1])
            P.op('dve', lambda e, c1_=c1_, nbeta=nbeta, j=j: e.tensor_tensor(c1_[:], c1_[:], nbeta[:, j:j + 1], ALU.mult), [kc1, ksm['nbeta']], [kc1])
            qtil = ded['qtil'][par][j]
            Erow = ded['Erow'][par][j]
            P.op('dve', lambda e, par=par, p4=p4, qtil=qtil, j=j, Erow=Erow: e.scalar_tensor_tensor(qtil[:], qf[p4][j // 2][:], scale, Erow[:], ALU.mult, ALU.mult), ['qf%d_%d' % (p4, j // 2), dkk[j]['Erow']], [dkk[j]['qtil']])
        yield
        for lv in range(1, 7):
            if lv < 6:
                bmn, kbmn = pbU.get()
                for j in range(4):
                    P.mm(sl(bmn, j), sl(B4, j), sl(M4, j), True, True, [kB4, kM4], [kbmn])
            bbn, kbbn = pbU.get()
            for j in range(4):
                P.mm(sl(bbn, j), sl(M4, j), sl(B4, j), True, True, [kB4, kM4], [kbbn])
            Bn4, kBn4 = tn4.get()
            P.op('act', lambda e, Bn4=Bn4, bbn=bbn: e.copy(Bn4[:], bbn[:]), [], [kbbn, kBn4])
            if lv < 6:
                Mn4, kMn4 = tn4.get()
                P.op('dve', lambda e, Mn4=Mn4, bmn=bmn: e.tensor_copy(Mn4[:], bmn[:]), [], [kbmn, kMn4])
            else:
                Mn4, kMn4 = None, None
            yield
            bpn, kbpn = pbU.get()
            for j in range(4):
                P.mm(sl(bpn, j), sl(Bn4, j), sl(P4, j), True, True, [kBn4, kP4], [kbpn])
            if lv < 6:
                Pn4, kPn4 = tn4.get()
            else:
                Pn4, kPn4 = TT4[par], 'TT4_%d' % par
            P.op('dve', lambda e, Pn4=Pn4, P4=P4, bpn=bpn: e.tensor_tensor(Pn4[:], bpn[:], P4[:], ALU.add), [kP4], [kbpn, kPn4])
            P4, kP4 = Pn4, kPn4
            B4, kB4 = Bn4, kBn4
            M4, kM4 = Mn4, kMn4
            yield

    def stageS(b_, n, step):
        par = n % 2
        p4 = n % 4
        row0 = (b_ * NTL + n) * 128
        ksm = {n_: 'sm_%s%d' % (n_, p4) for n_ in sm4}
        dkk = [{n_: 'ded_%s%d_%d' % (n_, par, j) for n_ in ded} for j in range(4)]
        kk_ = ['qk%d_%d' % (p4, 2 + j // 2) for j in range(4)]
        kTs = [qk[p4][2 + j // 2] for j in range(4)]
        bks, kbks = pbU.get()
        for j in range(4):
            P.mm(sl(bks, j), kTs[j][:], Sbf[j][:], True, True, [kk_[j], 'Sbf%d' % j], [kbks])
        rr = [tn.get() for j in range(4)]
        for j in range(4):
            P.op('dve', lambda e, par=par, p4=p4, j=j, r=rr[j][0], bks=bks: e.scalar_tensor_tensor(r[:], sl(bks, j), c1[par][j][:, 0:1], ded['vb'][par][j][:], ALU.mult, ALU.add),
                 ['c1%d_%d' % (par, j), dkk[j]['vb']], [kbks, rr[j][1]])
        step()
        bvn, kbvn = pbU.get()
        for j in range(4):
            P.mm(sl(bvn, j), sl(TT4[par], j), rr[j][0][:], True, True, ['TT4_%d' % par, rr[j][1]], [kbvn])
        vn4, kvn4 = tn4.get()
        P.op('act', lambda e, vn4=vn4, bvn=bvn: e.copy(vn4[:], bvn[:]), [], [kbvn, kvn4])
        vn = [(sl(vn4, j), kvn4) for j in range(4)]
        step()
        bo, kbo = pbU.get()
        bst, kbst = pbU.get()
        for j in range(4):
            P.mm(sl(bo, j), ded['qtil'][par][j][:], Sbf[j][:], True, False, [dkk[j]['qtil'], 'Sbf%d' % j], [kbo])
            P.mm(sl(bo, j), ded['QKdT'][par][j][:], vn[j][0], False, True, [dkk[j]['QKdT'], vn[j][1]], [kbo])
            P.mm(sl(bst, j), ded['kdec'][par][j][:], vn[j][0], True, True, [dkk[j]['kdec'], vn[j][1]], [kbst])
        for j in range(4):
            P.op('dve', lambda e, par=par, p4=p4, j=j, bst=bst: e.scalar_tensor_tensor(Sst[j][:], Sst[j][:], ded['Erow'][par][j][:, 127:128], sl(bst, j), ALU.mult, ALU.add),
                 [dkk[j]['Erow']], [kbst, 'S%d' % j])
        step()
        P.op('act', lambda e: e.copy(Sbf4[:], S4[:]), ['S%d' % j for j in range(4)], ['Sbf%d' % j for j in range(4)])
        Os4, kOs4 = tf4.get()
        P.op('dve', lambda e, Os4=Os4, bo=bo: e.tensor_copy(Os4[:], bo[:]), [], [kbo, kOs4])
        Oss = [(sl(Os4, j), kOs4) for j in range(4)]
        for j in range(4):
            Os, kOs = Oss[j]
            ssq, kss = t1.get()
            junk, kj = tf.get()
            P.op('pool', lambda e, ssq=ssq: e.memset(ssq[:], 0.0), [], [kss])
            P.op('act', lambda e, junk=junk, Os=Os, ssq=ssq: e.activation(junk[:], Os, AF.Square, accum_out=ssq[:]), [kOs], [kj, kss])
            P.op('dve', lambda e, ssq=ssq: e.tensor_scalar(ssq[:], ssq[:], 1.0 / 128, 1e-6, ALU.mult, ALU.add), [kss], [kss])
            P.op('act', lambda e, ssq=ssq: e.activation(ssq[:], ssq[:], AF.Sqrt), [kss], [kss])
            P.op('dve', lambda e, ssq=ssq: e.reciprocal(ssq[:], ssq[:]), [kss], [kss])
            P.op('dve', lambda e, Os=Os, ssq=ssq: e.scalar_tensor_tensor(Os, Os, ssq[:, 0:1], ng[:], ALU.mult, ALU.mult), [kss, 'ng'], [kOs])
            P.op('pool', lambda e, Os=Os, j=j, par=par, p4=p4: e.tensor_tensor(outt[par][:, j * 128:(j + 1) * 128], Os, zs[p4][:, j * 128:(j + 1) * 128], ALU.mult),
                 [kOs, 'zs%d' % p4], ['out%d' % par])
        P.ld(out_d[row0:row0 + 128, :], outt[par][:], reads=['out%d' % par])
        if dbg and n == 0 and b_ == 0:
            def dump(name, ap, key, shape, dt=F32):
                dd = nc.dram_tensor(name, shape, dt, kind="ExternalOutput").ap()
                P.ld(dd, ap, reads=[key])
            for c in range(4):
                dump('g_qk%d' % c, qk[p4][c][:], 'qk%d_%d' % (p4, c), [128, 128], BF16)
                dump('g_vT%d' % c, vT[p4][c][:], 'vT%d_%d' % (p4, c), [128, 128])
                dump('g_raw%d' % c, raw[par][c][:], 'raw%d_%d' % (par, c), [128, 131])
            for n_ in ('beta', 'g', 'gc'):
                dump('g_' + n_, sm4[n_][p4][:], ksm[n_], [128, 4])
            for n_ in ded:
                dump('g_' + n_, ded[n_][par][0][:], dkk[0][n_], [128, 128], F32 if n_ in ('Erow', 'vb') else BF16)
            dump('g_zs', zs[p4][:], 'zs%d' % p4, [128, 512])
            dump('g_c1', c1[par][0][:], 'c1%d_0' % par, [128, 1])
            dump('g_r', rr[0][0][:], rr[0][1], [128, 128], BF16)
            dump('g_vn', vn[0][0], vn[0][1], [128, 128], BF16)
            dump('g_Os', Oss[0][0], Oss[0][1], [128, 128])
            dump('g_out', outt[par][:], 'out%d' % par, [128, 512], BF16)


    import itertools
    for b_ in range(NB_):
        for j in range(4):
            P.op('pool', lambda e, j=j: e.memset(Sst[j][:], 0.0), [], ['S%d' % j])
            P.op('pool', lambda e, j=j: e.memset(Sbf[j][:], 0.0), [], ['Sbf%d' % j])
        for c in range(8):
            P.op('pool', lambda e, c=c: e.memset(raw[0][c][:, 0:3], 0.0), [], ['raw0_%d' % c])
        for nn in range(min(2, NTL)):
            for _ in genP(b_, nn):
                pass
        for n0 in range(0, NTL, 2):
            gens = [stageU(b_, n0 + i) for i in range(2) if n0 + i < NTL]
            fill = itertools.chain(*[genP(b_, n0 + 2 + i) for i in range(2) if n0 + 2 + i < NTL])

            def step(fill=fill):
                next(fill, None)
            alive = list(gens)
            while alive:
                for g in list(alive):
                    try:
                        next(g)
                    except StopIteration:
                        alive.remove(g)
                step()
            for i in range(2):
                if n0 + i < NTL:
                    stageS(b_, n0 + i, step)
            for _ in fill:
                pass


def build_dn(NB_, NTL, dbg=False):
    nc = bass.Bass("TRN2", target_bir_lowering=False)
    x_d = nc.dram_tensor("x1", [NB_ * NTL * 128, D], F32, kind="ExternalInput").ap()
    out_d = nc.dram_tensor("out", [NB_ * NTL * 128, 512], BF16, kind="ExternalOutput").ap()
    W = {}
    W['w_sl'] = nc.dram_tensor("w_sl", [D, 1544], F32, kind="ExternalInput").ap()
    W['cw_l'] = nc.dram_tensor("cw_l", [128, 8, 4], F32, kind="ExternalInput").ap()
    W['norm_g'] = nc.dram_tensor("norm_g", [128], F32, kind="ExternalInput").ap()
    W['alog_l'] = nc.dram_tensor("alog_l", [4], F32, kind="ExternalInput").ap()
    W['dtb_l'] = nc.dram_tensor("dtb_l", [4], F32, kind="ExternalInput").ap()
    with ExitStack() as es:
        P = Prog(nc, es)
        cst = load_consts(P, nc, ['c_ident', 'c_uti', 'c_ones', 'c_negu', 'c_negl', 'c_strl'])
        emit_dn(P, nc, es, NB_, NTL, x_d, W, out_d, cst, dbg)
        P.finish()
    return nc


def dn_inputs(core, dn_w_in, dn_conv_w, dn_a_log, dn_dt_bias, dn_norm_g):
    c = core
    qc = slice(2 * c * 128, (2 * c + 2) * 128)
    cols = [dn_w_in[:, qc], dn_w_in[:, 2048 + 2 * c * 128:2048 + (2 * c + 2) * 128],
            dn_w_in[:, 4096 + 4 * c * 128:4096 + (4 * c + 4) * 128],
            dn_w_in[:, 8192 + 4 * c * 128:8192 + (4 * c + 4) * 128],
            dn_w_in[:, 12288 + 4 * c:12288 + 4 * c + 4], dn_w_in[:, 12320 + 4 * c:12320 + 4 * c + 4]]
    w_sl = np.ascontiguousarray(np.concatenate(cols, axis=1))
    cwc = np.concatenate([dn_conv_w[:, qc], dn_conv_w[:, 2048 + 2 * c * 128:2048 + (2 * c + 2) * 128],
                          dn_conv_w[:, 4096 + 4 * c * 128:4096 + (4 * c + 4) * 128]], axis=1)
    cw_l = np.ascontiguousarray(cwc.reshape(4, 8, 128).transpose(2, 1, 0))
    return {'w_sl': w_sl, 'cw_l': cw_l, 'norm_g': np.ascontiguousarray(dn_norm_g),
            'alog_l': np.ascontiguousarray(dn_a_log[4 * c:4 * c + 4]), 'dtb_l': np.ascontiguousarray(dn_dt_bias[4 * c:4 * c + 4])}


def emit_outproj(P, nc, NT, o_d, x_d, w_out, ln_g, ln_b, xm_d, cst):
    ident = cst['c_ident']
    r_d = nc.dram_tensor("op_r", [NT * 128, D], F32, kind="Internal").ap()
    with ExitStack() as ph:
        def sb(shape, dt, name):
            return ph.enter_context(nc.sbuf_tensor(name, list(shape), dt))

        def ps(name, dt=F32, w=512):
            return ph.enter_context(nc.psum_tensor(name, [128, w], dt))
        identb = sb([128, 128], BF16, 'op_identb')
        P.op('dve', lambda e: e.tensor_copy(identb[:], ident[:]), ['c_ident'], ['op_identb'])
        wo = sb([128, 32, 1024], BF16, 'op_wo')
        ob = [sb([128, 4096], BF16, 'op_ob%d' % i) for i in range(2)]
        oT = [sb([128, 32, 128], BF16, 'op_oT%d' % i) for i in range(2)]
        xh = [sb([128, 1024], F32, 'op_xh%d' % i) for i in range(2)]
        ptr = [ps('op_ptr%d' % i, BF16, 1024) for i in range(2)]
        py = [ps('op_py%d' % i) for i in range(4)]
        n4 = 0
        npy = 0
        for ch in range(2):
            P.ld(wo[:], w_out.rearrange("(k p) d -> p k d", p=128)[:, :, ch * 1024:(ch + 1) * 1024], writes=['wo'], eng='pool')
            for t in range(NT):
                b = t % 2
                P.ld(ob[b][:], o_d[t * 128:(t + 1) * 128, :], writes=['ob%d' % b])
                P.ld(xh[b][:], x_d[t * 128:(t + 1) * 128, ch * 1024:(ch + 1) * 1024], writes=['xh%d' % b])
                for q in range(8):
                    pq = ptr[n4 % 2]; kp = 'ptr%d' % (n4 % 2); n4 += 1
                    for j in range(4):
                        k = q * 4 + j
                        P.tr(pq[:, j * 128:(j + 1) * 128], ob[b][:, k * 128:(k + 1) * 128], identb[:], ['ob%d' % b, 'op_identb'], [kp])
                    if q % 2:
                        P.op('act', lambda e, q=q, b=b, pq=pq: e.copy(oT[b][:, q * 4:(q + 1) * 4, :], pq[:, 0:512].rearrange("p (a c) -> p a c", a=4)), [kp], ['oT%d' % b])
                    else:
                        P.op('dve', lambda e, q=q, b=b, pq=pq: e.tensor_copy(oT[b][:, q * 4:(q + 1) * 4, :], pq[:, 0:512].rearrange("p (a c) -> p a c", a=4)), [kp], ['oT%d' % b])
                for cc in range(2):
                    pyy = py[npy % 4]; kpy = 'py%d' % (npy % 4); npy += 1
                    for k in range(32):
                        P.mm(pyy[:], oT[b][:, k, :], wo[:, k, cc * 512:(cc + 1) * 512], k == 0, k == 31, ['oT%d' % b, 'wo'], [kpy])
                    P.op('dve', lambda e, b=b, cc=cc, pyy=pyy: e.scalar_tensor_tensor(xh[b][:, cc * 512:(cc + 1) * 512], xh[b][:, cc * 512:(cc + 1) * 512], ALPHA, pyy[:], ALU.mult, ALU.add),
                         [kpy, 'xh%d' % b], ['xh%d' % b])
                P.ld(r_d[t * 128:(t + 1) * 128, ch * 1024:(ch + 1) * 1024], xh[b][:], reads=['xh%d' % b], writes=['r_d'])
        P.barrier()
        P.flush()
    with ExitStack() as ph:
        def sb(shape, dt, name):
            return ph.enter_context(nc.sbuf_tensor(name, list(shape), dt))
        xt = [sb([128, D], F32, 'op2_xt%d' % i) for i in range(2)]
        ot = [sb([128, D], F32, 'op2_ot%d' % i) for i in range(2)]
        gt = sb([128, D], F32, 'op2_g'); bt = sb([128, D], F32, 'op2_b')
        st = sb([128, 24], F32, 'op2_st'); ag = sb([128, 2], F32, 'op2_ag'); rstd = sb([128, 1], F32, 'op2_rstd')
        P.ld(gt[:], ln_g.partition_broadcast(128), writes=['lng'])
        P.ld(bt[:], ln_b.partition_broadcast(128), writes=['lnb'])
        for t in range(NT):
            b = t % 2
            P.ld(xt[b][:], r_d[t * 128:(t + 1) * 128, :], writes=['xt%d' % b])
            emit_ln(P, xt[b], ot[b], gt, bt, 'op2_', ['xt%d' % b], ['ot%d' % b], st, ag, rstd)
            P.ld(xm_d[t * 128:(t + 1) * 128, :], ot[b][:], reads=['ot%d' % b], writes=['xm_d'])
        P.barrier()
        P.flush()


def _moe_W(nc):
    W = {}
    W['router_w'] = nc.dram_tensor("router_w", [D, NE], F32, kind="ExternalInput").ap()
    W['router_b'] = nc.dram_tensor("router_b", [NE], F32, kind="ExternalInput").ap()
    W['w_gate_up'] = nc.dram_tensor("w_gate_up", [NE, D, 2 * FF], F32, kind="ExternalInput").ap()
    W['bgu_l'] = nc.dram_tensor("bgu_l", [128, NE * 32], F32, kind="ExternalInput").ap()
    W['w_down'] = nc.dram_tensor("w_down", [NE, FF, D], F32, kind="ExternalInput").ap()
    W['b_down'] = nc.dram_tensor("b_down", [NE, D], F32, kind="ExternalInput").ap()
    W['ln_g'] = nc.dram_tensor("ln2_g", [D], F32, kind="ExternalInput").ap()
    W['ln_b'] = nc.dram_tensor("ln2_b", [D], F32, kind="ExternalInput").ap()
    return W


def build_L1(NQ, C):
    nc = bass.Bass("TRN2", target_bir_lowering=False)
    xc = nc.dram_tensor("xc", [(16 + NQ) * 128, D], F32, kind="ExternalInput").ap()
    hv_d = nc.dram_tensor("hv", [128, 1], F32, kind="ExternalInput").ap()
    out_d = nc.dram_tensor("out", [NQ * 128, D], F32, kind="ExternalOutput").ap()
    xm_d = nc.dram_tensor("xmid", [NQ * 128, D], F32, kind="Internal").ap()
    WA = {}
    WA['rel_bias'] = nc.dram_tensor("rel_bias", [32, 24], F32, kind="ExternalInput").ap()
    WA['attn_w_in'] = nc.dram_tensor("attn_w_in", [D, 9216], F32, kind="ExternalInput").ap()
    WA['attn_w_out'] = nc.dram_tensor("attn_w_out", [1024, D], F32, kind="ExternalInput").ap()
    WA['ln_g'] = nc.dram_tensor("ln1_g", [D], F32, kind="ExternalInput").ap()
    WA['ln_b'] = nc.dram_tensor("ln1_b", [D], F32, kind="ExternalInput").ap()
    WM = _moe_W(nc)
    with ExitStack() as es:
        P = Prog(nc, es)
        cst = load_consts(P, nc, ['c_ident', 'c_uts', 'c_ones'])
        with ExitStack() as aes:
            emit_attn(P, nc, aes, NQ, xc, hv_d, WA, xm_d, cst)
        emit_moe(P, nc, es, NQ, C, xm_d, out_d, WM, cst)
        P.finish()
    return nc


def build_L3(NT, C):
    nc = bass.Bass("TRN2", target_bir_lowering=False)
    o_d = nc.dram_tensor("o_in", [NT * 128, 4096], BF16, kind="ExternalInput").ap()
    x_d = nc.dram_tensor("x_res", [NT * 128, D], F32, kind="ExternalInput").ap()
    out_d = nc.dram_tensor("out", [NT * 128, D], F32, kind="ExternalOutput").ap()
    xm_d = nc.dram_tensor("xmid", [NT * 128, D], F32, kind="Internal").ap()
    w_out = nc.dram_tensor("dn_w_out", [4096, D], F32, kind="ExternalInput").ap()
    ln_g = nc.dram_tensor("ln1_g", [D], F32, kind="ExternalInput").ap()
    ln_b = nc.dram_tensor("ln1_b", [D], F32, kind="ExternalInput").ap()
    WM = _moe_W(nc)
    with ExitStack() as es:
        P = Prog(nc, es)
        cst = load_consts(P, nc, ['c_ident', 'c_uts', 'c_ones'])
        emit_outproj(P, nc, NT, o_d, x_d, w_out, ln_g, ln_b, xm_d, cst)
        emit_moe(P, nc, es, NT, C, xm_d, out_d, WM, cst)
        P.finish()
    return nc


def _moe_in(layer, router_w, router_b, w_gate_up, b_gate_up, w_down, b_down, ln2_g, ln2_b):
    bgu_l = np.ascontiguousarray(b_gate_up[layer].reshape(NE, 32, 128).transpose(2, 0, 1).reshape(128, NE * 32))
    return {'router_w': router_w[layer], 'router_b': router_b[layer], 'w_gate_up': w_gate_up[layer], 'bgu_l': bgu_l,
            'w_down': w_down[layer], 'b_down': b_down[layer], 'ln2_g': ln2_g[layer], 'ln2_b': ln2_b[layer]}


NCORES = 8
SEQ = 16384
TPC = 4096
CAP = 640


def kernel(x, rel_bias, attn_w_in, attn_w_out, dn_w_in, dn_conv_w, dn_a_log, dn_dt_bias,
           dn_norm_g, dn_w_out, ln1_g, ln1_b, router_w, router_b, w_gate_up, b_gate_up,
           w_down, b_down, ln2_g, ln2_b):
    f32 = np.float32
    args = [x, rel_bias, attn_w_in, attn_w_out, dn_w_in, dn_conv_w, dn_a_log, dn_dt_bias, dn_norm_g, dn_w_out,
            ln1_g, ln1_b, router_w, router_b, w_gate_up, b_gate_up, w_down, b_down, ln2_g, ln2_b]
    (x, rel_bias, attn_w_in, attn_w_out, dn_w_in, dn_conv_w, dn_a_log, dn_dt_bias, dn_norm_g, dn_w_out,
     ln1_g, ln1_b, router_w, router_b, w_gate_up, b_gate_up, w_down, b_down, ln2_g, ln2_b) = [np.asarray(a, dtype=f32) for a in args]
    cst = consts_np()
    cst.update(dn_consts_np())
    NQ = TPC // 128
    cores = list(range(NCORES))
    c3 = {k: cst[k] for k in ('c_ident', 'c_uts', 'c_ones')}
    com = dict(c3, **attn_static(), rel_bias=rel_bias, attn_w_in=attn_w_in[0], attn_w_out=attn_w_out[0],
               ln1_g=ln1_g[0], ln1_b=ln1_b[0],
               **_moe_in(0, router_w, router_b, w_gate_up, b_gate_up, w_down, b_down, ln2_g, ln2_b))
    ims = []
    for c in cores:
        b, sg = c // 4, c % 4
        t0 = sg * TPC
        if sg == 0:
            xc = np.concatenate([np.zeros((HALO, D), f32), x[b, 0:TPC]], axis=0)
            hv = np.zeros((128, 1), f32)
        else:
            xc = np.ascontiguousarray(x[b, t0 - HALO:t0 + TPC])
            hv = np.ones((128, 1), f32)
        ims.append(dict(com, xc=xc, hv=hv))
    nc1 = build_L1(NQ, CAP)
    r1 = run_bass_kernel_spmd(nc1, ims, core_ids=cores)
    x1 = np.concatenate([np.asarray(r1.results[c]['out']) for c in cores], axis=0)
    del ims, r1
    c6 = {k: cst[k] for k in ('c_ident', 'c_uti', 'c_ones', 'c_negu', 'c_negl', 'c_strl')}
    ims = [dict(c6, x1=x1, **dn_inputs(c, dn_w_in[0], dn_conv_w[0], dn_a_log[0], dn_dt_bias[0], dn_norm_g[0])) for c in cores]
    nc2 = build_dn(2, SEQ // 128)
    r2 = run_bass_kernel_spmd(nc2, ims, core_ids=cores)
    o_full = np.concatenate([np.asarray(r2.results[c]['out']) for c in cores], axis=1)
    del ims, r2
    com = dict(c3, dn_w_out=dn_w_out[0], ln1_g=ln1_g[1], ln1_b=ln1_b[1],
               **_moe_in(1, router_w, router_b, w_gate_up, b_gate_up, w_down, b_down, ln2_g, ln2_b))
    ims = [dict(com, o_in=np.ascontiguousarray(o_full[c * TPC:(c + 1) * TPC]), x_res=np.ascontiguousarray(x1[c * TPC:(c + 1) * TPC])) for c in cores]
    nc3 = build_L3(NQ, CAP)
    r3 = run_bass_kernel_spmd(nc3, ims, core_ids=cores)
    out = np.concatenate([np.asarray(r3.results[c]['out']) for c in cores], axis=0)
    return out.reshape(2, SEQ, D).astype(f32)
```

```python
import numpy as np
import concourse.bass as bass
import concourse.mybir as mybir
from contextlib import ExitStack
from concourse.bass_utils import run_bass_kernel_spmd

F32 = mybir.dt.float32
BF16 = mybir.dt.bfloat16
I32 = mybir.dt.int32
ALU = mybir.AluOpType
AF = mybir.ActivationFunctionType
AX = mybir.AxisListType


class Prog:
    ENG = ('pe', 'dve', 'act', 'pool', 'sp')
    EPOCH = 20000
    ND = 40
    NDI = 4

    def __init__(self, nc, es):
        self.nc, self.es = nc, es
        self.q = {e: [] for e in self.ENG}
        self.cnt = {e: 0 for e in self.ENG}
        self.epoch = {e: 0 for e in self.ENG}
        self.sems = {}
        self.seen = {e: {} for e in self.ENG}
        self.lastw = {}
        self.reads = {}
        self.dcnt = [0] * (self.ND + self.NDI)
        self.dnext = 0
        self.inext = 0
        self.nsem = 0
        self.ntile = 0

    def sem(self, key):
        if key not in self.sems:
            self.nsem += 1
            self.sems[key] = self.es.enter_context(self.nc.semaphore('s%d' % self.nsem))
        return self.sems[key]

    def sb(self, shape, dt, name=None):
        self.ntile += 1
        return self.es.enter_context(self.nc.sbuf_tensor(name or 't%d' % self.ntile, list(shape), dt))

    def ps(self, shape, dt, name=None):
        self.ntile += 1
        return self.es.enter_context(self.nc.psum_tensor(name or 'p%d' % self.ntile, list(shape), dt))

    def _wait(self, eng, tok):
        key, val = tok
        if eng == 'pe' and key[0] == 'e' and key[1] == 'pe':
            return
        if self.seen[eng].get(key, 0) >= val:
            return
        self.seen[eng][key] = val
        s = self.sem(key)
        self.q[eng].append(lambda e, s=s, val=val: e.wait_ge(s, val))

    def _deps(self, eng, reads, writes):
        for k in reads:
            t = self.lastw.get(k)
            if t:
                self._wait(eng, t)
        for k in writes:
            t = self.lastw.get(k)
            if t:
                self._wait(eng, t)
            for t in self.reads.get(k, ()):
                self._wait(eng, t)

    def _commit(self, tok, reads, writes):
        for k in reads:
            lst = self.reads.setdefault(k, [])
            for i, t in enumerate(lst):
                if t[0] == tok[0]:
                    lst[i] = tok
                    break
            else:
                lst.append(tok)
        for k in writes:
            self.lastw[k] = tok
            self.reads[k] = []

    def op(self, eng, fn, reads=(), writes=()):
        self._deps(eng, reads, writes)
        self.cnt[eng] += 1
        if self.cnt[eng] > self.EPOCH:
            self.epoch[eng] += 1
            self.cnt[eng] = 1
        key = ('e', eng, self.epoch[eng])
        s = self.sem(key)
        self.q[eng].append(lambda e, s=s, fn=fn: fn(e).then_inc(s, 1))
        self._commit((key, self.cnt[eng]), reads, writes)

    def dma(self, eng, fn, reads=(), writes=(), ind=False):
        if ind:
            i = self.ND + self.inext
            self.inext = (self.inext + 1) % self.NDI
        else:
            i = self.dnext
            self.dnext = (i + 1) % self.ND
        key = ('d', i)
        if self.dcnt[i]:
            self._wait(eng, (key, self.dcnt[i]))
        self._deps(eng, reads, writes)
        self.dcnt[i] += 16
        s = self.sem(key)
        self.q[eng].append(lambda e, s=s, fn=fn: fn(e).then_inc(s, 16))
        self._commit((key, self.dcnt[i]), reads, writes)

    def finish(self):
        for i in range(self.ND + self.NDI):
            if self.dcnt[i]:
                self._wait('sp', (('d', i), self.dcnt[i]))
        for e in ('pe', 'dve', 'act', 'pool'):
            if self.cnt[e]:
                self._wait('sp', (('e', e, self.epoch[e]), self.cnt[e]))
        self.flush()

    def flush(self):
        q = self.q
        self.q = {e: [] for e in self.ENG}
        with self.nc.Block() as block:
            @block.tensor
            def _(e):
                for f in q['pe']:
                    f(e)

            @block.vector
            def _(e):
                for f in q['dve']:
                    f(e)

            @block.scalar
            def _(e):
                for f in q['act']:
                    f(e)

            @block.gpsimd
            def _(e):
                for f in q['pool']:
                    f(e)

            @block.sync
            def _(e):
                for f in q['sp']:
                    f(e)

    def mm(self, out, lhsT, rhs, start=True, stop=True, reads=(), writes=()):
        self.op('pe', lambda e: e.matmul(out, lhsT, rhs, start=start, stop=stop), reads, writes)

    def tr(self, out, in_, ident, reads=(), writes=()):
        self.op('pe', lambda e: e.transpose(out, in_, ident), reads, writes)

    def ld(self, out, in_, reads=(), writes=(), eng='sp'):
        self.dma(eng, lambda e: e.dma_start(out=out, in_=in_), reads, writes)

    def barrier(self):
        toks = []
        for i in range(self.ND + self.NDI):
            if self.dcnt[i]:
                toks.append((('d', i), self.dcnt[i]))
        for e in ('pe', 'dve', 'act', 'pool'):
            for ep in range(self.epoch[e] + 1):
                c = self.cnt[e] if ep == self.epoch[e] else self.EPOCH
                if c:
                    toks.append((('e', e, ep), c))
        for eng in self.ENG:
            for t in toks:
                self._wait(eng, t)
        self.lastw = {}
        self.reads = {}


D = 2048
ALPHA = 2.0 ** 0.5
LN_EPS = 1e-5
NE = 32
FF = 2048


def consts_np():
    c = {}
    c['c_ident'] = np.eye(128, dtype=np.float32)
    c['c_uts'] = np.triu(np.ones((128, 128), np.float32), 1)
    c['c_uti'] = np.triu(np.ones((128, 128), np.float32), 0)
    c['c_ones'] = np.ones((128, 128), np.float32)
    return c


def load_consts(P, nc, names):
    out = {}
    for n in names:
        d = nc.dram_tensor(n, [128, 128], F32, kind="ExternalInput").ap()
        t = P.sb([128, 128], F32, name='sb_' + n)
        P.ld(t[:], d, writes=[n])
        out[n] = t
    return out


def emit_ln(P, src, dst, gt, bt, tmpk, keys_r, keys_w, st, ag, rstd):
    for i in range(4):
        P.op('dve', lambda e, i=i: e.bn_stats(st[:, i * 6:(i + 1) * 6], src[:, i * 512:(i + 1) * 512]), keys_r, [tmpk + 'st'])
    P.op('dve', lambda e: e.bn_aggr(ag[:], st[:]), [tmpk + 'st'], [tmpk + 'ag'])
    P.op('dve', lambda e: e.tensor_scalar(rstd[:], ag[:, 1:2], LN_EPS, None, ALU.add), [tmpk + 'ag'], [tmpk + 'rs'])
    P.op('act', lambda e: e.activation(rstd[:], rstd[:], AF.Sqrt), [tmpk + 'rs'], [tmpk + 'rs'])
    P.op('dve', lambda e: e.reciprocal(rstd[:], rstd[:]), [tmpk + 'rs'], [tmpk + 'rs'])
    P.op('dve', lambda e: e.tensor_scalar(dst[:], src[:], ag[:, 0:1], rstd[:, 0:1], ALU.subtract, ALU.mult),
         list(keys_r) + [tmpk + 'ag', tmpk + 'rs'], keys_w)
    P.op('pool', lambda e: e.tensor_tensor(dst[:], dst[:], gt[:], ALU.mult), list(keys_w) + ['lng'], keys_w)
    P.op('pool', lambda e: e.tensor_tensor(dst[:], dst[:], bt[:], ALU.add), list(keys_w) + ['lnb'], keys_w)


def emit_moe(P, nc, es, NT, C, x_in, out_d, W, cst, dbg=False):
    NSLOT = NE * C
    NB = C // 128
    H = C // 2
    BIG = float(NSLOT + 4096)
    kind = "ExternalOutput" if dbg else "Internal"
    xg = nc.dram_tensor("moe_xg", [NSLOT + 1, D], BF16, kind=kind).ap()
    yd = nc.dram_tensor("moe_y", [NSLOT + 1, D], F32, kind=kind).ap()
    if dbg:
        d_sl = nc.dram_tensor("dbg_slots", [128, NT * 4], I32, kind="ExternalOutput").ap()
        d_ga = nc.dram_tensor("dbg_gates", [128, NT * 4], F32, kind="ExternalOutput").ap()
    ident, uts, ones = cst['c_ident'], cst['c_uts'], cst['c_ones']
    slots_all = P.sb([128, NT, 4], I32, name='slots_all')
    gates_all = P.sb([128, NT, 4], F32, name='gates_all')
    identb = P.sb([128, 128], BF16, name='identb')
    P.op('dve', lambda e: e.tensor_copy(identb[:], ident[:]), ['c_ident'], ['identb'])
    scat_keys = []
    with ExitStack() as ph:
        def sb(shape, dt, name):
            return ph.enter_context(nc.sbuf_tensor(name, list(shape), dt))

        def ps(name):
            return ph.enter_context(nc.psum_tensor(name, [128, 512], F32))
        xt = [sb([128, D], F32, 'r_xt%d' % i) for i in range(2)]
        xb = [sb([128, D], BF16, 'r_xb%d' % i) for i in range(2)]
        xT = sb([128, 16, 128], F32, 'r_xT')
        rw = sb([128, 16, NE], F32, 'r_rw')
        rb = sb([128, NE], F32, 'r_rb')
        eoff = sb([128, NE], F32, 'r_eoff')
        base = sb([128, NE], F32, 'r_base')
        zt = sb([128, D], F32, 'r_zero')
        pt = [ps('r_pt%d' % i) for i in range(2)]
        pl = ps('r_pl'); pr = ps('r_pr'); pb = ps('r_pb')
        sm = {n: sb([128, NE], F32, 'r_' + n) for n in ('lg', 'mask', 'ex', 'G', 'rank', 'ov', 'slot', 'v', 'oh')}
        t8 = sb([128, 8], F32, 'r_t8'); v8 = sb([128, 8], F32, 'r_v8')
        s1 = {n: sb([128, 1], F32, 'r_' + n) for n in ('negm', 'ssum', 'rs')}
        slotf = sb([128, 4], F32, 'r_slotf')
        P.ld(rw[:], W['router_w'].rearrange("(k p) e -> p k e", p=128), writes=['rw'])
        P.ld(rb[:], W['router_b'].partition_broadcast(128), writes=['rb'])
        P.op('pool', lambda e: e.iota(eoff[:], [[C, NE]], base=0, channel_multiplier=0, allow_small_or_imprecise_dtypes=True), [], ['eoff'])
        P.op('pool', lambda e: e.memset(base[:], 0.0), [], ['base'])
        P.op('pool', lambda e: e.memset(zt[:], 0.0), [], ['zt'])
        P.ld(yd[NSLOT:NSLOT + 1, :], zt[0:1, :], reads=['zt'], writes=['ydummy'])
        for t in range(NT):
            b = t % 2
            kx, kb = 'xt%d' % b, 'xb%d' % b
            P.ld(xt[b][:], x_in[t * 128:(t + 1) * 128, :], writes=[kx])
            P.op('act', lambda e, b=b: e.copy(xb[b][:], xt[b][:]), [kx], [kb])
            for q in range(4):
                pq = pt[q % 2]
                kp = 'pt%d' % (q % 2)
                for j in range(4):
                    k = q * 4 + j
                    P.tr(pq[:, j * 128:(j + 1) * 128], xt[b][:, k * 128:(k + 1) * 128], ident[:], [kx, 'c_ident'], [kp])
                eng = 'dve' if q % 2 == 0 else 'act'
                if eng == 'dve':
                    P.op('dve', lambda e, q=q, pq=pq: e.tensor_copy(xT[:, q * 4:(q + 1) * 4, :], pq[:].rearrange("p (a b) -> p a b", a=4)), [kp], ['xT%d' % q])
                else:
                    P.op('act', lambda e, q=q, pq=pq: e.copy(xT[:, q * 4:(q + 1) * 4, :], pq[:].rearrange("p (a b) -> p a b", a=4)), [kp], ['xT%d' % q])
            for k in range(16):
                P.mm(pl[:, 0:NE], xT[:, k, :], rw[:, k, :], k == 0, k == 15, ['xT%d' % (k // 4), 'rw'], ['pl'])
            lg, mask, ex, G, rank, ov, slot, v, oh = (sm[n] for n in ('lg', 'mask', 'ex', 'G', 'rank', 'ov', 'slot', 'v', 'oh'))
            P.op('dve', lambda e: e.tensor_tensor(lg[:], pl[:, 0:NE], rb[:], ALU.add), ['pl', 'rb'], ['lg'])
            P.op('dve', lambda e: e.max(t8[:], lg[:]), ['lg'], ['t8'])
            P.op('dve', lambda e: e.tensor_scalar(mask[:], lg[:], t8[:, 3:4], None, ALU.is_ge), ['lg', 't8'], ['mask'])
            P.op('dve', lambda e: e.tensor_scalar(s1['negm'][:], t8[:, 0:1], -1.0, None, ALU.mult), ['t8'], ['negm'])
            P.op('act', lambda e: e.activation(ex[:], lg[:], AF.Exp, bias=s1['negm'][:, 0:1], scale=1.0), ['lg', 'negm'], ['ex'])
            P.op('dve', lambda e: e.tensor_tensor(ex[:], ex[:], mask[:], ALU.mult), ['ex', 'mask'], ['ex'])
            P.op('dve', lambda e: e.reduce_sum(s1['ssum'][:], ex[:], AX.X), ['ex'], ['ssum'])
            P.op('dve', lambda e: e.reciprocal(s1['rs'][:], s1['ssum'][:]), ['ssum'], ['rs'])
            P.op('dve', lambda e: e.tensor_scalar(G[:], ex[:], s1['rs'][:, 0:1], None, ALU.mult), ['ex', 'rs'], ['G'])
            P.mm(pr[:, 0:NE], uts[:], mask[:], True, True, ['c_uts', 'mask'], ['pr'])
            P.mm(pb[:, 0:NE], ones[:], mask[:], True, True, ['c_ones', 'mask'], ['pb'])
            P.op('dve', lambda e: e.tensor_tensor(rank[:], pr[:, 0:NE], base[:], ALU.add), ['pr', 'base'], ['rank'])
            P.op('dve', lambda e: e.tensor_tensor(base[:], pb[:, 0:NE], base[:], ALU.add), ['pb', 'base', 'rank'], ['base'])
            P.op('dve', lambda e: e.tensor_scalar(ov[:], rank[:], float(C), None, ALU.is_lt), ['rank'], ['ov'])
            P.op('dve', lambda e: e.tensor_tensor(slot[:], rank[:], eoff[:], ALU.add), ['rank', 'eoff'], ['slot'])
            P.op('dve', lambda e: e.tensor_scalar(slot[:], slot[:], float(-NSLOT), None, ALU.add), ['slot'], ['slot'])
            P.op('dve', lambda e: e.tensor_tensor(slot[:], slot[:], ov[:], ALU.mult), ['slot', 'ov'], ['slot'])
            P.op('dve', lambda e: e.tensor_scalar(slot[:], slot[:], float(NSLOT), None, ALU.add), ['slot'], ['slot'])
            P.op('dve', lambda e: e.tensor_tensor(G[:], G[:], ov[:], ALU.mult), ['G', 'ov'], ['G'])
            P.op('dve', lambda e: e.tensor_scalar(v[:], slot[:], -1.0, BIG, ALU.mult, ALU.add), ['slot'], ['v'])
            P.op('dve', lambda e: e.tensor_tensor(v[:], v[:], mask[:], ALU.mult), ['v', 'mask'], ['v'])
            P.op('dve', lambda e: e.max(v8[:], v[:]), ['v'], ['v8'])
            P.op('dve', lambda e: e.tensor_scalar(slotf[:], v8[:, 0:4], -1.0, BIG, ALU.mult, ALU.add), ['v8'], ['slotf'])
            ks = 'slots%d' % t
            P.op('dve', lambda e, t=t: e.tensor_copy(slots_all[:, t, :], slotf[:]), ['slotf'], [ks])
            for c in range(4):
                P.op('dve', lambda e, c=c: e.tensor_scalar(oh[:], v[:], v8[:, c:c + 1], None, ALU.is_equal), ['v', 'v8'], ['oh'])
                P.op('dve', lambda e: e.tensor_tensor(oh[:], oh[:], G[:], ALU.mult), ['oh', 'G'], ['oh'])
                P.op('dve', lambda e, t=t, c=c: e.reduce_sum(gates_all[:, t, c:c + 1], oh[:], AX.X), ['oh'], ['gates%d_%d' % (t, c)])
            for c in range(4):
                sk = 'scat%d_%d' % (t, c)
                scat_keys.append(sk)
                P.dma('pool', lambda e, t=t, c=c, b=b: e.indirect_dma_start(
                    out=xg, out_offset=bass.IndirectOffsetOnAxis(ap=slots_all[:, t, c:c + 1], axis=0),
                    in_=xb[b][:], in_offset=None), [kb, ks], [sk], ind=True)
        if dbg:
            d_lg = nc.dram_tensor("dbg_lg", [128, NE], F32, kind="ExternalOutput").ap()
            d_xT = nc.dram_tensor("dbg_xT", [128, 16 * 128], F32, kind="ExternalOutput").ap()
            P.barrier()
            P.ld(d_lg, sm['lg'][:])
            P.ld(d_xT, xT[:].rearrange("p a b -> p (a b)"))
            P.ld(d_sl, slots_all[:].rearrange("p t c -> p (t c)"))
            P.ld(d_ga, gates_all[:].rearrange("p t c -> p (t c)"))
        P.barrier()
        P.flush()
    with ExitStack() as ph:
        def sb(shape, dt, name):
            return ph.enter_context(nc.sbuf_tensor(name, list(shape), dt))

        def ps(name, dt=F32, w=512):
            return ph.enter_context(nc.psum_tensor(name, [128, w], dt))
        xgt = sb([128, NB, D], BF16, 'e_xgt')
        xT = sb([128, 16, C], BF16, 'e_xT')
        wgu = [sb([128, 16, 512], BF16, 'e_wgu%d' % i) for i in range(2)]
        wd = [sb([128, 16, 512], BF16, 'e_wd%d' % i) for i in range(2)]
        actT = sb([128, 16, C], BF16, 'e_actT')
        bgu = sb([128, NE * 32], F32, 'e_bgu')
        bd = sb([128, D], F32, 'e_bd')
        gl = [sb([128, H], F32, 'e_gl%d' % i) for i in range(2)]
        sg = [sb([128, H], F32, 'e_sg%d' % i) for i in range(2)]
        ln = [sb([128, H], F32, 'e_ln%d' % i) for i in range(2)]
        ysb = [sb([128, 512], F32, 'e_ysb%d' % i) for i in range(2)]
        ptr = [ps('e_ptr%d' % i, BF16, 1024) for i in range(2)]
        pg = [ps('e_pg%d' % i) for i in range(2)]
        pn = [ps('e_pn%d' % i) for i in range(2)]
        py = [ps('e_py%d' % i) for i in range(2)]
        P.ld(bgu[:], W['bgu_l'], writes=['bgu'])
        nw = 0
        nd = 0
        ny = 0
        nel = 0
        for ex_ in range(NE):
            P.ld(xgt[:], xg[ex_ * C:(ex_ + 1) * C, :].rearrange("(b p) d -> p b d", p=128), writes=['xgt'])
            P.ld(bd[:], W['b_down'][ex_, :].partition_broadcast(128), writes=['bd'])
            n4 = 0
            for bk in range(NB):
                for q in range(4):
                    pq = ptr[n4 % 2]; kp = 'ptr%d' % (n4 % 2)
                    for j in range(4):
                        k = q * 4 + j
                        P.tr(pq[:, j * 128:(j + 1) * 128], xgt[:, bk, k * 128:(k + 1) * 128], identb[:], ['xgt', 'identb'], [kp])
                    if n4 % 2 == 0:
                        P.op('dve', lambda e, q=q, bk=bk, pq=pq: e.tensor_copy(xT[:, q * 4:(q + 1) * 4, bk * 128:(bk + 1) * 128], pq[:, 0:512].rearrange("p (a b) -> p a b", a=4)), [kp], ['xT'])
                    else:
                        P.op('act', lambda e, q=q, bk=bk, pq=pq: e.copy(xT[:, q * 4:(q + 1) * 4, bk * 128:(bk + 1) * 128], pq[:, 0:512].rearrange("p (a b) -> p a b", a=4)), [kp], ['xT'])
                    n4 += 1
            for g in range(8):
                wb_ = wgu[nw % 2]; kw = 'wgu%d' % (nw % 2); nw += 1
                wv = W['w_gate_up'][ex_].rearrange("(k p) f -> p k f", p=128)
                P.ld(wb_[:, :, 0:256], wv[:, :, g * 256:(g + 1) * 256], writes=[kw + 'a'], eng='pool')
                P.ld(wb_[:, :, 256:512], wv[:, :, FF + g * 256:FF + (g + 1) * 256], writes=[kw + 'b'], eng='pool')
                for j in range(2):
                    fj = g * 2 + j
                    for s in range(2):
                        i2 = nel % 2; nel += 1
                        kg, kn = 'pg%d' % i2, 'pn%d' % i2
                        for k in range(16):
                            P.mm(pg[i2][:, 0:H], wb_[:, k, j * 128:(j + 1) * 128], xT[:, k, s * H:(s + 1) * H], k == 0, k == 15, [kw + 'a', 'xT'], [kg])
                        for k in range(16):
                            P.mm(pn[i2][:, 0:H], wb_[:, k, 256 + j * 128:256 + (j + 1) * 128], xT[:, k, s * H:(s + 1) * H], k == 0, k == 15, [kw + 'b', 'xT'], [kn])
                        cg = ex_ * 32 + fj
                        cl = ex_ * 32 + 16 + fj
                        P.op('dve', lambda e, i2=i2, cg=cg: e.tensor_scalar(gl[i2][:], pg[i2][:, 0:H], bgu[:, cg:cg + 1], 7.0, ALU.add, ALU.min), [kg, 'bgu'], ['gl%d' % i2])
                        P.op('act', lambda e, i2=i2: e.activation(sg[i2][:], gl[i2][:], AF.Sigmoid, scale=1.702), ['gl%d' % i2], ['sg%d' % i2])
                        P.op('dve', lambda e, i2=i2, cl=cl: e.tensor_scalar(ln[i2][:], pn[i2][:, 0:H], bgu[:, cl:cl + 1], 7.0, ALU.add, ALU.min), [kn, 'bgu'], ['ln%d' % i2])
                        P.op('pool', lambda e, i2=i2: e.tensor_scalar(ln[i2][:], ln[i2][:], -7.0, 1.0, ALU.max, ALU.add), ['ln%d' % i2], ['ln%d' % i2])
                        P.op('pool', lambda e, i2=i2: e.tensor_tensor(gl[i2][:], gl[i2][:], sg[i2][:], ALU.mult), ['gl%d' % i2, 'sg%d' % i2], ['gl%d' % i2])
                        P.op('dve', lambda e, i2=i2, fj=fj, s=s: e.tensor_tensor(actT[:, fj, s * H:(s + 1) * H], gl[i2][:], ln[i2][:], ALU.mult), ['gl%d' % i2, 'ln%d' % i2], ['actT'])
            for c in range(4):
                wb_ = wd[nd % 2]; kw = 'wd%d' % (nd % 2); nd += 1
                P.ld(wb_[:], W['w_down'][ex_].rearrange("(k p) d -> p k d", p=128)[:, :, c * 512:(c + 1) * 512], writes=[kw], eng='pool')
                for bk in range(NB):
                    i2 = ny % 2; ny += 1
                    for k in range(16):
                        P.mm(py[i2][:], actT[:, k, bk * 128:(bk + 1) * 128], wb_[:, k, :], k == 0, k == 15, ['actT', kw], ['py%d' % i2])
                    P.op('dve', lambda e, i2=i2, c=c: e.tensor_tensor(ysb[i2][:], py[i2][:], bd[:, c * 512:(c + 1) * 512], ALU.add), ['py%d' % i2, 'bd'], ['ysb%d' % i2])
                    P.ld(yd[ex_ * C + bk * 128:ex_ * C + (bk + 1) * 128, c * 512:(c + 1) * 512], ysb[i2][:], reads=['ysb%d' % i2], writes=['yd'], eng='sp')
        P.barrier()
        P.flush()
    with ExitStack() as ph:
        def sb(shape, dt, name):
            return ph.enter_context(nc.sbuf_tensor(name, list(shape), dt))
        xt = [sb([128, D], F32, 'c_xt%d' % i) for i in range(2)]
        yg = [sb([128, D], F32, 'c_yg%d' % i) for i in range(4)]
        ot = [sb([128, D], F32, 'c_ot%d' % i) for i in range(2)]
        gt = sb([128, D], F32, 'c_g'); bt = sb([128, D], F32, 'c_b')
        st = sb([128, 24], F32, 'c_st'); ag = sb([128, 2], F32, 'c_ag'); rstd = sb([128, 1], F32, 'c_rstd')
        P.ld(gt[:], W['ln_g'].partition_broadcast(128), writes=['lng'])
        P.ld(bt[:], W['ln_b'].partition_broadcast(128), writes=['lnb'])
        for t in range(NT):
            b = t % 2
            kx = 'cxt%d' % b
            P.ld(xt[b][:], x_in[t * 128:(t + 1) * 128, :], writes=[kx])
            for c in range(4):
                P.dma('pool', lambda e, t=t, c=c: e.indirect_dma_start(
                    out=yg[c][:], out_offset=None, in_=yd,
                    in_offset=bass.IndirectOffsetOnAxis(ap=slots_all[:, t, c:c + 1], axis=0),
                    ), [], ['yg%d' % c], ind=True)
            P.op('act', lambda e, b=b: e.mul(xt[b][:], xt[b][:], ALPHA), [kx], [kx])
            for c in range(4):
                P.op('dve', lambda e, b=b, c=c, t=t: e.scalar_tensor_tensor(xt[b][:], yg[c][:], gates_all[:, t, c:c + 1], xt[b][:], ALU.mult, ALU.add), ['yg%d' % c, kx], [kx])
            ko = 'cot%d' % b
            emit_ln(P, xt[b], ot[b], gt, bt, 'c_', [kx], [ko], st, ag, rstd)
            P.ld(out_d[t * 128:(t + 1) * 128, :], ot[b][:], reads=[ko])
        P.barrier()
        P.flush()


def build_moe(NT, C, dbg=False):
    nc = bass.Bass("TRN2", target_bir_lowering=False)
    x_in = nc.dram_tensor("x_in", [NT * 128, D], F32, kind="ExternalInput").ap()
    out_d = nc.dram_tensor("out", [NT * 128, D], F32, kind="ExternalOutput").ap()
    W = {}
    W['router_w'] = nc.dram_tensor("router_w", [D, NE], F32, kind="ExternalInput").ap()
    W['router_b'] = nc.dram_tensor("router_b", [NE], F32, kind="ExternalInput").ap()
    W['w_gate_up'] = nc.dram_tensor("w_gate_up", [NE, D, 2 * FF], F32, kind="ExternalInput").ap()
    W['bgu_l'] = nc.dram_tensor("bgu_l", [128, NE * 32], F32, kind="ExternalInput").ap()
    W['w_down'] = nc.dram_tensor("w_down", [NE, FF, D], F32, kind="ExternalInput").ap()
    W['b_down'] = nc.dram_tensor("b_down", [NE, D], F32, kind="ExternalInput").ap()
    W['ln_g'] = nc.dram_tensor("ln_g", [D], F32, kind="ExternalInput").ap()
    W['ln_b'] = nc.dram_tensor("ln_b", [D], F32, kind="ExternalInput").ap()
    with ExitStack() as es:
        P = Prog(nc, es)
        cst = load_consts(P, nc, ['c_ident', 'c_uts', 'c_ones'])
        emit_moe(P, nc, es, NT, C, x_in, out_d, W, cst, dbg)
        P.finish()
    return nc


def moe_inputs(x_sh, router_w, router_b, w_gate_up, b_gate_up, w_down, b_down, ln_g, ln_b):
    c = consts_np()
    bgu_l = np.ascontiguousarray(b_gate_up.reshape(NE, 32, 128).transpose(2, 0, 1).reshape(128, NE * 32))
    com = {'router_w': router_w, 'router_b': router_b, 'w_gate_up': w_gate_up, 'bgu_l': bgu_l,
           'w_down': w_down, 'b_down': b_down, 'ln_g': ln_g, 'ln_b': ln_b,
           'c_ident': c['c_ident'], 'c_uts': c['c_uts'], 'c_ones': c['c_ones']}
    return [dict(com, x_in=np.ascontiguousarray(xs)) for xs in x_sh]


GROUPS = ((128, 1), (512, 4), (2048, 16))
NEG = -30000.0
HALO = 2048


def attn_static():
    import math
    out = {}
    for g, (w, d) in enumerate(GROUPS):
        L = w + 256
        S = np.zeros((33, L), np.float32)
        for n in range(L):
            dl = n - 127
            if dl >= 0 and dl % d == 0 and dl <= w:
                if dl < 16:
                    bk = dl
                else:
                    v = np.log(np.float32(max(dl, 1)) / np.float32(16)) / np.float32(math.log(2048 / 16)) * np.float32(16)
                    bk = min(16 + int(np.float32(v).astype(np.int32)), 31)
                S[bk, n] = 1.0
            else:
                S[32, n] = NEG
        out['c_S%d' % g] = S
    return out


def emit_attn(P, nc, es, NQ, xc, hv_d, W, out_d, cst):
    NTT = 16 + NQ
    NTOK = NTT * 128
    ident = cst['c_ident']
    xT_d = nc.dram_tensor("a_xT", [16, 128, NTOK], BF16, kind="Internal").ap()
    qT_d = nc.dram_tensor("a_qT", [3, 8, 128, NQ * 128], BF16, kind="Internal").ap()
    kT_d = nc.dram_tensor("a_kT", [3, 8, 128, NTOK], BF16, kind="Internal").ap()
    v_d = nc.dram_tensor("a_v", [3, 8, NTOK, 128], BF16, kind="Internal").ap()
    Zs = [[nc.dram_tensor("a_Z%d_%d" % (g, h), [128, GROUPS[g][0] + 256], F32, kind="Internal") for h in range(8)] for g in range(3)]
    identb = es.enter_context(nc.sbuf_tensor('a_identb', [128, 128], BF16))
    P.op('dve', lambda e: e.tensor_copy(identb[:], ident[:]), ['c_ident'], ['a_identb'])
    o_all = es.enter_context(nc.sbuf_tensor('a_oall', [128, NQ, 1024], BF16))
    with ExitStack() as ph:
        def sb(shape, dt, name):
            return ph.enter_context(nc.sbuf_tensor(name, list(shape), dt))
        rbT = sb([33, 24], F32, 'a0_rbT')
        lb = sb([33, 128], F32, 'a0_lb')
        Ssb = [sb([33, GROUPS[g][0] + 256], F32, 'a0_S%d' % g) for g in range(3)]
        fb = [sb([128, 2304], F32, 'a0_fb%d' % i) for i in range(2)]
        pz = [ph.enter_context(nc.psum_tensor('a0_pz%d' % i, [128, 512], F32)) for i in range(2)]
        P.op('pool', lambda e: e.memset(rbT[:], 1.0), [], ['rbT'])
        P.ld(rbT[0:32, :], W['rel_bias'], writes=['rbT'])
        for g in range(3):
            Sd = nc.dram_tensor('c_S%d' % g, [33, GROUPS[g][0] + 256], F32, kind="ExternalInput").ap()
            P.ld(Ssb[g][:], Sd, writes=['S%d' % g])
        n = 0
        for g in range(3):
            L = GROUPS[g][0] + 256
            for h in range(8):
                c = g * 8 + h
                P.op('dve', lambda e, c=c: e.tensor_copy(lb[:], rbT[:, c:c + 1].to_broadcast([33, 128])), ['rbT'], ['lb'])
                f = fb[n % 2]; kf = 'fb%d' % (n % 2); n += 1
                for ci, c0 in enumerate(range(0, L, 512)):
                    cw = min(512, L - c0)
                    pzz = pz[ci % 2]
                    P.mm(pzz[:, 0:cw], lb[:], Ssb[g][:, c0:c0 + cw], True, True, ['lb', 'S%d' % g], ['pz%d' % (ci % 2)])
                    P.op('act', lambda e, pzz=pzz, f=f, c0=c0, cw=cw: e.copy(f[:, c0:c0 + cw], pzz[:, 0:cw]), ['pz%d' % (ci % 2)], [kf])
                P.ld(Zs[g][h].ap(), f[:, 0:L], reads=[kf], writes=['Z'])
        P.barrier()
        P.flush()
    with ExitStack() as ph:
        def sb(shape, dt, name):
            return ph.enter_context(nc.sbuf_tensor(name, list(shape), dt))

        def ps(name, dt=F32, w=512):
            return ph.enter_context(nc.psum_tensor(name, [128, w], dt))
        xt = [sb([128, D], F32, 'a1_xt%d' % i) for i in range(2)]
        xb = [sb([128, D], BF16, 'a1_xb%d' % i) for i in range(2)]
        xTt = [sb([128, 16, 128], BF16, 'a1_xTt%d' % i) for i in range(2)]
        wblk = [sb([128, 16, 512], BF16, 'a1_w%d' % i) for i in range(2)]
        xTg = [sb([128, 16, 512], BF16, 'a1_xTg%d' % i) for i in range(2)]
        stg = [sb([128, 512], BF16, 'a1_stg%d' % i) for i in range(4)]
        ptr = [ps('a1_ptr%d' % i, BF16, 1024) for i in range(2)]
        pm = [ps('a1_pm%d' % i) for i in range(4)]
        n4 = 0
        for t in range(NTT):
            b = t % 2
            P.ld(xt[b][:], xc[t * 128:(t + 1) * 128, :], writes=['xt%d' % b])
            P.op('act', lambda e, b=b: e.copy(xb[b][:], xt[b][:]), ['xt%d' % b], ['xb%d' % b])
            for q in range(4):
                pq = ptr[n4 % 2]; kp = 'ptr%d' % (n4 % 2)
                for j in range(4):
                    k = q * 4 + j
                    P.tr(pq[:, j * 128:(j + 1) * 128], xb[b][:, k * 128:(k + 1) * 128], identb[:], ['xb%d' % b, 'a_identb'], [kp])
                P.op('dve', lambda e, q=q, b=b, pq=pq: e.tensor_copy(xTt[b][:, q * 4:(q + 1) * 4, :], pq[:, 0:512].rearrange("p (a b) -> p a b", a=4)), [kp], ['xTt%d' % b])
                n4 += 1
            P.ld(xT_d[:, :, t * 128:(t + 1) * 128].rearrange("k p t -> p k t"), xTt[b][:], reads=['xTt%d' % b], writes=['xT_d'])
        P.barrier()
        ns = 0
        npm = 0
        for cb in range(18):
            g, part, hh = cb // 6, (cb % 6) // 2, cb % 2
            wb_ = wblk[cb % 2]; kw = 'w%d' % (cb % 2)
            P.ld(wb_[:], W['attn_w_in'].rearrange("(k p) f -> p k f", p=128)[:, :, cb * 512:(cb + 1) * 512], writes=[kw], eng='pool')
            tg0 = 4 if part == 0 else (3 if g < 2 else 0)
            for tg in range(tg0, NTT // 4):
                xg_ = xTg[tg % 2]; kx = 'xTg%d' % (tg % 2)
                P.ld(xg_[:], xT_d[:, :, tg * 512:(tg + 1) * 512].rearrange("k p t -> p k t"), writes=[kx])
                for j in range(4):
                    pmm = pm[npm % 4]; kpm = 'pm%d' % (npm % 4); npm += 1
                    st_ = stg[ns % 4]; kst = 'stg%d' % (ns % 4); ns += 1
                    if part < 2:
                        h = hh * 4 + j
                        for k in range(16):
                            P.mm(pmm[:], wb_[:, k, j * 128:(j + 1) * 128], xg_[:, k, :], k == 0, k == 15, [kw, kx], [kpm])
                        if ns % 2:
                            P.op('act', lambda e, st_=st_, pmm=pmm: e.copy(st_[:], pmm[:]), [kpm], [kst])
                        else:
                            P.op('dve', lambda e, st_=st_, pmm=pmm: e.tensor_copy(st_[:], pmm[:]), [kpm], [kst])
                        if part == 0:
                            P.ld(qT_d[g, h, :, (tg - 4) * 512:(tg - 3) * 512], st_[:], reads=[kst], writes=['qT_d'])
                        else:
                            P.ld(kT_d[g, h, :, tg * 512:(tg + 1) * 512], st_[:], reads=[kst], writes=['kT_d'])
                    else:
                        for k in range(16):
                            P.mm(pmm[:], xg_[:, k, j * 128:(j + 1) * 128], wb_[:, k, :], k == 0, k == 15, [kw, kx], [kpm])
                        if ns % 2:
                            P.op('act', lambda e, st_=st_, pmm=pmm: e.copy(st_[:], pmm[:]), [kpm], [kst])
                        else:
                            P.op('dve', lambda e, st_=st_, pmm=pmm: e.tensor_copy(st_[:], pmm[:]), [kpm], [kst])
                        r0 = tg * 512 + j * 128
                        P.ld(v_d[g, hh * 4:(hh + 1) * 4, r0:r0 + 128, :].rearrange("h t d -> t h d"),
                             st_[:].rearrange("p (h d) -> p h d", h=4), reads=[kst], writes=['v_d'])
        P.barrier()
        P.flush()
    scale = 128.0 ** -0.5
    with ExitStack() as ph:
        def sb(shape, dt, name):
            return ph.enter_context(nc.sbuf_tensor(name, list(shape), dt))

        def ps(name, dt=F32, w=512):
            return ph.enter_context(nc.psum_tensor(name, [128, w], dt))
        kT = [sb([128, NTOK], BF16, 'a2_kT%d' % g) for g in range(3)]
        qT = [sb([128, NQ * 128], BF16, 'a2_qT%d' % g) for g in range(3)]
        vs = [sb([128, NTT, 129], BF16, 'a2_v%d' % g) for g in range(3)]
        Bt = sb([128, 24, 128], F32, 'a2_Bt')
        tmp = [sb([128, 512], F32, 'a2_tmp%d' % i) for i in range(2)]
        pT = [sb([128, 512], BF16, 'a2_pT%d' % i) for i in range(3)]
        rc = [sb([128, 1], F32, 'a2_rc%d' % i) for i in range(2)]
        hv = sb([128, 1], F32, 'a2_hv')
        pS = [ps('a2_pS%d' % i) for i in range(3)]
        pO = [ps('a2_pO%d' % i) for i in range(2)]
        P.ld(hv[:], hv_d, writes=['hv'])
        for g in range(3):
            P.op('pool', lambda e, g=g: e.memset(vs[g][:, :, 128:129], 1.0), [], ['vs%d' % g])
            P.op('dve', lambda e, g=g: e.tensor_scalar(vs[g][:, 0:16, 128:129], vs[g][:, 0:16, 128:129], hv[:, 0:1], None, ALU.mult), ['vs%d' % g, 'hv'], ['vs%d' % g])
        chunks = []
        bi = 0
        for g in range(3):
            nm = GROUPS[g][0] // 128 + 1
            for m0 in range(0, nm, 4):
                ms = list(range(m0, min(m0 + 4, nm)))
                chunks.append((g, ms, bi))
                bi += len(ms)
        nS = 0
        nO = 0
        for h in range(8):
            for g in range(3):
                P.ld(kT[g][:], kT_d[g, h], writes=['kT%d' % g])
                P.ld(qT[g][:], qT_d[g, h], writes=['qT%d' % g])
                P.ld(vs[g][:, :, 0:128], v_d[g, h].rearrange("(t p) d -> p t d", p=128), writes=['vs%d' % g])
            bi = 0
            for g in range(3):
                L = GROUPS[g][0] + 256
                for m in range(GROUPS[g][0] // 128 + 1):
                    tap = bass.AP(tensor=Zs[g][h], offset=m * 128 + 127, ap=[[L - 1, 128], [1, 128]])
                    P.ld(Bt[:, bi, :], tap, writes=['Bt'])
                    bi += 1
            items = []
            for qt in range(NQ):
                io = nO % 2; nO += 1
                for ci, (g, ms, b0) in enumerate(chunks):
                    items.append((qt, io, g, ms, b0, ci == 0, ci == len(chunks) - 1, nS % 3, nS % 2))
                    nS += 1
            SKEW = 2

            def front(it):
                qt, io, g, ms, b0, isf, isl, i3, i2 = it
                T = 16 + qt
                n = len(ms)
                for mi, m in enumerate(ms):
                    KT = T - m
                    P.mm(pS[i3][:, mi * 128:(mi + 1) * 128], kT[g][:, KT * 128:(KT + 1) * 128], qT[g][:, qt * 128:(qt + 1) * 128], True, True,
                         ['kT%d' % g, 'qT%d' % g], ['pS%d' % i3])
                P.op('dve', lambda e, i3=i3, i2=i2, n=n, b0=b0: e.scalar_tensor_tensor(
                    tmp[i2][:, 0:n * 128], pS[i3][:, 0:n * 128], scale, Bt[:, b0:b0 + n, :].rearrange("p a b -> p (a b)"), ALU.mult, ALU.add),
                    ['pS%d' % i3, 'Bt'], ['tmp%d' % i2])
                P.op('act', lambda e, i3=i3, i2=i2, n=n: e.activation(pT[i3][:, 0:n * 128], tmp[i2][:, 0:n * 128], AF.Exp), ['tmp%d' % i2], ['pT%d' % i3])

            def back(it, h=h):
                qt, io, g, ms, b0, isf, isl, i3, i2 = it
                T = 16 + qt
                for mi, m in enumerate(ms):
                    KT = T - m
                    P.mm(pO[io][:, 0:129], pT[i3][:, mi * 128:(mi + 1) * 128], vs[g][:, KT, :], isf and mi == 0, isl and mi == len(ms) - 1,
                         ['pT%d' % i3, 'vs%d' % g], ['pO%d' % io])
                if isl:
                    P.op('dve', lambda e, io=io: e.reciprocal(rc[io][:], pO[io][:, 128:129]), ['pO%d' % io], ['rc%d' % io])
                    P.op('act', lambda e, io=io, qt=qt, h=h: e.activation(o_all[:, qt, h * 128:(h + 1) * 128], pO[io][:, 0:128], AF.Copy, scale=rc[io][:, 0:1]),
                         ['pO%d' % io, 'rc%d' % io], ['oall%d' % qt])
            for i in range(len(items) + SKEW):
                if i < len(items):
                    front(items[i])
                if i >= SKEW:
                    back(items[i - SKEW])
        P.barrier()
        P.flush()
    with ExitStack() as ph:
        def sb(shape, dt, name):
            return ph.enter_context(nc.sbuf_tensor(name, list(shape), dt))

        def ps(name, dt=F32, w=512):
            return ph.enter_context(nc.psum_tensor(name, [128, w], dt))
        wo = sb([128, 8, D], BF16, 'a3_wo')
        oT = [sb([128, 8, 128], BF16, 'a3_oT%d' % i) for i in range(2)]
        xt = [sb([128, D], F32, 'a3_xt%d' % i) for i in range(2)]
        ot = [sb([128, D], F32, 'a3_ot%d' % i) for i in range(2)]
        gt = sb([128, D], F32, 'a3_g'); bt = sb([128, D], F32, 'a3_b')
        st = sb([128, 24], F32, 'a3_st'); ag = sb([128, 2], F32, 'a3_ag'); rstd = sb([128, 1], F32, 'a3_rstd')
        ptr = [ps('a3_ptr%d' % i, BF16, 1024) for i in range(2)]
        py = [ps('a3_py%d' % i) for i in range(4)]
        P.ld(wo[:], W['attn_w_out'].rearrange("(k p) d -> p k d", p=128), writes=['wo'], eng='pool')
        P.ld(gt[:], W['ln_g'].partition_broadcast(128), writes=['lng'])
        P.ld(bt[:], W['ln_b'].partition_broadcast(128), writes=['lnb'])
        for qt in range(NQ):
            b = qt % 2
            P.ld(xt[b][:], xc[(16 + qt) * 128:(17 + qt) * 128, :], writes=['xt%d' % b])
            for q in range(2):
                pq = ptr[q]; kp = 'ptr%d' % q
                for j in range(4):
                    k = q * 4 + j
                    P.tr(pq[:, j * 128:(j + 1) * 128], o_all[:, qt, k * 128:(k + 1) * 128], identb[:], ['oall%d' % qt, 'a_identb'], [kp])
                P.op('dve', lambda e, q=q, b=b, pq=pq: e.tensor_copy(oT[b][:, q * 4:(q + 1) * 4, :], pq[:, 0:512].rearrange("p (a b) -> p a b", a=4)), [kp], ['oT%d' % b])
            for c in range(4):
                for k in range(8):
                    P.mm(py[c][:], oT[b][:, k, :], wo[:, k, c * 512:(c + 1) * 512], k == 0, k == 7, ['oT%d' % b, 'wo'], ['py%d' % c])
                P.op('dve', lambda e, b=b, c=c: e.scalar_tensor_tensor(xt[b][:, c * 512:(c + 1) * 512], xt[b][:, c * 512:(c + 1) * 512], ALPHA, py[c][:], ALU.mult, ALU.add),
                     ['py%d' % c, 'xt%d' % b], ['xt%d' % b])
            emit_ln(P, xt[b], ot[b], gt, bt, 'a3_', ['xt%d' % b], ['ot%d' % b], st, ag, rstd)
            P.ld(out_d[qt * 128:(qt + 1) * 128, :], ot[b][:], reads=['ot%d' % b])
        P.barrier()
        P.flush()


def build_attn(NQ):
    nc = bass.Bass("TRN2", target_bir_lowering=False)
    xc = nc.dram_tensor("xc", [(16 + NQ) * 128, D], F32, kind="ExternalInput").ap()
    hv_d = nc.dram_tensor("hv", [128, 1], F32, kind="ExternalInput").ap()
    out_d = nc.dram_tensor("out", [NQ * 128, D], F32, kind="ExternalOutput").ap()
    W = {}
    W['rel_bias'] = nc.dram_tensor("rel_bias", [32, 24], F32, kind="ExternalInput").ap()
    W['attn_w_in'] = nc.dram_tensor("attn_w_in", [D, 9216], F32, kind="ExternalInput").ap()
    W['attn_w_out'] = nc.dram_tensor("attn_w_out", [1024, D], F32, kind="ExternalInput").ap()
    W['ln_g'] = nc.dram_tensor("ln_g", [D], F32, kind="ExternalInput").ap()
    W['ln_b'] = nc.dram_tensor("ln_b", [D], F32, kind="ExternalInput").ap()
    with ExitStack() as es:
        P = Prog(nc, es)
        cst = load_consts(P, nc, ['c_ident'])
        emit_attn(P, nc, es, NQ, xc, hv_d, W, out_d, cst)
        P.finish()
    return nc


class RR:
    def __init__(self, items):
        self.items = items
        self.i = 0

    def get(self):
        it = self.items[self.i % len(self.items)]
        self.i += 1
        return it


def dn_consts_np():
    c = {}
    iu = np.triu(np.ones((128, 128), np.float32), 1)
    c['c_negu'] = (-1e4 * iu).astype(np.float32)
    c['c_negl'] = (-1e4 * iu.T).astype(np.float32)
    c['c_strl'] = iu.T.copy()
    return c


def emit_dn(P, nc, es, NB_, NTL, x_d, W, out_d, cst, dbg=False):
    ident, uti, ones = cst['c_ident'], cst['c_uti'], cst['c_ones']
    negu, negl, strl = cst['c_negu'], cst['c_negl'], cst['c_strl']
    scale = 128.0 ** -0.5

    def sb(shape, dt, name):
        return es.enter_context(nc.sbuf_tensor(name, list(shape), dt))

    identb = sb([128, 128], BF16, 'd_identb')
    P.op('dve', lambda e: e.tensor_copy(identb[:], ident[:]), ['c_ident'], ['d_identb'])
    wsl = sb([128, 16, 1544], BF16, 'd_wsl')
    P.ld(wsl[:], W['w_sl'].rearrange("(k p) f -> p k f", p=128), writes=['wsl'], eng='pool')
    cw = sb([128, 8, 4], F32, 'd_cw')
    P.ld(cw[:], W['cw_l'], writes=['cw'])
    ng = sb([128, 128], F32, 'd_ng')
    P.ld(ng[:], W['norm_g'].partition_broadcast(128), writes=['ng'])
    negA = sb([128, 4], F32, 'd_negA')
    dtb = sb([128, 4], F32, 'd_dtb')
    P.ld(negA[:], W['alog_l'].partition_broadcast(128), writes=['negA'])
    P.ld(dtb[:], W['dtb_l'].partition_broadcast(128), writes=['dtb'])
    P.op('act', lambda e: e.activation(negA[:], negA[:], AF.Exp), ['negA'], ['negA'])
    P.op('dve', lambda e: e.tensor_scalar(negA[:], negA[:], -1.0, None, ALU.mult), ['negA'], ['negA'])
    xt = [sb([128, D], F32, 'd_xt0')]
    xb = [sb([128, D], BF16, 'd_xb%d' % i) for i in range(2)]
    xT = [sb([128, 16, 128], BF16, 'd_xT%d' % i) for i in range(2)]
    raw = [[sb([128, 131], F32, 'd_raw%d_%d' % (i, c)) for c in range(8)] for i in range(2)]
    qk = [[sb([128, 128], BF16, 'd_qk%d_%d' % (i, c)) for c in range(4)] for i in range(4)]
    vT = [[sb([128, 128], F32, 'd_vT%d_%d' % (i, c)) for c in range(4)] for i in range(4)]
    qf = [[sb([128, 128], F32, 'd_qf%d_%d' % (i, c)) for c in range(2)] for i in range(4)]
    zs = [sb([128, 512], F32, 'd_zs%d' % i) for i in range(4)]
    outt = [sb([128, 512], BF16, 'd_out%d' % i) for i in range(2)]
    sm4 = {n: [sb([128, 4], F32, 'd_%s%d' % (n, i)) for i in range(4)] for n in ('beta', 'nbeta', 'g', 'gc', 'ngc', 'xa')}
    ded = {n: [[sb([128, 128], F32 if n in ('Erow', 'vb') else BF16, 'd_%s%d_%d' % (n, i, j)) for j in range(4)] for i in range(2)]
           for n in ('kdec', 'vb', 'qtil')}
    ded['Erow'] = [[None] * 4 for _ in range(2)]
    ded['QKdT'] = [[None] * 4 for _ in range(2)]
    c1 = [[sb([128, 1], F32, 'd_c1%d_%d' % (i, j)) for j in range(4)] for i in range(2)]
    growl = [[sb([128, 1], F32, 'd_gl%d_%d' % (i, j)) for j in range(4)] for i in range(2)]
    S4 = sb([128, 512], F32, 'd_S4')
    Sbf4 = sb([128, 512], BF16, 'd_Sbf4')
    Sst = [S4[:, j * 128:(j + 1) * 128] for j in range(4)]
    Sbf = [Sbf4[:, j * 128:(j + 1) * 128] for j in range(4)]
    E4 = [sb([128, 512], F32, 'd_E4_%d' % i) for i in range(2)]
    Q4 = [sb([128, 512], BF16, 'd_Q4_%d' % i) for i in range(2)]
    for i in range(2):
        for j in range(4):
            ded['Erow'][i][j] = E4[i][:, j * 128:(j + 1) * 128]
            ded['QKdT'][i][j] = Q4[i][:, j * 128:(j + 1) * 128]
    c4 = {}
    for nm, src_ in (('negu4', negu), ('negl4', negl), ('strl4', strl)):
        c4[nm] = sb([128, 512], F32, 'd_' + nm)
        for j in range(4):
            P.op('pool', lambda e, t_=c4[nm], src_=src_, j=j: e.tensor_copy(t_[:, j * 128:(j + 1) * 128], src_[:]), ['c_' + nm[:4]], ['d_' + nm])
    tf4 = RR([(sb([128, 512], F32, 'd_tf4_%d' % i), 'tf4_%d' % i) for i in range(10)])
    growl4 = [sb([128, 4], F32, 'd_gl4_%d' % i) for i in range(2)]
    tf = RR([(sb([128, 128], F32, 'd_tf%d' % i), 'tf%d' % i) for i in range(16)])
    tb = RR([(sb([128, 128], BF16, 'd_tb%d' % i), 'tb%d' % i) for i in range(8)])
    tn = RR([(sb([128, 128], BF16, 'd_tn%d' % i), 'tn%d' % i) for i in range(8)])
    tn4 = RR([(sb([128, 512], BF16, 'd_tn4_%d' % i), 'tn4_%d' % i) for i in range(16)])
    identb4 = sb([128, 512], BF16, 'd_identb4')
    for j4 in range(4):
        P.op('dve', lambda e, j4=j4: e.tensor_copy(identb4[:, j4 * 128:(j4 + 1) * 128], ident[:]), ['c_ident'], ['d_identb4'])
    TT4 = [sb([128, 512], BF16, 'd_TT4_%d' % i) for i in range(2)]
    identb2 = identb
    t1 = RR([(sb([128, 1], F32, 'd_t1%d' % i), 't1%d' % i) for i in range(16)])
    pxt = es.enter_context(nc.psum_tensor('d_pxt', [128, 1024], BF16))
    pkt = es.enter_context(nc.psum_tensor('d_pkt', [128, 1024], BF16))
    pz = es.enter_context(nc.psum_tensor('d_pz', [128, 512], F32))
    pm0b = es.enter_context(nc.psum_tensor('d_pm0b', [128, 1024], BF16))
    pbk = RR([(es.enter_context(nc.psum_tensor('d_pb%d' % i, [128, 512], F32)), 'pb%d' % i) for i in range(4)])

    def sl(t, j):
        return t[:, j * 128:(j + 1) * 128]

    rtm = [sb([128, 512], BF16, 'd_rtm%d' % i) for i in range(2)]
    tfP = RR([(sb([128, 128], F32, 'd_tfP%d' % i), 'tfP%d' % i) for i in range(24)])
    pbP = RR([pbk.items[3]])
    pbU = RR(pbk.items[0:3])

    def genP(b_, n):
        par = n % 2
        p4 = n % 4
        row0 = (b_ * NTL + n) * 128
        kx, kb_, kT_ = 'xt0', 'xb%d' % par, 'xT%d' % par
        P.ld(xt[0][:], x_d[row0:row0 + 128, :], writes=[kx])
        P.op('act', lambda e, par=par, p4=p4: e.copy(xb[par][:], xt[0][:]), [kx], [kb_])
        for q in range(4):
            half = q % 2
            for jj in range(4):
                k = q * 4 + jj
                P.tr(pxt[:, half * 512 + jj * 128:half * 512 + (jj + 1) * 128], xb[par][:, k * 128:(k + 1) * 128], identb[:], [kb_, 'd_identb'], ['pxt'])
            P.op('dve', lambda e, q=q, par=par, p4=p4, half=half: e.tensor_copy(xT[par][:, q * 4:(q + 1) * 4, :], pxt[:, half * 512:(half + 1) * 512].rearrange("p (a b) -> p a b", a=4)), [], ['pxt', kT_])
        yield
        ys = [None] * 4
        for grp in range(2):
            bk_, kbk = pbP.get()
            for k in range(16):
                P.mm(bk_[:, 0:512], xT[par][:, k, :], wsl[:, k, grp * 512:(grp + 1) * 512], k == 0, k == 15, ['wsl', kT_], [kbk])
            P.op('act', lambda e, bk_=bk_, grp=grp: e.copy(rtm[grp][:], bk_[:, 0:512]), [], [kbk, 'rtm%d' % grp])
            for ci in range(4):
                P.tr(pxt[:, grp * 512 + ci * 128:grp * 512 + (ci + 1) * 128], rtm[grp][:, ci * 128:(ci + 1) * 128], identb[:], ['rtm%d' % grp, 'd_identb'], ['pxt'])
            for ci in range(4):
                c = grp * 4 + ci
                kr = 'raw%d_%d' % (par, c)
                P.op('act', lambda e, c=c, par=par, p4=p4, grp=grp, ci=ci: e.copy(raw[par][c][:, 3:131], pxt[:, grp * 512 + ci * 128:grp * 512 + (ci + 1) * 128]), [], ['pxt', kr])
                P.op('pool', lambda e, c=c, par=par, p4=p4: e.tensor_copy(raw[1 - par][c][:, 0:3], raw[par][c][:, 128:131]), [kr], ['raw%d_%d' % (1 - par, c)])
                acc, ka = tfP.get()
                P.op('dve', lambda e, c=c, par=par, p4=p4, acc=acc: e.tensor_scalar(acc[:], raw[par][c][:, 0:128], cw[:, c, 0:1], None, ALU.mult), [kr, 'cw'], [ka])
                for jt in range(1, 4):
                    P.op('dve', lambda e, c=c, par=par, p4=p4, acc=acc, jt=jt: e.scalar_tensor_tensor(acc[:], raw[par][c][:, jt:jt + 128], cw[:, c, jt:jt + 1], acc[:], ALU.mult, ALU.add), [kr, 'cw', ka], [ka])
                if c >= 4:
                    P.op('act', lambda e, c=c, par=par, p4=p4, acc=acc: e.activation(vT[p4][c - 4][:], acc[:], AF.Silu), [ka], ['vT%d_%d' % (p4, c - 4)])
                else:
                    y, ky = tfP.get()
                    P.op('act', lambda e, acc=acc, y=y: e.activation(y[:], acc[:], AF.Silu), [ka], [ky])
                    ys[c] = (y, ky)
                yield
        bk_, kbk = pbP.get()
        sqs = []
        for c in range(4):
            y, ky = ys[c]
            sq, ksq = tfP.get()
            P.op('pool', lambda e, y=y, sq=sq: e.tensor_tensor(sq[:], y[:], y[:], ALU.mult), [ky], [ksq])
            sqs.append((sq, ksq))
        for c in range(4):
            P.mm(sl(bk_, c), ones[:], sqs[c][0][:], True, True, ['c_ones', sqs[c][1]], [kbk])
        for c in range(4):
            y, ky = ys[c]
            rn, krn = tfP.get()
            P.op('dve', lambda e, rn=rn, bk_=bk_, c=c: e.tensor_scalar(rn[:], sl(bk_, c), 1e-6, None, ALU.add), [], [kbk, krn])
            P.op('act', lambda e, rn=rn: e.activation(rn[:], rn[:], AF.Sqrt), [krn], [krn])
            P.op('dve', lambda e, rn=rn: e.reciprocal(rn[:], rn[:]), [krn], [krn])
            P.op('dve', lambda e, rn=rn, y=y, c=c, par=par, p4=p4: e.tensor_tensor(qk[p4][c][:], y[:], rn[:], ALU.mult), [krn, ky], ['qk%d_%d' % (p4, c)])
            if c < 2:
                P.op('pool', lambda e, rn=rn, y=y, c=c, par=par, p4=p4: e.tensor_tensor(qf[p4][c][:], y[:], rn[:], ALU.mult), [krn, ky], ['qf%d_%d' % (p4, c)])
        yield
        for k in range(16):
            P.mm(pz[:], xT[par][:, k, :], wsl[:, k, 1024:1536], k == 0, k == 15, [kT_, 'wsl'], ['pz'])
        P.op('act', lambda e, par=par, p4=p4: e.activation(zs[p4][:], pz[:], AF.Silu), [], ['pz', 'zs%d' % p4])
        yield
        pba, kpba = pbP.get()
        for k in range(16):
            P.mm(pba[:, 0:8], xT[par][:, k, :], wsl[:, k, 1536:1544], k == 0, k == 15, [kT_, 'wsl'], [kpba])
        beta, nbeta, g_, gc, ngc, xa = (sm4[n_][p4] for n_ in ('beta', 'nbeta', 'g', 'gc', 'ngc', 'xa'))
        ksm = {n_: 'sm_%s%d' % (n_, p4) for n_ in sm4}
        P.op('act', lambda e, beta=beta, pba=pba: e.activation(beta[:], pba[:, 0:4], AF.Sigmoid), [], [kpba, ksm['beta']])
        P.op('dve', lambda e, beta=beta, nbeta=nbeta: e.tensor_scalar(nbeta[:], beta[:], -1.0, None, ALU.mult), [ksm['beta']], [ksm['nbeta']])
        P.op('dve', lambda e, xa=xa, pba=pba: e.tensor_tensor(xa[:], pba[:, 4:8], dtb[:], ALU.add), ['dtb'], [kpba, ksm['xa']])
        P.op('act', lambda e, xa=xa: e.activation(xa[:], xa[:], AF.Exp), [ksm['xa']], [ksm['xa']])
        P.op('dve', lambda e, xa=xa: e.tensor_scalar(xa[:], xa[:], 1.0, None, ALU.add), [ksm['xa']], [ksm['xa']])
        P.op('act', lambda e, xa=xa: e.activation(xa[:], xa[:], AF.Ln), [ksm['xa']], [ksm['xa']])
        P.op('dve', lambda e, xa=xa, g_=g_: e.tensor_tensor(g_[:], xa[:], negA[:], ALU.mult), [ksm['xa'], 'negA'], [ksm['g']])
        P.mm(pba[:, 128:132], uti[:], g_[:], True, True, ['c_uti', ksm['g']], [kpba])
        P.op('dve', lambda e, gc=gc, pba=pba: e.tensor_copy(gc[:], pba[:, 128:132]), [], [kpba, ksm['gc']])
        P.op('dve', lambda e, gc=gc, ngc=ngc: e.tensor_scalar(ngc[:], gc[:], -1.0, None, ALU.mult), [ksm['gc']], [ksm['ngc']])

    def stageU(b_, n):
        par = n % 2
        p4 = n % 4
        row0 = (b_ * NTL + n) * 128
        beta, nbeta, g_, gc, ngc, xa = (sm4[n_][p4] for n_ in ('beta', 'nbeta', 'g', 'gc', 'ngc', 'xa'))
        ksm = {n_: 'sm_%s%d' % (n_, p4) for n_ in sm4}
        dkk = [{n_: 'ded_%s%d_%d' % (n_, par, j) for n_ in ded} for j in range(4)]
        kq_ = ['qk%d_%d' % (p4, j // 2) for j in range(4)]
        kk_ = ['qk%d_%d' % (p4, 2 + j // 2) for j in range(4)]
        qTs = [qk[p4][j // 2] for j in range(4)]
        kTs = [qk[p4][2 + j // 2] for j in range(4)]
        bgr, kbgr = pbU.get()
        for j in range(4):
            gbc, kgbc = tf.get()
            P.op('dve', lambda e, gbc=gbc, g_=g_, j=j: e.tensor_scalar(gbc[:], ones[:], g_[:, j:j + 1], None, ALU.mult), ['c_ones', ksm['g']], [kgbc])
            P.mm(sl(bgr, j), gbc[:], uti[:], True, True, [kgbc, 'c_uti'], [kbgr])
        P.op('act', lambda e, bgr=bgr, par=par, p4=p4: e.activation(E4[par][:], bgr[:], AF.Exp), [], [kbgr] + [dkk[j]['Erow'] for j in range(4)])
        P.op('dve', lambda e, bgr=bgr, par=par, p4=p4: e.tensor_copy(growl4[par][:], bgr[:, 127:512:128]), [], [kbgr] + ['gl%d_%d' % (par, j) for j in range(4)])
        tD4, ktD4 = tf4.get()
        P.op('dve', lambda e, bgr=bgr, tD4=tD4: e.scalar_tensor_tensor(tD4[:], bgr[:], -1.0, c4['negu4'][:], ALU.mult, ALU.add), ['d_negu4'], [kbgr, ktD4])
        tDT4, ktDT4 = tf4.get()
        P.op('dve', lambda e, bgr=bgr, tDT4=tDT4: e.tensor_tensor(tDT4[:], bgr[:], c4['negl4'][:], ALU.add), ['d_negl4'], [kbgr, ktDT4])
        for j in range(4):
            P.op('act', lambda e, tD4=tD4, gc=gc, j=j: e.activation(sl(tD4, j), sl(tD4, j), AF.Exp, bias=gc[:, j:j + 1], scale=1.0), [ktD4, ksm['gc']], [ktD4])
        for j in range(4):
            P.op('act', lambda e, tDT4=tDT4, ngc=ngc, j=j: e.activation(sl(tDT4, j), sl(tDT4, j), AF.Exp, bias=ngc[:, j:j + 1], scale=1.0), [ktDT4, ksm['ngc']], [ktDT4])
        Ds4, kDs4 = tf4.get()
        P.op('pool', lambda e, Ds4=Ds4, tD4=tD4: e.tensor_tensor(Ds4[:], tD4[:], c4['strl4'][:], ALU.mult), [ktD4, 'd_strl4'], [kDs4])
        Dss = [(sl(Ds4, j), kDs4) for j in range(4)]
        tDTs = [(sl(tDT4, j), ktDT4) for j in range(4)]
        yield
        bkk, kbkk = pbU.get()
        for j in range(4):
            P.mm(sl(bkk, j), kTs[j][:], kTs[j][:], True, True, [kk_[j]], [kbkk])
        B4, kB4 = tn4.get()
        for j in range(4):
            Ds, kDs = Dss[j]
            P.op('dve', lambda e, bkk=bkk, B4=B4, nbeta=nbeta, Ds=Ds, j=j: e.scalar_tensor_tensor(sl(B4, j), sl(bkk, j), nbeta[:, j:j + 1], Ds, ALU.mult, ALU.mult), [ksm['nbeta'], kDs], [kbkk, kB4])
        yield
        bm0, kbm0 = pm0b, 'pm0b'
        for j in range(4):
            P.tr(sl(bm0, j), sl(B4, j), identb[:], [kB4, 'd_identb'], [kbm0])
        M4, kM4 = tn4.get()
        P.op('dve', lambda e, M4=M4: e.tensor_copy(M4[:], pm0b[:, 0:512]), [], [kbm0, kM4])
        P4, kP4 = tn4.get()
        P.op('dve', lambda e, P4=P4: e.tensor_tensor(P4[:], pm0b[:, 0:512], identb4[:], ALU.add), ['d_identb4'], [kbm0, kP4])
        yield
        bkq, kbkq = pbU.get()
        for j in range(4):
            P.mm(sl(bkq, j), kTs[j][:], qTs[j][:], True, True, [kk_[j], kq_[j]], [kbkq])
        P.op('dve', lambda e, bkq=bkq, par=par, p4=p4, tDT4=tDT4: e.scalar_tensor_tensor(Q4[par][:], bkq[:], scale, tDT4[:], ALU.mult, ALU.mult), [ktDT4], [kbkq] + [dkk[j]['QKdT'] for j in range(4)])
        yield
        for j in range(4):
            P.tr(pkt[:, j * 128:(j + 1) * 128], kTs[j][:], identb[:], [kk_[j], 'd_identb'], ['pkt'])
        for j in range(4):
            gl_ = growl4[par][:, j:j + 1]
            decf, kdf = t1.get()
            P.op('act', lambda e, decf=decf, gc=gc, gl_=gl_, j=j: e.activation(decf[:], gc[:, j:j + 1], AF.Exp, bias=gl_, scale=-1.0), [ksm['gc'], 'gl%d_%d' % (par, j)], [kdf])
            kdec = ded['kdec'][par][j]
            P.op('dve', lambda e, kdec=kdec, decf=decf, j=j: e.tensor_scalar(kdec[:], pkt[:, j * 128:(j + 1) * 128], decf[:, 0:1], None, ALU.mult), [kdf], ['pkt', dkk[j]['kdec']])
        yield
        bvt, kbvt = pbU.get()
        for j in range(4):
            P.tr(sl(bvt, j), vT[p4][j][:], ident[:], ['vT%d_%d' % (p4, j), 'c_ident'], [kbvt])
        for j in range(4):
            vb = ded['vb'][par][j]
            P.op('dve', lambda e, bvt=bvt, vb=vb, beta=beta, j=j: e.tensor_scalar(vb[:], sl(bvt, j), beta[:, j:j + 1], None, ALU.mult), [ksm['beta']], [kbvt, dkk[j]['vb']])
            c1_ = c1[par][j]; kc1 = 'c1%d_%d' % (par, j)
            P.op('act', lambda e, c1_=c1_, gc=gc, j=j: e.activation(c1_[:], gc[:, j:j + 1], AF.Exp), [ksm['gc']], [kc1])
            P.op('dve', lambda e, c1_=c1_, nbeta=nbeta, j=j: e.tensor_tensor(c1_[:], c1_[:], nbeta[:, j:j + 1], ALU.mult), [kc1, ksm['nbeta']], [kc1])
            qtil = ded['qtil'][par][j]
            Erow = ded['Erow'][par][j]
            P.op('dve', lambda e, par=par, p4=p4, qtil=qtil, j=j, Erow=Erow: e.scalar_tensor_tensor(qtil[:], qf[p4][j // 2][:], scale, Erow[:], ALU.mult, ALU.mult), ['qf%d_%d' % (p4, j // 2), dkk[j]['Erow']], [dkk[j]['qtil']])
        yield
        for lv in range(1, 7):
            if lv < 6:
                bmn, kbmn = pbU.get()
                for j in range(4):
                    P.mm(sl(bmn, j), sl(B4, j), sl(M4, j), True, True, [kB4, kM4], [kbmn])
            bbn, kbbn = pbU.get()
            for j in range(4):
                P.mm(sl(bbn, j), sl(M4, j), sl(B4, j), True, True, [kB4, kM4], [kbbn])
            Bn4, kBn4 = tn4.get()
            P.op('act', lambda e, Bn4=Bn4, bbn=bbn: e.copy(Bn4[:], bbn[:]), [], [kbbn, kBn4])
            if lv < 6:
                Mn4, kMn4 = tn4.get()
                P.op('dve', lambda e, Mn4=Mn4, bmn=bmn: e.tensor_copy(Mn4[:], bmn[:]), [], [kbmn, kMn4])
            else:
                Mn4, kMn4 = None, None
            yield
            bpn, kbpn = pbU.get()
            for j in range(4):
                P.mm(sl(bpn, j), sl(Bn4, j), sl(P4, j), True, True, [kBn4, kP4], [kbpn])
            if lv < 6:
                Pn4, kPn4 = tn4.get()
            else:
                Pn4, kPn4 = TT4[par], 'TT4_%d' % par
            P.op('dve', lambda e, Pn4=Pn4, P4=P4, bpn=bpn: e.tensor_tensor(Pn4[:], bpn[:], P4[:], ALU.add), [kP4], [kbpn, kPn4])
            P4, kP4 = Pn4, kPn4
            B4, kB4 = Bn4, kBn4
            M4, kM4 = Mn4, kMn4
            yield

    def stageS(b_, n, step):
        par = n % 2
        p4 = n % 4
        row0 = (b_ * NTL + n) * 128
        ksm = {n_: 'sm_%s%d' % (n_, p4) for n_ in sm4}
        dkk = [{n_: 'ded_%s%d_%d' % (n_, par, j) for n_ in ded} for j in range(4)]
        kk_ = ['qk%d_%d' % (p4, 2 + j // 2) for j in range(4)]
        kTs = [qk[p4][2 + j // 2] for j in range(4)]
        bks, kbks = pbU.get()
        for j in range(4):
            P.mm(sl(bks, j), kTs[j][:], Sbf[j][:], True, True, [kk_[j], 'Sbf%d' % j], [kbks])
        rr = [tn.get() for j in range(4)]
        for j in range(4):
            P.op('dve', lambda e, par=par, p4=p4, j=j, r=rr[j][0], bks=bks: e.scalar_tensor_tensor(r[:], sl(bks, j), c1[par][j][:, 0:1], ded['vb'][par][j][:], ALU.mult, ALU.add),
                 ['c1%d_%d' % (par, j), dkk[j]['vb']], [kbks, rr[j][1]])
        step()
        bvn, kbvn = pbU.get()
        for j in range(4):
            P.mm(sl(bvn, j), sl(TT4[par], j), rr[j][0][:], True, True, ['TT4_%d' % par, rr[j][1]], [kbvn])
        vn4, kvn4 = tn4.get()
        P.op('act', lambda e, vn4=vn4, bvn=bvn: e.copy(vn4[:], bvn[:]), [], [kbvn, kvn4])
        vn = [(sl(vn4, j), kvn4) for j in range(4)]
        step()
        bo, kbo = pbU.get()
        bst, kbst = pbU.get()
        for j in range(4):
            P.mm(sl(bo, j), ded['qtil'][par][j][:], Sbf[j][:], True, False, [dkk[j]['qtil'], 'Sbf%d' % j], [kbo])
            P.mm(sl(bo, j), ded['QKdT'][par][j][:], vn[j][0], False, True, [dkk[j]['QKdT'], vn[j][1]], [kbo])
            P.mm(sl(bst, j), ded['kdec'][par][j][:], vn[j][0], True, True, [dkk[j]['kdec'], vn[j][1]], [kbst])
        for j in range(4):
            P.op('dve', lambda e, par=par, p4=p4, j=j, bst=bst: e.scalar_tensor_tensor(Sst[j][:], Sst[j][:], ded['Erow'][par][j][:, 127:128], sl(bst, j), ALU.mult, ALU.add),
                 [dkk[j]['Erow']], [kbst, 'S%d' % j])
        step()
        P.op('act', lambda e: e.copy(Sbf4[:], S4[:]), ['S%d' % j for j in range(4)], ['Sbf%d' % j for j in range(4)])
        Os4, kOs4 = tf4.get()
        P.op('dve', lambda e, Os4=Os4, bo=bo: e.tensor_copy(Os4[:], bo[:]), [], [kbo, kOs4])
        Oss = [(sl(Os4, j), kOs4) for j in range(4)]
        for j in range(4):
            Os, kOs = Oss[j]
            ssq, kss = t1.get()
            junk, kj = tf.get()
            P.op('pool', lambda e, ssq=ssq: e.memset(ssq[:], 0.0), [], [kss])
            P.op('act', lambda e, junk=junk, Os=Os, ssq=ssq: e.activation(junk[:], Os, AF.Square, accum_out=ssq[:]), [kOs], [kj, kss])
            P.op('dve', lambda e, ssq=ssq: e.tensor_scalar(ssq[:], ssq[:], 1.0 / 128, 1e-6, ALU.mult, ALU.add), [kss], [kss])
            P.op('act', lambda e, ssq=ssq: e.activation(ssq[:], ssq[:], AF.Sqrt), [kss], [kss])
            P.op('dve', lambda e, ssq=ssq: e.reciprocal(ssq[:], ssq[:]), [kss], [kss])
            P.op('dve', lambda e, Os=Os, ssq=ssq: e.scalar_tensor_tensor(Os, Os, ssq[:, 0:1], ng[:], ALU.mult, ALU.mult), [kss, 'ng'], [kOs])
            P.op('pool', lambda e, Os=Os, j=j, par=par, p4=p4: e.tensor_tensor(outt[par][:, j * 128:(j + 1) * 128], Os, zs[p4][:, j * 128:(j + 1) * 128], ALU.mult),
                 [kOs, 'zs%d' % p4], ['out%d' % par])
        P.ld(out_d[row0:row0 + 128, :], outt[par][:], reads=['out%d' % par])
        if dbg and n == 0 and b_ == 0:
            def dump(name, ap, key, shape, dt=F32):
                dd = nc.dram_tensor(name, shape, dt, kind="ExternalOutput").ap()
                P.ld(dd, ap, reads=[key])
            for c in range(4):
                dump('g_qk%d' % c, qk[p4][c][:], 'qk%d_%d' % (p4, c), [128, 128], BF16)
                dump('g_vT%d' % c, vT[p4][c][:], 'vT%d_%d' % (p4, c), [128, 128])
                dump('g_raw%d' % c, raw[par][c][:], 'raw%d_%d' % (par, c), [128, 131])
            for n_ in ('beta', 'g', 'gc'):
                dump('g_' + n_, sm4[n_][p4][:], ksm[n_], [128, 4])
            for n_ in ded:
                dump('g_' + n_, ded[n_][par][0][:], dkk[0][n_], [128, 128], F32 if n_ in ('Erow', 'vb') else BF16)
            dump('g_zs', zs[p4][:], 'zs%d' % p4, [128, 512])
            dump('g_c1', c1[par][0][:], 'c1%d_0' % par, [128, 1])
            dump('g_r', rr[0][0][:], rr[0][1], [128, 128], BF16)
            dump('g_vn', vn[0][0], vn[0][1], [128, 128], BF16)
            dump('g_Os', Oss[0][0], Oss[0][1], [128, 128])
            dump('g_out', outt[par][:], 'out%d' % par, [128, 512], BF16)


    import itertools
    for b_ in range(NB_):
        for j in range(4):
            P.op('pool', lambda e, j=j: e.memset(Sst[j][:], 0.0), [], ['S%d' % j])
            P.op('pool', lambda e, j=j: e.memset(Sbf[j][:], 0.0), [], ['Sbf%d' % j])
        for c in range(8):
            P.op('pool', lambda e, c=c: e.memset(raw[0][c][:, 0:3], 0.0), [], ['raw0_%d' % c])
        for nn in range(min(2, NTL)):
            for _ in genP(b_, nn):
                pass
        for n0 in range(0, NTL, 2):
            gens = [stageU(b_, n0 + i) for i in range(2) if n0 + i < NTL]
            fill = itertools.chain(*[genP(b_, n0 + 2 + i) for i in range(2) if n0 + 2 + i < NTL])

            def step(fill=fill):
                next(fill, None)
            alive = list(gens)
            while alive:
                for g in list(alive):
                    try:
                        next(g)
                    except StopIteration:
                        alive.remove(g)
                step()
            for i in range(2):
                if n0 + i < NTL:
                    stageS(b_, n0 + i, step)
            for _ in fill:
                pass


def build_dn(NB_, NTL, dbg=False):
    nc = bass.Bass("TRN2", target_bir_lowering=False)
    x_d = nc.dram_tensor("x1", [NB_ * NTL * 128, D], F32, kind="ExternalInput").ap()
    out_d = nc.dram_tensor("out", [NB_ * NTL * 128, 512], BF16, kind="ExternalOutput").ap()
    W = {}
    W['w_sl'] = nc.dram_tensor("w_sl", [D, 1544], F32, kind="ExternalInput").ap()
    W['cw_l'] = nc.dram_tensor("cw_l", [128, 8, 4], F32, kind="ExternalInput").ap()
    W['norm_g'] = nc.dram_tensor("norm_g", [128], F32, kind="ExternalInput").ap()
    W['alog_l'] = nc.dram_tensor("alog_l", [4], F32, kind="ExternalInput").ap()
    W['dtb_l'] = nc.dram_tensor("dtb_l", [4], F32, kind="ExternalInput").ap()
    with ExitStack() as es:
        P = Prog(nc, es)
        cst = load_consts(P, nc, ['c_ident', 'c_uti', 'c_ones', 'c_negu', 'c_negl', 'c_strl'])
        emit_dn(P, nc, es, NB_, NTL, x_d, W, out_d, cst, dbg)
        P.finish()
    return nc


def dn_inputs(core, dn_w_in, dn_conv_w, dn_a_log, dn_dt_bias, dn_norm_g):
    c = core
    qc = slice(2 * c * 128, (2 * c + 2) * 128)
    cols = [dn_w_in[:, qc], dn_w_in[:, 2048 + 2 * c * 128:2048 + (2 * c + 2) * 128],
            dn_w_in[:, 4096 + 4 * c * 128:4096 + (4 * c + 4) * 128],
            dn_w_in[:, 8192 + 4 * c * 128:8192 + (4 * c + 4) * 128],
            dn_w_in[:, 12288 + 4 * c:12288 + 4 * c + 4], dn_w_in[:, 12320 + 4 * c:12320 + 4 * c + 4]]
    w_sl = np.ascontiguousarray(np.concatenate(cols, axis=1))
    cwc = np.concatenate([dn_conv_w[:, qc], dn_conv_w[:, 2048 + 2 * c * 128:2048 + (2 * c + 2) * 128],
                          dn_conv_w[:, 4096 + 4 * c * 128:4096 + (4 * c + 4) * 128]], axis=1)
    cw_l = np.ascontiguousarray(cwc.reshape(4, 8, 128).transpose(2, 1, 0))
    return {'w_sl': w_sl, 'cw_l': cw_l, 'norm_g': np.ascontiguousarray(dn_norm_g),
            'alog_l': np.ascontiguousarray(dn_a_log[4 * c:4 * c + 4]), 'dtb_l': np.ascontiguousarray(dn_dt_bias[4 * c:4 * c + 4])}


def emit_outproj(P, nc, NT, o_d, x_d, w_out, ln_g, ln_b, xm_d, cst):
    ident = cst['c_ident']
    r_d = nc.dram_tensor("op_r", [NT * 128, D], F32, kind="Internal").ap()
    with ExitStack() as ph:
        def sb(shape, dt, name):
            return ph.enter_context(nc.sbuf_tensor(name, list(shape), dt))

        def ps(name, dt=F32, w=512):
            return ph.enter_context(nc.psum_tensor(name, [128, w], dt))
        identb = sb([128, 128], BF16, 'op_identb')
        P.op('dve', lambda e: e.tensor_copy(identb[:], ident[:]), ['c_ident'], ['op_identb'])
        wo = sb([128, 32, 1024], BF16, 'op_wo')
        ob = [sb([128, 4096], BF16, 'op_ob%d' % i) for i in range(2)]
        oT = [sb([128, 32, 128], BF16, 'op_oT%d' % i) for i in range(2)]
        xh = [sb([128, 1024], F32, 'op_xh%d' % i) for i in range(2)]
        ptr = [ps('op_ptr%d' % i, BF16, 1024) for i in range(2)]
        py = [ps('op_py%d' % i) for i in range(4)]
        n4 = 0
        npy = 0
        for ch in range(2):
            P.ld(wo[:], w_out.rearrange("(k p) d -> p k d", p=128)[:, :, ch * 1024:(ch + 1) * 1024], writes=['wo'], eng='pool')
            for t in range(NT):
                b = t % 2
                P.ld(ob[b][:], o_d[t * 128:(t + 1) * 128, :], writes=['ob%d' % b])
                P.ld(xh[b][:], x_d[t * 128:(t + 1) * 128, ch * 1024:(ch + 1) * 1024], writes=['xh%d' % b])
                for q in range(8):
                    pq = ptr[n4 % 2]; kp = 'ptr%d' % (n4 % 2); n4 += 1
                    for j in range(4):
                        k = q * 4 + j
                        P.tr(pq[:, j * 128:(j + 1) * 128], ob[b][:, k * 128:(k + 1) * 128], identb[:], ['ob%d' % b, 'op_identb'], [kp])
                    if q % 2:
                        P.op('act', lambda e, q=q, b=b, pq=pq: e.copy(oT[b][:, q * 4:(q + 1) * 4, :], pq[:, 0:512].rearrange("p (a c) -> p a c", a=4)), [kp], ['oT%d' % b])
                    else:
                        P.op('dve', lambda e, q=q, b=b, pq=pq: e.tensor_copy(oT[b][:, q * 4:(q + 1) * 4, :], pq[:, 0:512].rearrange("p (a c) -> p a c", a=4)), [kp], ['oT%d' % b])
                for cc in range(2):
                    pyy = py[npy % 4]; kpy = 'py%d' % (npy % 4); npy += 1
                    for k in range(32):
                        P.mm(pyy[:], oT[b][:, k, :], wo[:, k, cc * 512:(cc + 1) * 512], k == 0, k == 31, ['oT%d' % b, 'wo'], [kpy])
                    P.op('dve', lambda e, b=b, cc=cc, pyy=pyy: e.scalar_tensor_tensor(xh[b][:, cc * 512:(cc + 1) * 512], xh[b][:, cc * 512:(cc + 1) * 512], ALPHA, pyy[:], ALU.mult, ALU.add),
                         [kpy, 'xh%d' % b], ['xh%d' % b])
                P.ld(r_d[t * 128:(t + 1) * 128, ch * 1024:(ch + 1) * 1024], xh[b][:], reads=['xh%d' % b], writes=['r_d'])
        P.barrier()
        P.flush()
    with ExitStack() as ph:
        def sb(shape, dt, name):
            return ph.enter_context(nc.sbuf_tensor(name, list(shape), dt))
        xt = [sb([128, D], F32, 'op2_xt%d' % i) for i in range(2)]
        ot = [sb([128, D], F32, 'op2_ot%d' % i) for i in range(2)]
        gt = sb([128, D], F32, 'op2_g'); bt = sb([128, D], F32, 'op2_b')
        st = sb([128, 24], F32, 'op2_st'); ag = sb([128, 2], F32, 'op2_ag'); rstd = sb([128, 1], F32, 'op2_rstd')
        P.ld(gt[:], ln_g.partition_broadcast(128), writes=['lng'])
        P.ld(bt[:], ln_b.partition_broadcast(128), writes=['lnb'])
        for t in range(NT):
            b = t % 2
            P.ld(xt[b][:], r_d[t * 128:(t + 1) * 128, :], writes=['xt%d' % b])
            emit_ln(P, xt[b], ot[b], gt, bt, 'op2_', ['xt%d' % b], ['ot%d' % b], st, ag, rstd)
            P.ld(xm_d[t * 128:(t + 1) * 128, :], ot[b][:], reads=['ot%d' % b], writes=['xm_d'])
        P.barrier()
        P.flush()


def _moe_W(nc):
    W = {}
    W['router_w'] = nc.dram_tensor("router_w", [D, NE], F32, kind="ExternalInput").ap()
    W['router_b'] = nc.dram_tensor("router_b", [NE], F32, kind="ExternalInput").ap()
    W['w_gate_up'] = nc.dram_tensor("w_gate_up", [NE, D, 2 * FF], F32, kind="ExternalInput").ap()
    W['bgu_l'] = nc.dram_tensor("bgu_l", [128, NE * 32], F32, kind="ExternalInput").ap()
    W['w_down'] = nc.dram_tensor("w_down", [NE, FF, D], F32, kind="ExternalInput").ap()
    W['b_down'] = nc.dram_tensor("b_down", [NE, D], F32, kind="ExternalInput").ap()
    W['ln_g'] = nc.dram_tensor("ln2_g", [D], F32, kind="ExternalInput").ap()
    W['ln_b'] = nc.dram_tensor("ln2_b", [D], F32, kind="ExternalInput").ap()
    return W


def build_L1(NQ, C):
    nc = bass.Bass("TRN2", target_bir_lowering=False)
    xc = nc.dram_tensor("xc", [(16 + NQ) * 128, D], F32, kind="ExternalInput").ap()
    hv_d = nc.dram_tensor("hv", [128, 1], F32, kind="ExternalInput").ap()
    out_d = nc.dram_tensor("out", [NQ * 128, D], F32, kind="ExternalOutput").ap()
    xm_d = nc.dram_tensor("xmid", [NQ * 128, D], F32, kind="Internal").ap()
    WA = {}
    WA['rel_bias'] = nc.dram_tensor("rel_bias", [32, 24], F32, kind="ExternalInput").ap()
    WA['attn_w_in'] = nc.dram_tensor("attn_w_in", [D, 9216], F32, kind="ExternalInput").ap()
    WA['attn_w_out'] = nc.dram_tensor("attn_w_out", [1024, D], F32, kind="ExternalInput").ap()
    WA['ln_g'] = nc.dram_tensor("ln1_g", [D], F32, kind="ExternalInput").ap()
    WA['ln_b'] = nc.dram_tensor("ln1_b", [D], F32, kind="ExternalInput").ap()
    WM = _moe_W(nc)
    with ExitStack() as es:
        P = Prog(nc, es)
        cst = load_consts(P, nc, ['c_ident', 'c_uts', 'c_ones'])
        with ExitStack() as aes:
            emit_attn(P, nc, aes, NQ, xc, hv_d, WA, xm_d, cst)
        emit_moe(P, nc, es, NQ, C, xm_d, out_d, WM, cst)
        P.finish()
    return nc


def build_L3(NT, C):
    nc = bass.Bass("TRN2", target_bir_lowering=False)
    o_d = nc.dram_tensor("o_in", [NT * 128, 4096], BF16, kind="ExternalInput").ap()
    x_d = nc.dram_tensor("x_res", [NT * 128, D], F32, kind="ExternalInput").ap()
    out_d = nc.dram_tensor("out", [NT * 128, D], F32, kind="ExternalOutput").ap()
    xm_d = nc.dram_tensor("xmid", [NT * 128, D], F32, kind="Internal").ap()
    w_out = nc.dram_tensor("dn_w_out", [4096, D], F32, kind="ExternalInput").ap()
    ln_g = nc.dram_tensor("ln1_g", [D], F32, kind="ExternalInput").ap()
    ln_b = nc.dram_tensor("ln1_b", [D], F32, kind="ExternalInput").ap()
    WM = _moe_W(nc)
    with ExitStack() as es:
        P = Prog(nc, es)
        cst = load_consts(P, nc, ['c_ident', 'c_uts', 'c_ones'])
        emit_outproj(P, nc, NT, o_d, x_d, w_out, ln_g, ln_b, xm_d, cst)
        emit_moe(P, nc, es, NT, C, xm_d, out_d, WM, cst)
        P.finish()
    return nc


def _moe_in(layer, router_w, router_b, w_gate_up, b_gate_up, w_down, b_down, ln2_g, ln2_b):
    bgu_l = np.ascontiguousarray(b_gate_up[layer].reshape(NE, 32, 128).transpose(2, 0, 1).reshape(128, NE * 32))
    return {'router_w': router_w[layer], 'router_b': router_b[layer], 'w_gate_up': w_gate_up[layer], 'bgu_l': bgu_l,
            'w_down': w_down[layer], 'b_down': b_down[layer], 'ln2_g': ln2_g[layer], 'ln2_b': ln2_b[layer]}


NCORES = 8
SEQ = 16384
TPC = 4096
CAP = 640


def kernel(x, rel_bias, attn_w_in, attn_w_out, dn_w_in, dn_conv_w, dn_a_log, dn_dt_bias,
           dn_norm_g, dn_w_out, ln1_g, ln1_b, router_w, router_b, w_gate_up, b_gate_up,
           w_down, b_down, ln2_g, ln2_b):
    f32 = np.float32
    args = [x, rel_bias, attn_w_in, attn_w_out, dn_w_in, dn_conv_w, dn_a_log, dn_dt_bias, dn_norm_g, dn_w_out,
            ln1_g, ln1_b, router_w, router_b, w_gate_up, b_gate_up, w_down, b_down, ln2_g, ln2_b]
    (x, rel_bias, attn_w_in, attn_w_out, dn_w_in, dn_conv_w, dn_a_log, dn_dt_bias, dn_norm_g, dn_w_out,
     ln1_g, ln1_b, router_w, router_b, w_gate_up, b_gate_up, w_down, b_down, ln2_g, ln2_b) = [np.asarray(a, dtype=f32) for a in args]
    cst = consts_np()
    cst.update(dn_consts_np())
    NQ = TPC // 128
    cores = list(range(NCORES))
    c3 = {k: cst[k] for k in ('c_ident', 'c_uts', 'c_ones')}
    com = dict(c3, **attn_static(), rel_bias=rel_bias, attn_w_in=attn_w_in[0], attn_w_out=attn_w_out[0],
               ln1_g=ln1_g[0], ln1_b=ln1_b[0],
               **_moe_in(0, router_w, router_b, w_gate_up, b_gate_up, w_down, b_down, ln2_g, ln2_b))
    ims = []
    for c in cores:
        b, sg = c // 4, c % 4
        t0 = sg * TPC
        if sg == 0:
            xc = np.concatenate([np.zeros((HALO, D), f32), x[b, 0:TPC]], axis=0)
            hv = np.zeros((128, 1), f32)
        else:
            xc = np.ascontiguousarray(x[b, t0 - HALO:t0 + TPC])
            hv = np.ones((128, 1), f32)
        ims.append(dict(com, xc=xc, hv=hv))
    nc1 = build_L1(NQ, CAP)
    r1 = run_bass_kernel_spmd(nc1, ims, core_ids=cores)
    x1 = np.concatenate([np.asarray(r1.results[c]['out']) for c in cores], axis=0)
    del ims, r1
    c6 = {k: cst[k] for k in ('c_ident', 'c_uti', 'c_ones', 'c_negu', 'c_negl', 'c_strl')}
    ims = [dict(c6, x1=x1, **dn_inputs(c, dn_w_in[0], dn_conv_w[0], dn_a_log[0], dn_dt_bias[0], dn_norm_g[0])) for c in cores]
    nc2 = build_dn(2, SEQ // 128)
    r2 = run_bass_kernel_spmd(nc2, ims, core_ids=cores)
    o_full = np.concatenate([np.asarray(r2.results[c]['out']) for c in cores], axis=1)
    del ims, r2
    com = dict(c3, dn_w_out=dn_w_out[0], ln1_g=ln1_g[1], ln1_b=ln1_b[1],
               **_moe_in(1, router_w, router_b, w_gate_up, b_gate_up, w_down, b_down, ln2_g, ln2_b))
    ims = [dict(com, o_in=np.ascontiguousarray(o_full[c * TPC:(c + 1) * TPC]), x_res=np.ascontiguousarray(x1[c * TPC:(c + 1) * TPC])) for c in cores]
    nc3 = build_L3(NQ, CAP)
    r3 = run_bass_kernel_spmd(nc3, ims, core_ids=cores)
    out = np.concatenate([np.asarray(r3.results[c]['out']) for c in cores], axis=0)
    return out.reshape(2, SEQ, D).astype(f32)
```
